# Optimizing a Trainium2 kernel written in Bass

```python
import math
import jax
import jax.numpy as jnp
from jax import lax
import numpy as np

D_MODEL = 1024
BATCH = 1
SEQ = 16384
DEPTH = 2

GRID_W = 64
CTX_LEN = 256
EPS = 1e-6
N_MOD = 6

SSD_HEADS = 16
SSD_HEAD_DIM = 64
SSD_INNER = SSD_HEADS * SSD_HEAD_DIM
SSD_GROUPS = 2
SSD_STATE = 128
SSD_GN = SSD_GROUPS * SSD_STATE
SSD_CONV = 3
SSD_CHUNK = 128

DIFF_HEADS = 8
DIFF_HEAD_DIM = 64
DIFF_WIDTH = DIFF_HEADS * 2 * DIFF_HEAD_DIM
ROPE_BASE = 10000.0
Q_BLOCK = 128

CONF_WIDTH = D_MODEL
CONF_KERNEL = 31

N_BRANCHES = 3

N_EXPERTS = 16
EXPERT_FF = 2 * D_MODEL
CAPACITY_FACTOR = 2

COL_SIZES = (SSD_INNER, SSD_GN, 2 * SSD_HEADS, DIFF_WIDTH, DIFF_WIDTH, SSD_GN, DIFF_WIDTH, SSD_INNER, 2 * CONF_WIDTH, N_BRANCHES * D_MODEL)
N_COLS = sum(COL_SIZES)
N_STATE_COLS = sum(COL_SIZES[:5])

kernel_name = 'hybrid_ssd_diffattn_conformer_ecmoe_dit'


def split_last(a, sizes):
    idx = np.cumsum(sizes)[:-1].tolist()
    return jnp.split(a, idx, axis=-1)


def rms_norm(x, w, eps=EPS):
    xf = x.astype(jnp.float32)
    y = xf * lax.rsqrt(jnp.mean(xf * xf, axis=-1, keepdims=True) + eps)
    return (y * w.astype(jnp.float32)).astype(x.dtype)


def layer_norm(x, w, b, eps=EPS):
    xf = x.astype(jnp.float32)
    mu = jnp.mean(xf, axis=-1, keepdims=True)
    xc = xf - mu
    y = xc * lax.rsqrt(jnp.mean(xc * xc, axis=-1, keepdims=True) + eps)
    return (y * w.astype(jnp.float32) + b.astype(jnp.float32)).astype(x.dtype)


def modulate(x, w, shift, scale):
    return rms_norm(x, w) * (1 + scale) + shift


def flip_seq(a):
    return jnp.flip(a, axis=1)


def dwconv(u, w, b):
    k = w.shape[0]
    out = lax.conv_general_dilated(u, w.astype(u.dtype)[:, None, :], window_strides=(1,), padding=[(k // 2, k // 2)], dimension_numbers=('NWC', 'WIO', 'NWC'), feature_group_count=u.shape[-1])
    return out + b.astype(u.dtype)


def axial_rope_tables(n_rows):
    row = jnp.repeat(jnp.arange(n_rows, dtype=jnp.float32), GRID_W)
    col = jnp.tile(jnp.arange(GRID_W, dtype=jnp.float32), n_rows)
    half = DIFF_HEAD_DIM // 2
    freqs = ROPE_BASE ** (-jnp.arange(0, half, 2, dtype=jnp.float32) / half)
    ang = jnp.concatenate([row[:, None] * freqs, col[:, None] * freqs], axis=-1)
    return jnp.cos(ang), jnp.sin(ang)


def apply_axial_rope(t, cos, sin):
    n = t.shape[1]
    q4 = DIFF_HEAD_DIM // 4
    tr = t.reshape(t.shape[:-1] + (2, 2, q4))
    cs = cos.reshape(n, 1, 1, 2, q4).astype(t.dtype)
    sn = sin.reshape(n, 1, 1, 2, q4).astype(t.dtype)
    t1, t2 = tr[..., 0, :], tr[..., 1, :]
    out = jnp.stack([t1 * cs - t2 * sn, t2 * cs + t1 * sn], axis=-2)
    return out.reshape(t.shape)


def expand_groups(m):
    b, n = m.shape[0], m.shape[1]
    return jnp.repeat(m.reshape(b, n, SSD_GROUPS, SSD_STATE), SSD_HEADS // SSD_GROUPS, axis=2)


def ssd_steps(pdt, dt_bias, a_log):
    b, n = pdt.shape[0], pdt.shape[1]
    dt = jax.nn.softplus(pdt.reshape(b, n, 2, SSD_HEADS).astype(jnp.float32) + dt_bias.astype(jnp.float32))
    return dt, dt * (-jnp.exp(a_log.astype(jnp.float32)))


def ssd_scan(X, A, Bm, Cm, h0):
    b, l, h, p = X.shape
    n = Bm.shape[-1]
    nc, L = l // SSD_CHUNK, SSD_CHUNK
    Xc = X.reshape(b, nc, L, h, p)
    Bc = Bm.reshape(b, nc, L, h, n)
    Cc = Cm.reshape(b, nc, L, h, n)
    Acum = jnp.cumsum(A.astype(jnp.float32).reshape(b, nc, L, h), axis=2)
    seg = Acum[:, :, :, None, :] - Acum[:, :, None, :, :]
    lower = jnp.tril(jnp.ones((L, L), dtype=bool))[None, None, :, :, None]
    decay = jnp.exp(jnp.where(lower, seg, -jnp.inf))
    scores = jnp.einsum('bclhn,bcshn->bclsh', Cc, Bc) * decay
    y_diag = jnp.einsum('bclsh,bcshp->bclhp', scores, Xc)
    to_end = jnp.exp(Acum[:, :, -1:, :] - Acum)
    states = jnp.einsum('bclhn,bclh,bclhp->bchpn', Bc, to_end, Xc)
    chunk_decay = jnp.exp(Acum[:, :, -1, :])

    def step(hc, inp):
        st, dec = inp
        return hc * dec[:, :, None, None] + st, hc

    final, h_in = lax.scan(step, h0.astype(jnp.float32), (jnp.moveaxis(states, 1, 0), jnp.moveaxis(chunk_decay, 1, 0)))
    h_in = jnp.moveaxis(h_in, 0, 1)
    y_off = jnp.einsum('bclhn,bchpn,bclh->bclhp', Cc, h_in, jnp.exp(Acum))
    return (y_diag + y_off).reshape(b, l, h, p), final


def ssd_final_state(X, A, Bm):
    Acum = jnp.cumsum(A.astype(jnp.float32), axis=1)
    w = jnp.exp(Acum[:, -1:, :] - Acum)
    return jnp.einsum('blhn,blh,blhp->bhpn', Bm, w, X)


def ssd_bidirectional(xs, Bm, Cm, pdt, dt_bias, a_log, d_skip, h0_f, h0_b):
    b, n = xs.shape[0], xs.shape[1]
    xs = xs.reshape(b, n, SSD_HEADS, SSD_HEAD_DIM)
    Bh, Ch = expand_groups(Bm), expand_groups(Cm)
    dt, a = ssd_steps(pdt, dt_bias, a_log)
    y_f, s_f = ssd_scan(xs * dt[:, :, 0, :, None], a[:, :, 0], Bh, Ch, h0_f)
    y_b, s_b = ssd_scan(flip_seq(xs * dt[:, :, 1, :, None]), flip_seq(a[:, :, 1]), flip_seq(Bh), flip_seq(Ch), h0_b)
    y = y_f + flip_seq(y_b) + xs * d_skip[:, None].astype(xs.dtype)
    return y.reshape(b, n, SSD_INNER), s_f, s_b


def gated_norm(y, z, w):
    b, n = y.shape[0], y.shape[1]
    g = SSD_INNER // SSD_GROUPS
    yz = (y * jax.nn.silu(z)).reshape(b, n, SSD_GROUPS, g)
    return rms_norm(yz, w.reshape(SSD_GROUPS, g)).reshape(b, n, SSD_INNER)


def diff_attention(q, k, v, lam):
    b, n = q.shape[0], q.shape[1]
    nb = n // Q_BLOCK
    qb = jnp.moveaxis(q.reshape((b, nb, Q_BLOCK) + q.shape[2:]), 1, 0)
    scale = DIFF_HEAD_DIM ** -0.5

    def block(qi):
        s = jnp.einsum('bqhmd,bkhmd->bhmqk', qi, k, preferred_element_type=jnp.float32) * scale
        a = jax.nn.softmax(s, axis=-1)
        w = a[:, :, 0] - lam * a[:, :, 1]
        return jnp.einsum('bhqk,bkhe->bqhe', w.astype(v.dtype), v)

    o = lax.map(block, qb)
    return jnp.moveaxis(o, 0, 1).reshape((b, n) + o.shape[3:])


def conformer_conv(pglu, dw_w, dw_b, ln_w, ln_b, w_out):
    a, g = jnp.split(pglu, 2, axis=-1)
    u = dwconv(a * jax.nn.sigmoid(g), dw_w, dw_b)
    return jax.nn.silu(layer_norm(u, ln_w, ln_b)) @ w_out


def gated_merge(pgate, y_a, y_b, y_c, w_o):
    g = jax.nn.sigmoid(pgate.astype(jnp.float32)).reshape(pgate.shape[:-1] + (N_BRANCHES, D_MODEL))
    m = g[..., 0, :] * y_a + g[..., 1, :] * y_b + g[..., 2, :] * y_c
    return m.astype(w_o.dtype) @ w_o


def token_mixer(h, w_in, mp, h0_f, h0_b, kv_prefix, rope, lam, lam_init):
    (conv_w, conv_b, dt_bias, a_log, d_skip, ssd_norm_w, ssd_out, subln_w, diff_out, dw_w, dw_b, ln_w, ln_b, conf_out, w_o) = mp
    b, n = h.shape[0], h.shape[1]
    px, pB, pdt, pk, pv, pC, pq, pz, pglu, pgate = split_last(h @ w_in, COL_SIZES)
    xbc = jax.nn.silu(dwconv(jnp.concatenate([px, pB, pC], axis=-1), conv_w, conv_b))
    xs, Bm, Cm = split_last(xbc, (SSD_INNER, SSD_GN, SSD_GN))
    y_ssd, s_f, s_b = ssd_bidirectional(xs, Bm, Cm, pdt, dt_bias, a_log, d_skip, h0_f, h0_b)
    y_ssd = gated_norm(y_ssd, pz, ssd_norm_w) @ ssd_out
    q = pq.reshape(b, n, DIFF_HEADS, 2, DIFF_HEAD_DIM)
    k = pk.reshape(b, n, DIFF_HEADS, 2, DIFF_HEAD_DIM)
    v = pv.reshape(b, n, DIFF_HEADS, 2 * DIFF_HEAD_DIM)
    if rope is not None:
        q = apply_axial_rope(q, rope[0], rope[1])
        k = apply_axial_rope(k, rope[0], rope[1])
    if kv_prefix is None:
        k_all, v_all = k, v
    else:
        k_all = jnp.concatenate([kv_prefix[0], k], axis=1)
        v_all = jnp.concatenate([kv_prefix[1], v], axis=1)
    o = diff_attention(q, k_all, v_all, lam)
    y_diff = (rms_norm(o, subln_w) * (1 - lam_init)).reshape(b, n, DIFF_WIDTH) @ diff_out
    y_conf = conformer_conv(pglu, dw_w, dw_b, ln_w, ln_b, conf_out)
    return gated_merge(pgate, y_ssd, y_diff, y_conf, w_o), k, v, s_f, s_b


def context_state(h, w_in, conv_w, conv_b, dt_bias, a_log):
    b, n = h.shape[0], h.shape[1]
    px, pB, pdt, pk, pv = split_last(h @ w_in[:, :N_STATE_COLS], COL_SIZES[:5])
    nxb = SSD_INNER + SSD_GN
    xb = jax.nn.silu(dwconv(jnp.concatenate([px, pB], axis=-1), conv_w[:, :nxb], conv_b[:nxb]))
    xs, Bm = split_last(xb, (SSD_INNER, SSD_GN))
    xs = xs.reshape(b, n, SSD_HEADS, SSD_HEAD_DIM)
    Bh = expand_groups(Bm)
    dt, a = ssd_steps(pdt, dt_bias, a_log)
    s_f = ssd_final_state(xs * dt[:, :, 0, :, None], a[:, :, 0], Bh)
    s_b = ssd_final_state(flip_seq(xs * dt[:, :, 1, :, None]), flip_seq(a[:, :, 1]), flip_seq(Bh))
    k = pk.reshape(b, n, DIFF_HEADS, 2, DIFF_HEAD_DIM)
    v = pv.reshape(b, n, DIFF_HEADS, 2 * DIFF_HEAD_DIM)
    return k, v, s_f, s_b


def expert_choice_ffn(h, router_w, w1, w3, w2):
    n = h.shape[1]
    cap = CAPACITY_FACTOR * n // N_EXPERTS

    def route_set(hs):
        aff = jax.nn.softmax((hs @ router_w).astype(jnp.float32), axis=-1)
        g, idx = lax.top_k(aff.T, cap)
        xe = hs[idx]
        he = jax.nn.silu(jnp.einsum('ecd,edf->ecf', xe, w1)) * jnp.einsum('ecd,edf->ecf', xe, w3)
        ye = jnp.einsum('ecf,efd->ecd', he, w2) * g[..., None].astype(hs.dtype)
        return jnp.zeros_like(hs).at[idx.reshape(-1)].add(ye.reshape(-1, hs.shape[-1]).astype(hs.dtype))

    return jax.vmap(route_set)(h)


def setup_inputs(seed: int = 0) -> dict:
    key = jax.random.key(seed)
    keys = jax.random.split(key, 40)
    order = iter(range(40))

    def normal(shape, scale):
        return jax.random.normal(keys[next(order)], shape, jnp.float32) * scale

    def gain(shape):
        return 1.0 + normal(shape, 0.02)

    L, D, E, F = DEPTH, D_MODEL, N_EXPERTS, EXPERT_FF
    conv_ch = SSD_INNER + 2 * SSD_GN
    dt0 = jnp.exp(jax.random.uniform(keys[next(order)], (L, 2, SSD_HEADS), jnp.float32, minval=math.log(1e-3), maxval=math.log(1e-1)))
    a0 = jax.random.uniform(keys[next(order)], (L, 2, SSD_HEADS), jnp.float32, minval=1.0, maxval=16.0)
    return {
        'x': normal((BATCH, SEQ, D), 1.0),
        'c': normal((BATCH, D), 1.0),
        'ctx': normal((BATCH, CTX_LEN, D), 1.0),
        'c_ctx': normal((D,), 1.0),
        'ada_w': normal((L, D, N_MOD * D), 0.5 * D ** -0.5),
        'ada_b': normal((L, N_MOD * D), 0.02),
        'norm1_w': gain((L, D)),
        'norm2_w': gain((L, D)),
        'w_in': normal((L, D, N_COLS), D ** -0.5),
        'ssd_conv_w': normal((L, SSD_CONV, conv_ch), SSD_CONV ** -0.5),
        'ssd_conv_b': normal((L, conv_ch), 0.02),
        'ssd_dt_bias': dt0 + jnp.log(-jnp.expm1(-dt0)),
        'ssd_a_log': jnp.log(a0),
        'ssd_d': gain((L, SSD_HEADS)),
        'ssd_norm_w': gain((L, SSD_INNER)),
        'ssd_out': normal((L, SSD_INNER, D), SSD_INNER ** -0.5),
        'diff_lambda': normal((L, 4, DIFF_HEAD_DIM), 0.1),
        'diff_subln_w': gain((L, 2 * DIFF_HEAD_DIM)),
        'diff_out': normal((L, DIFF_WIDTH, D), DIFF_WIDTH ** -0.5),
        'conf_dw_w': normal((L, CONF_KERNEL, CONF_WIDTH), CONF_KERNEL ** -0.5),
        'conf_dw_b': normal((L, CONF_WIDTH), 0.02),
        'conf_ln_w': gain((L, CONF_WIDTH)),
        'conf_ln_b': normal((L, CONF_WIDTH), 0.02),
        'conf_out': normal((L, CONF_WIDTH, D), CONF_WIDTH ** -0.5),
        'w_o': normal((L, D, D), D ** -0.5),
        'router_w': normal((L, D, E), D ** -0.5),
        'exp_w1': normal((L, E, D, F), D ** -0.5),
        'exp_w3': normal((L, E, D, F), D ** -0.5),
        'exp_w2': normal((L, E, F, D), F ** -0.5),
        'final_norm_w': gain((D,)),
    }


def reference(x, c, ctx, c_ctx, ada_w, ada_b, norm1_w, norm2_w, w_in, ssd_conv_w, ssd_conv_b, ssd_dt_bias, ssd_a_log, ssd_d, ssd_norm_w, ssd_out, diff_lambda, diff_subln_w, diff_out, conf_dw_w, conf_dw_b, conf_ln_w, conf_ln_b, conf_out, w_o, router_w, exp_w1, exp_w3, exp_w2, final_norm_w):
    b = x.shape[0]
    n_rows = x.shape[1] // GRID_W
    rope = axial_rope_tables(n_rows)
    x_lat, x_ctx = x, ctx
    for l in range(DEPTH):
        last = l == DEPTH - 1
        lam_init = 0.8 - 0.6 * math.exp(-0.3 * l)
        lq1, lk1, lq2, lk2 = diff_lambda[l].astype(jnp.float32)
        lam = jnp.exp(jnp.sum(lq1 * lk1)) - jnp.exp(jnp.sum(lq2 * lk2)) + lam_init
        m_lat = jnp.split((jax.nn.silu(c) @ ada_w[l] + ada_b[l])[:, None, :], N_MOD, axis=-1)
        m_ctx = jnp.split((jax.nn.silu(c_ctx) @ ada_w[l] + ada_b[l])[None, None, :], N_MOD, axis=-1)
        mp = (ssd_conv_w[l], ssd_conv_b[l], ssd_dt_bias[l], ssd_a_log[l], ssd_d[l], ssd_norm_w[l], ssd_out[l], diff_subln_w[l], diff_out[l], conf_dw_w[l], conf_dw_b[l], conf_ln_w[l], conf_ln_b[l], conf_out[l], w_o[l])
        h_c = modulate(x_ctx, norm1_w[l], m_ctx[0], m_ctx[1])
        if last:
            k_c, v_c, s_f, s_b = context_state(h_c, w_in[l], ssd_conv_w[l], ssd_conv_b[l], ssd_dt_bias[l], ssd_a_log[l])
        else:
            zero = jnp.zeros((b, SSD_HEADS, SSD_HEAD_DIM, SSD_STATE), jnp.float32)
            y_c, k_c, v_c, s_f, s_b = token_mixer(h_c, w_in[l], mp, zero, zero, None, None, lam, lam_init)
            x_ctx = x_ctx + (m_ctx[2] * y_c).astype(x_ctx.dtype)
        h_l = modulate(x_lat, norm1_w[l], m_lat[0], m_lat[1])
        y_l = token_mixer(h_l, w_in[l], mp, s_f, s_b, (k_c, v_c), rope, lam, lam_init)[0]
        x_lat = x_lat + (m_lat[2] * y_l).astype(x_lat.dtype)
        h2 = modulate(x_lat, norm2_w[l], m_lat[3], m_lat[4])
        x_lat = x_lat + (m_lat[5] * expert_choice_ffn(h2, router_w[l], exp_w1[l], exp_w3[l], exp_w2[l])).astype(x_lat.dtype)
        if not last:
            h2c = modulate(x_ctx, norm2_w[l], m_ctx[3], m_ctx[4])
            x_ctx = x_ctx + (m_ctx[5] * expert_choice_ffn(h2c, router_w[l], exp_w1[l], exp_w3[l], exp_w2[l])).astype(x_ctx.dtype)
    return rms_norm(x_lat, final_norm_w)
```

```python
import math
from contextlib import ExitStack
import numpy as np
import concourse.bass as bass
import concourse.mybir as mybir

F32 = mybir.dt.float32
BF16 = mybir.dt.bfloat16
I32 = mybir.dt.int32
AF = mybir.ActivationFunctionType
ALU = mybir.AluOpType
AX = mybir.AxisListType

NDS = 48


class Trk:
    __slots__ = ("w", "r")

    def __init__(self):
        self.w = None
        self.r = {}


class Buf:
    def __init__(self, kb, t, name):
        self.kb, self.t, self.name = kb, t, name
        self.base = Trk()
        self.reg = {}

    def __getitem__(self, k):
        return self.t[k]

    def r(self, key):
        return (self, key)

    def view(self, ap):
        v = Buf(self.kb, ap, self.name)
        v.base, v.reg = self.base, self.reg
        return v


class KB:
    def __init__(self, nc):
        self.nc = nc
        self.es = ExitStack()
        self.eng = {"pe": nc.tensor, "act": nc.scalar, "dve": nc.vector, "pool": nc.gpsimd, "sp": nc.sync}
        self.sem = {k: self.es.enter_context(nc.semaphore("s_" + k)) for k in self.eng}
        self.cnt = {k: 0 for k in self.eng}
        self.seen = {k: {} for k in self.eng}
        self.dsem = [self.es.enter_context(nc.semaphore("d%d" % i)) for i in range(NDS)]
        self.dcnt = [0] * NDS
        self.dnext = 0
        self.nins = 0
        self.uid = 0
        self.freed = {}

    def _on_free(self, b):
        for tr in [b.base] + list(b.reg.values()):
            toks = list(tr.r.items())
            if tr.w is not None:
                toks.append(tr.w)
            for k, v in toks:
                if self.freed.get(k, 0) < v:
                    self.freed[k] = v

    def _new(self, stack, t, name):
        b = Buf(self, t, name)
        b.base.r = dict(self.freed)
        stack.callback(self._on_free, b)
        return b

    def sb(self, stack, shape, dtype, name=None):
        self.uid += 1
        name = "%s_%d" % (name or "t", self.uid)
        t = stack.enter_context(self.nc.sbuf_tensor(name, list(shape), dtype))
        return self._new(stack, t, name)

    def ps(self, stack, shape, dtype=F32, name=None):
        self.uid += 1
        name = "%s_%d" % (name or "p", self.uid)
        t = stack.enter_context(self.nc.psum_tensor(name, list(shape), dtype))
        return self._new(stack, t, name)

    def dram(self, name, shape, dtype, kind="Internal"):
        t = self.nc.dram_tensor(name, list(shape), dtype, kind=kind)
        return Buf(self, t.ap(), name)

    def _wait(self, e, tok):
        if tok is None:
            return
        key, val = tok
        if self.seen[e].get(key, 0) >= val:
            return
        sem = self.sem[key] if isinstance(key, str) else self.dsem[key[1]]
        self.eng[e].wait_ge(sem, val)
        self.nins += 1
        self.seen[e][key] = val

    @staticmethod
    def _norm(x):
        if isinstance(x, Buf):
            return (x, None)
        return x

    def _trks(self, b, key):
        if key is None:
            return [b.base] + list(b.reg.values())
        if key not in b.reg:
            b.reg[key] = Trk()
        return [b.base, b.reg[key]]

    def _deps(self, e, reads, writes, is_dma):
        toks = []
        for (b, key) in reads:
            for tr in self._trks(b, key):
                if tr.w is not None:
                    toks.append(tr.w)
        for (b, key) in writes:
            for tr in self._trks(b, key):
                if tr.w is not None:
                    if is_dma or tr.w[0] != e or e != "pe":
                        toks.append(tr.w)
                for k, v in tr.r.items():
                    toks.append((k, v))
        for tok in toks:
            self._wait(e, tok)

    def _mark(self, tok, reads, writes):
        for (b, key) in reads:
            if key is None:
                trs = [b.base]
            else:
                trs = [self._trks(b, key)[1]]
            for tr in trs:
                if tr.r.get(tok[0], 0) < tok[1]:
                    tr.r[tok[0]] = tok[1]
        for (b, key) in writes:
            if key is None:
                b.base.w = tok
                b.base.r = {}
                b.reg = {}
            else:
                tr = self._trks(b, key)[1]
                tr.w = tok
                tr.r = {}

    def op(self, e, fn, reads=(), writes=(), sig=True):
        reads = [self._norm(x) for x in reads]
        writes = [self._norm(x) for x in writes]
        self._deps(e, reads, writes, False)
        ins = fn(self.eng[e])
        self.nins += 1
        if sig:
            self.cnt[e] += 1
            ins.then_inc(self.sem[e], 1)
            tok = (e, self.cnt[e])
        else:
            tok = (e, self.cnt[e] + 1)
        self._mark(tok, reads, writes)
        return ins

    def dma(self, out, in_, reads=(), writes=(), q="sp", **kw):
        reads = [self._norm(x) for x in reads]
        writes = [self._norm(x) for x in writes]
        self._deps(q, reads, writes, True)
        s = self.dnext
        self.dnext = (self.dnext + 1) % NDS
        if self.dcnt[s] > 0:
            self._wait(q, (("d", s), self.dcnt[s]))
        ins = self.eng[q].dma_start(out=out, in_=in_, **kw)
        self.dcnt[s] += 16
        ins.then_inc(self.dsem[s], 16)
        self.nins += 1
        tok = (("d", s), self.dcnt[s])
        self._mark(tok, reads, writes)
        return ins

    def allgather(self, src, dst, n=8):
        q = "pool"
        reads, writes = [(src, None)], [(dst, None)]
        self._deps(q, reads, writes, True)
        s = self.dnext
        self.dnext = (self.dnext + 1) % NDS
        if self.dcnt[s] > 0:
            self._wait(q, (("d", s), self.dcnt[s]))
        ins = self.nc.gpsimd.collective_compute("AllGather", ALU.bypass, replica_groups=[list(range(n))],
                                                ins=[src.t.opt()], outs=[dst.t.opt()])
        self.dcnt[s] += 1
        ins.then_inc(self.dsem[s], 1)
        self.nins += 1
        self._mark((("d", s), self.dcnt[s]), reads, writes)

    def finish(self):
        for s in range(NDS):
            if self.dcnt[s] > 0:
                self._wait("sp", (("d", s), self.dcnt[s]))
        for e in ("pe", "act", "dve", "pool"):
            if self.cnt[e] > 0:
                self._wait("sp", (e, self.cnt[e]))
        self.es.close()


D = 1024
HALO = 16
EPS = 1e-6
C_X, C_B, C_DT, C_K, C_V, C_C, C_Q, C_Z, C_GLU, C_GATE = 0, 1024, 1280, 1312, 2336, 3360, 3616, 4640, 5664, 7712
NCOLS = 10784


def make_consts():
    c = {}
    c["ident"] = np.eye(128, dtype=np.float32)
    k = np.arange(128)
    c["triu"] = (k[:, None] <= k[None, :]).astype(np.float32)
    c["tril"] = (k[:, None] >= k[None, :]).astype(np.float32)
    c["negu"] = np.where(k[:, None] <= k[None, :], 0.0, -30000.0).astype(np.float32)
    c["negl"] = np.where(k[:, None] >= k[None, :], 0.0, -30000.0).astype(np.float32)
    c["ones"] = np.ones((128, 128), np.float32)
    blk = (k[:, None] // 8 == k[None, :] // 8).astype(np.float32)
    c["blk8"] = blk
    sel = np.zeros((128, 16), np.float32)
    sel[np.arange(16) * 8, np.arange(16)] = 1.0
    c["sel16"] = np.pad(sel, ((0, 0), (0, 112)))
    names = ["ident", "triu", "tril", "negu", "negl", "ones", "blk8", "sel16"]
    arr = np.concatenate([c[n] for n in names], axis=1)
    return names, arr


CONST_NAMES, CONST_ARR = make_consts()


class Consts:
    def __init__(self, kb, stack, cst_dram):
        n = len(CONST_NAMES)
        self.f = kb.sb(stack, [128, n * 128], F32, "cstf")
        self.b = kb.sb(stack, [128, n * 128], BF16, "cstb")
        kb.dma(self.f[:, :], cst_dram[:, :], reads=[cst_dram], writes=[self.f])
        kb.op("dve", lambda e: e.tensor_copy(out=self.b[:, :], in_=self.f[:, :]), reads=[self.f], writes=[self.b])
        self.idx = {nm: i for i, nm in enumerate(CONST_NAMES)}
        self.eps = kb.sb(stack, [128, 2], F32, "epsc")
        kb.op("dve", lambda e: e.memset(self.eps[:, 0:1], EPS), writes=[self.eps])
        kb.op("dve", lambda e: e.memset(self.eps[:, 1:2], 1.0), writes=[self.eps])

    def F(self, nm, rows=128, cols=128):
        i = self.idx[nm]
        return self.f[0:rows, i * 128:i * 128 + cols]

    def B(self, nm, rows=128, cols=128):
        i = self.idx[nm]
        return self.b[0:rows, i * 128:i * 128 + cols]


def phase_ada(kb, stack, cs, cc_d, adaw_d, adab_d, n1_d, n2_d):
    out = {}
    mods = kb.sb(stack, [128, 48, 2], F32, "mods")
    for nm in ("gw1", "gw2"):
        out[nm] = kb.sb(stack, [128, 8, 2], F32, nm)
    with ExitStack() as st:
        cT = kb.sb(st, [128, 2, 8], F32, "cT")
        sc = kb.sb(st, [128, 8, 2], F32, "sc")
        ab = kb.sb(st, [128, 48], F32, "ab")
        nw = kb.sb(st, [128, 2, 8], F32, "nw")
        for v in range(2):
            kb.dma(cT[:, v, :], cc_d[v, :].rearrange("(t p) -> p t", p=128), reads=[cc_d], writes=[cT],
                   allow_slow_non_contiguous=True)
        kb.dma(ab[:, :], adab_d[:].rearrange("(t p) -> p t", p=128), reads=[adab_d], writes=[ab],
               allow_slow_non_contiguous=True)
        kb.dma(nw[:, 0, :], n1_d[:].rearrange("(t p) -> p t", p=128), reads=[n1_d], writes=[nw],
               allow_slow_non_contiguous=True)
        kb.dma(nw[:, 1, :], n2_d[:].rearrange("(t p) -> p t", p=128), reads=[n2_d], writes=[nw],
               allow_slow_non_contiguous=True)
        kb.op("act", lambda e: e.activation(out=sc[:, :, :], in_=cT[:, :, :].rearrange("p v t -> p t v"), func=AF.Silu),
              reads=[cT], writes=[sc])
        pm = kb.ps(st, [128, 48, 2], F32, "pm")
        wb = [kb.sb(st, [128, 6144], F32, "adaw%d" % i) for i in range(2)]
        for dt in range(8):
            w = wb[dt % 2]
            kb.dma(w[:, :], adaw_d[dt * 128:(dt + 1) * 128, :], reads=[adaw_d], writes=[w])
            for ft in range(48):
                kb.op("pe", lambda e: e.matmul(pm[:, ft, :], lhsT=w[:, ft * 128:(ft + 1) * 128], rhs=sc[:, dt, :],
                                               start=(dt == 0 and ft == 0), stop=(dt == 7 and ft == 47), skip_group_check=True),
                      reads=[w, sc], writes=[pm], sig=(ft == 47))
        for v in range(2):
            kb.op("dve", lambda e: e.tensor_tensor(out=mods[:, :, v], in0=pm[:, :, v], in1=ab[:, :], op=ALU.add),
                  reads=[pm, ab], writes=[mods])
        for i, nm in enumerate(("gw1", "gw2")):
            sc0 = 8 + 24 * i
            for v in range(2):
                kb.op("dve", lambda e: e.scalar_tensor_tensor(out=out[nm][:, :, v], in0=mods[:, sc0:sc0 + 8, v], scalar=1.0,
                                                              in1=nw[:, i, :], op0=ALU.add, op1=ALU.mult),
                      reads=[mods, nw], writes=[out[nm]])
    out["mods"] = mods
    return out


def load_T(kb, st, dst, src_d, J, C, cs, pspool):
    with ExitStack() as s2:
        tmp = kb.sb(s2, [J, C], F32, "ldT")
        kb.dma(tmp[:, :], src_d[:, :], reads=[src_d], writes=[tmp])
        for ct in range(C // 128):
            kb.op("pe", lambda e: e.transpose(out=pspool[:, 0:J], in_=tmp[0:J, ct * 128:(ct + 1) * 128], identity=cs.F("ident", J, J)),
                  reads=[tmp, cs.f], writes=[pspool])
            kb.op("dve", lambda e: e.tensor_copy(out=dst[:, ct, 0:J], in_=pspool[:, 0:J]), reads=[pspool], writes=[dst])


class WLoader:
    def __init__(self, kb, st, kt=8, n=512, nbuf=2, nstg=None):
        self.kb = kb
        self.kt = kt
        self.nstg = nstg or nbuf
        self.stg = [kb.sb(st, [128, kt, n], F32, "wst") for _ in range(self.nstg)]
        self.wbf = [kb.sb(st, [128, kt, n], BF16, "wbf") for _ in range(nbuf)]
        self.i = 0
        self.j = 0
        self.nbuf = nbuf

    def load(self, w_d, r0, c0, n, q="sp"):
        kb = self.kb
        i = self.i
        self.i = (self.i + 1) % self.nbuf
        s, b = self.stg[self.j], self.wbf[i]
        self.j = (self.j + 1) % self.nstg
        kb.dma(s[:, :, 0:n], w_d[r0:r0 + self.kt * 128, c0:c0 + n].rearrange("(t p) c -> p t c", p=128),
               reads=[w_d], writes=[s], q=q)
        kb.op("pool", lambda e: e.tensor_copy(out=b[:, :, 0:n], in_=s[:, :, 0:n]), reads=[s], writes=[b])
        return b, s


def blocks(t0, t1, n=512):
    out = []
    while t0 < t1:
        m = min(n, t1 - t0)
        out.append((t0, m))
        t0 += m
    return out


def phase_a(kb, cs, A, v, T, xh_d, hm_d, W, O, rope=None, glu_split=1, stop=99):
    TE = T + 2 * HALO
    NT = T // 128
    with ExitStack() as st:
        hT = kb.sb(st, [128, 8, TE], BF16, "hT")
        hm = kb.sb(st, [128, 2], F32, "hm")
        kb.dma(hm[:, :], hm_d[:, :], reads=[hm_d], writes=[hm])
        mods, gw1 = A["mods"], A["gw1"]
        with ExitStack() as s1:
            xb = [kb.sb(s1, [128, 1024], F32, "xb") for _ in range(2)]
            junk = kb.sb(s1, [128, 1024], F32, "junk")
            xn = [kb.sb(s1, [128, 1024], BF16, "xn") for _ in range(2)]
            ss = [kb.sb(s1, [128, 2], F32, "ss") for _ in range(2)]
            pt = [kb.ps(s1, [128, 8, 128], BF16, "pt") for _ in range(2)]
            for i in range((TE + 127) // 128):
                r = min(128, TE - i * 128)
                x_, xn_, ss_, pt_ = xb[i % 2], xn[i % 2], ss[i % 2], pt[i % 2]
                kb.dma(x_[0:r, :], xh_d[i * 128:i * 128 + r, :], reads=[xh_d], writes=[x_])
                kb.op("dve", lambda e: e.memset(ss_[:, :], 0.0), writes=[ss_])
                kb.op("act", lambda e: e.activation(out=junk[0:r, :], in_=x_[0:r, :], func=AF.Square, accum_out=ss_[0:r, 0:1]),
                      reads=[x_], writes=[junk, ss_])
                kb.op("act", lambda e: e.activation(out=ss_[0:r, 1:2], in_=ss_[0:r, 0:1], func=AF.Sqrt, scale=1.0 / D, bias=cs.eps[0:r, 0:1]),
                      reads=[ss_, cs.eps], writes=[ss_])
                kb.op("dve", lambda e: e.reciprocal(out=ss_[0:r, 0:1], in_=ss_[0:r, 1:2]), reads=[ss_], writes=[ss_])
                kb.op("dve", lambda e: e.tensor_scalar(out=xn_[0:r, :], in0=x_[0:r, :], scalar1=ss_[0:r, 0:1], scalar2=None,
                                                       op0=ALU.mult), reads=[x_, ss_], writes=[xn_])
                for dt in range(8):
                    kb.op("pe", lambda e: e.transpose(out=pt_[:, dt, 0:r], in_=xn_[0:r, dt * 128:(dt + 1) * 128],
                                                      identity=cs.B("ident", r, r)), reads=[xn_, cs.b], writes=[pt_], sig=(dt == 7))
                for dt in range(8):
                    kb.op("act", lambda e: e.activation(out=hT[:, dt, i * 128:i * 128 + r], in_=pt_[:, dt, 0:r], func=AF.Identity,
                                                        scale=gw1[:, dt, v:v + 1], bias=mods[:, dt, v:v + 1]),
                          reads=[pt_, gw1, mods], writes=[hT.r(i)])
        win = W["w_in"]
        wl = WLoader(kb, st)
        psA = [kb.ps(st, [128, 512], F32, "psA") for _ in range(2)]
        psB = [kb.ps(st, [128, 512], F32, "psB") for _ in range(2)]
        pcnt = [0]

        def fm(ps, wb, cloc, ncol, t0, n):
            for dt in range(8):
                kb.op("pe", lambda e: e.matmul(ps[0:ncol, 0:n], lhsT=wb[:, dt, cloc:cloc + ncol], rhs=hT[:, dt, t0:t0 + n],
                                               start=(dt == 0), stop=(dt == 7)), reads=[wb, hT], writes=[ps], sig=(dt == 7))

        def tm(ps, wb, tt0, n):
            for dt in range(8):
                kb.op("pe", lambda e: e.matmul(ps[:, 0:n], lhsT=hT[:, dt, tt0:tt0 + 128], rhs=wb[:, dt, 0:n],
                                               start=(dt == 0), stop=(dt == 7)), reads=[wb, hT], writes=[ps], sig=(dt == 7))

        def nps(pool):
            pcnt[0] += 1
            return pool[pcnt[0] % 2]

        cwb = kb.sb(st, [128, 12, 4], F32, "cwb")
        load_T(kb, st, cwb, W["convp"], 4, 1536, cs, psA[0])
        if stop <= 0:
            return
        with ExitStack() as s2:
            Prow = [kb.sb(s2, [128, TE], F32, "Prow") for _ in range(2)]
            acc = kb.sb(s2, [128, T], F32, "acc")
            U = [kb.sb(s2, [128, T], BF16, "U") for _ in range(2)]
            Xtm = kb.sb(s2, [128, NT, 1024], BF16, "Xtm")
            Btm = kb.sb(s2, [128, NT, 256], BF16, "Btm")
            ptb = [kb.ps(s2, [128, 8, 128], BF16, "ptb") for _ in range(2)]
            k = 0
            for (gname, c0, ng, cch0) in (("x", C_X, 1024, 0), ("b", C_B, 256, 1024), ("c", C_C, 256, 1280)):
                for cc0 in range(0, ng, 512):
                    n = min(512, ng - cc0)
                    wb, _ = wl.load(win, 0, c0 + cc0, n)
                    for ci in range(n // 128):
                        ctg = (cc0 + ci * 128) // 128
                        cch = (cch0 // 128) + ctg
                        P_, U_ = Prow[k % 2], U[k % 2]
                        k += 1
                        for (t0, nn) in blocks(0, TE):
                            ps = nps(psA)
                            fm(ps, wb, ci * 128, 128, t0, nn)
                            kb.op("act", lambda e: e.copy(out=P_[:, t0:t0 + nn], in_=ps[:, 0:nn]), reads=[ps], writes=[P_])
                        kb.op("dve", lambda e: e.tensor_scalar(out=P_[:, 0:HALO], in0=P_[:, 0:HALO], scalar1=hm[:, 0:1], scalar2=None,
                                                               op0=ALU.mult), reads=[P_, hm], writes=[P_])
                        kb.op("dve", lambda e: e.tensor_scalar(out=P_[:, TE - HALO:TE], in0=P_[:, TE - HALO:TE], scalar1=hm[:, 1:2],
                                                               scalar2=None, op0=ALU.mult), reads=[P_, hm], writes=[P_])
                        kb.op("dve", lambda e: e.tensor_scalar(out=acc[:, :], in0=P_[:, 15:15 + T], scalar1=cwb[:, cch, 0:1], scalar2=None,
                                                               op0=ALU.mult), reads=[P_, cwb], writes=[acc])
                        for j in (1, 2):
                            kb.op("dve", lambda e: e.scalar_tensor_tensor(out=acc[:, :], in0=P_[:, 15 + j:15 + j + T],
                                                                          scalar=cwb[:, cch, j:j + 1], in1=acc[:, :],
                                                                          op0=ALU.mult, op1=ALU.add), reads=[P_, cwb, acc], writes=[acc])
                        kb.op("act", lambda e: e.activation(out=U_[:, :], in_=acc[:, :], func=AF.Silu, bias=cwb[:, cch, 3:4]),
                              reads=[acc, cwb], writes=[U_])
                        if gname in ("b", "c"):
                            od = O["BT"] if gname == "b" else O["CT"]
                            kb.dma(od[ctg * 128:(ctg + 1) * 128, :], U_[:, :], reads=[U_], writes=[od.r(ctg)])
                        if gname in ("x", "b"):
                            dst = Xtm if gname == "x" else Btm
                            for tt0 in range(0, NT, 8):
                                nt = min(8, NT - tt0)
                                p_ = nps(ptb)
                                for tt in range(nt):
                                    kb.op("pe", lambda e: e.transpose(out=p_[:, tt, :], in_=U_[:, (tt0 + tt) * 128:(tt0 + tt + 1) * 128],
                                                                      identity=cs.B("ident")), reads=[U_, cs.b], writes=[p_], sig=(tt == nt - 1))
                                kb.op("dve", lambda e: e.tensor_copy(out=dst[:, tt0:tt0 + nt, ctg * 128:(ctg + 1) * 128], in_=p_[:, 0:nt, :]),
                                      reads=[p_], writes=[dst])
            kb.dma(O["X"][:, :].rearrange("(t p) c -> p t c", p=128), Xtm[:, :, :], reads=[Xtm], writes=[O["X"]])
            kb.dma(O["B"][:, :].rearrange("(t p) c -> p t c", p=128), Btm[:, :, :], reads=[Btm], writes=[O["B"]])
        if stop <= 1:
            return
        with ExitStack() as s2:
            dtp = kb.sb(s2, [32, 4], F32, "dtp")
            kb.dma(dtp[:, 0:2], W["dtp"][:, :], reads=[W["dtp"]], writes=[dtp])
            kb.op("act", lambda e: e.activation(out=dtp[:, 2:3], in_=dtp[:, 1:2], func=AF.Exp), reads=[dtp], writes=[dtp])
            kb.op("dve", lambda e: e.tensor_scalar(out=dtp[:, 3:4], in0=dtp[:, 2:3], scalar1=-1.0, scalar2=None, op0=ALU.mult),
                  reads=[dtp], writes=[dtp])
            ex = kb.sb(s2, [32, T], F32, "ex")
            dta = kb.sb(s2, [32, 2, T], F32, "dta")
            DTtm = kb.sb(s2, [128, NT, 64], F32, "DTtm")
            wb, _ = wl.load(win, 0, C_DT, 32)
            for (t0, nn) in blocks(HALO, HALO + T):
                ps = nps(psA)
                fm(ps, wb, 0, 32, t0, nn)
                kb.op("act", lambda e: e.activation(out=ex[:, t0 - HALO:t0 - HALO + nn], in_=ps[0:32, 0:nn], func=AF.Exp, bias=dtp[:, 0:1]),
                      reads=[ps, dtp], writes=[ex])
            kb.op("act", lambda e: e.activation(out=dta[:, 0, :], in_=ex[:, :], func=AF.Ln, bias=cs.eps[0:32, 1:2]), reads=[ex, cs.eps], writes=[dta])
            kb.op("dve", lambda e: e.tensor_scalar(out=dta[:, 1, :], in0=dta[:, 0, :], scalar1=dtp[:, 3:4], scalar2=None, op0=ALU.mult),
                  reads=[dta, dtp], writes=[dta])
            for tt in range(NT):
                ps = nps(psA)
                for j in range(2):
                    kb.op("pe", lambda e: e.transpose(out=ps[:, j * 32:(j + 1) * 32], in_=dta[:, j, tt * 128:(tt + 1) * 128],
                                                      identity=cs.F("ident", 32, 32)), reads=[dta, cs.f], writes=[ps])
                kb.op("dve", lambda e: e.tensor_copy(out=DTtm[:, tt, :], in_=ps[:, 0:64]), reads=[ps], writes=[DTtm])
            kb.dma(O["DTA"][:, :].rearrange("(t p) c -> p t c", p=128), DTtm[:, :, :], reads=[DTtm], writes=[O["DTA"]])
        if stop <= 2:
            return
        with ExitStack() as s2:
            if rope is not None:
                cos = kb.sb(s2, [128, T], F32, "cos")
                sin = kb.sb(s2, [128, T], F32, "sin")
                kb.dma(cos[:, :], rope[0][:, :], reads=[rope[0]], writes=[cos])
                kb.dma(sin[:, :], rope[1][:, :], reads=[rope[1]], writes=[sin])
                wrot = kb.sb(s2, [128, 8, 512], BF16, "wrot")
                t1 = kb.sb(s2, [128, 512], F32, "rt1")
                t2 = kb.sb(s2, [128, 512], F32, "rt2")
            R = [kb.sb(s2, [128, T], BF16, "R") for _ in range(2)]
            k = 0
            for (c0, od) in ((C_K, O["KT"]), (C_Q, O["QT"])):
                for cc0 in range(0, 1024, 512):
                    wb, stg = wl.load(win, 0, c0 + cc0, 512)
                    if rope is not None:
                        sv = stg[:, :, :].rearrange("p t (g h f) -> p t g h f", h=2, f=16)
                        rv = wrot[:, :, :].rearrange("p t (g h f) -> p t g h f", h=2, f=16)
                        for h in range(2):
                            kb.op("pool", lambda e: e.tensor_copy(out=rv[:, :, :, h, :], in_=sv[:, :, :, 1 - h, :]), reads=[stg], writes=[wrot])
                    for ci in range(4):
                        ht = (cc0 // 128) + ci
                        R_ = R[k % 2]
                        k += 1
                        for (t0, nn) in blocks(HALO, HALO + T):
                            o0 = t0 - HALO
                            ps = nps(psA)
                            fm(ps, wb, ci * 128, 128, t0, nn)
                            if rope is None:
                                kb.op("act", lambda e: e.copy(out=R_[:, o0:o0 + nn], in_=ps[:, 0:nn]), reads=[ps], writes=[R_])
                            else:
                                ps2 = nps(psB)
                                fm(ps2, wrot, ci * 128, 128, t0, nn)
                                kb.op("dve", lambda e: e.tensor_tensor(out=t1[:, 0:nn], in0=ps[:, 0:nn], in1=cos[:, o0:o0 + nn], op=ALU.mult),
                                      reads=[ps, cos], writes=[t1])
                                kb.op("dve", lambda e: e.tensor_tensor(out=t2[:, 0:nn], in0=ps2[:, 0:nn], in1=sin[:, o0:o0 + nn], op=ALU.mult),
                                      reads=[ps2, sin], writes=[t2])
                                kb.op("pool", lambda e: e.tensor_tensor(out=R_[:, o0:o0 + nn], in0=t1[:, 0:nn], in1=t2[:, 0:nn], op=ALU.add),
                                      reads=[t1, t2], writes=[R_])
                        kb.dma(od[ht * 128:(ht + 1) * 128, :], R_[:, :], reads=[R_], writes=[od.r(ht)])
        if stop <= 3:
            return
        with ExitStack() as s2:
            Vtm = kb.sb(s2, [128, NT, 1024], BF16, "Vtm")
            for (c0, od, fn) in ((C_V, O["V"], None), (C_Z, O["ZS"], AF.Silu)):
                for cc0 in range(0, 1024, 512):
                    wb, _ = wl.load(win, 0, c0 + cc0, 512)
                    for tt in range(NT):
                        ps = nps(psA)
                        tm(ps, wb, HALO + tt * 128, 512)
                        if fn is None:
                            kb.op("act", lambda e: e.copy(out=Vtm[:, tt, cc0:cc0 + 512], in_=ps[:, :]), reads=[ps], writes=[Vtm])
                        else:
                            kb.op("act", lambda e: e.activation(out=Vtm[:, tt, cc0:cc0 + 512], in_=ps[:, :], func=fn), reads=[ps], writes=[Vtm])
                kb.dma(od[:, :].rearrange("(t p) c -> p t c", p=128), Vtm[:, :, :], reads=[Vtm], writes=[od])
        if stop <= 4:
            return
        with ExitStack() as s2:
            Gb = [kb.sb(s2, [128, T], BF16, "Gb") for _ in range(2)]
            k = 0
            for cc0 in range(0, 3072, 512):
                wb, _ = wl.load(win, 0, C_GATE + cc0, 512)
                for ci in range(4):
                    ct = cc0 // 128 + ci
                    G_ = Gb[k % 2]
                    k += 1
                    for (t0, nn) in blocks(HALO, HALO + T):
                        ps = nps(psA)
                        fm(ps, wb, ci * 128, 128, t0, nn)
                        kb.op("act", lambda e: e.activation(out=G_[:, t0 - HALO:t0 - HALO + nn], in_=ps[:, 0:nn], func=AF.Sigmoid),
                              reads=[ps], writes=[G_])
                    kb.dma(O["G"][ct * 128:(ct + 1) * 128, :], G_[:, :], reads=[G_], writes=[O["G"].r(ct)])
        if stop <= 5:
            return
        with ExitStack() as s2:
            cfp = kb.sb(s2, [128, 8, 34], F32, "cfp")
            load_T(kb, s2, cfp, W["confp"], 34, 1024, cs, psA[0])
            TH = T // glu_split
            THE = TH + 2 * HALO
            Urow = [kb.sb(s2, [128, THE], F32, "Urow") for _ in range(2)]
            sig = kb.sb(s2, [128, 512], F32, "sig")
            accA = kb.sb(s2, [128, TH], F32, "accA")
            accB = kb.sb(s2, [128, TH], F32, "accB")
            CV = kb.sb(s2, [128, 8, TH], F32, "CV")
            sq = kb.sb(s2, [128, 512], F32, "sq")
            mean = kb.sb(s2, [128, 512], F32, "mean")
            rstd = kb.sb(s2, [128, 512], F32, "rstd")
            tmpn = kb.sb(s2, [128, 512], F32, "tmpn")
            UcT = kb.sb(s2, [128, 8, 512], BF16, "UcT")
            yo = [kb.sb(s2, [128, 512], F32, "yo") for _ in range(2)]
            k = 0
            for hh in range(glu_split):
                base = hh * TH
                for half in range(2):
                    wa, _ = wl.load(win, 0, C_GLU + half * 512, 512)
                    wg, _ = wl.load(win, 0, C_GLU + 1024 + half * 512, 512)
                    for ci in range(4):
                        ct = half * 4 + ci
                        U_ = Urow[k % 2]
                        k += 1
                        for (t0, nn) in blocks(0, THE):
                            pa_, pg_ = nps(psA), nps(psB)
                            fm(pa_, wa, ci * 128, 128, base + t0, nn)
                            fm(pg_, wg, ci * 128, 128, base + t0, nn)
                            kb.op("act", lambda e: e.activation(out=sig[:, 0:nn], in_=pg_[:, 0:nn], func=AF.Sigmoid), reads=[pg_], writes=[sig])
                            kb.op("dve", lambda e: e.tensor_tensor(out=U_[:, t0:t0 + nn], in0=pa_[:, 0:nn], in1=sig[:, 0:nn], op=ALU.mult),
                                  reads=[pa_, sig], writes=[U_])
                        if hh == 0:
                            kb.op("dve", lambda e: e.tensor_scalar(out=U_[:, 0:HALO], in0=U_[:, 0:HALO], scalar1=hm[:, 0:1], scalar2=None,
                                                                   op0=ALU.mult), reads=[U_, hm], writes=[U_])
                        if hh == glu_split - 1:
                            kb.op("dve", lambda e: e.tensor_scalar(out=U_[:, THE - HALO:THE], in0=U_[:, THE - HALO:THE], scalar1=hm[:, 1:2],
                                                                   scalar2=None, op0=ALU.mult), reads=[U_, hm], writes=[U_])
                        kb.op("dve", lambda e: e.tensor_scalar(out=accA[:, :], in0=U_[:, 1:1 + TH], scalar1=cfp[:, ct, 0:1], scalar2=cfp[:, ct, 31:32],
                                                               op0=ALU.mult, op1=ALU.add), reads=[U_, cfp], writes=[accA])
                        for j in range(1, 31):
                            dst = CV[:, ct, :] if j == 30 else accA[:, :]
                            kb.op("dve", lambda e: e.scalar_tensor_tensor(out=dst, in0=U_[:, 1 + j:1 + j + TH], scalar=cfp[:, ct, j:j + 1],
                                                                          in1=accA[:, :], op0=ALU.mult, op1=ALU.add),
                                  reads=[U_, cfp, accA], writes=[accA, CV])
                for (t0, nn) in blocks(0, TH):
                    p1, p2 = nps(psA), nps(psB)
                    for ct in range(8):
                        kb.op("act", lambda e: e.activation(out=sq[:, 0:nn], in_=CV[:, ct, t0:t0 + nn], func=AF.Square), reads=[CV], writes=[sq])
                        kb.op("pe", lambda e: e.matmul(p1[:, 0:nn], lhsT=cs.F("ones"), rhs=CV[:, ct, t0:t0 + nn], start=(ct == 0), stop=(ct == 7)),
                              reads=[CV, cs.f], writes=[p1])
                        kb.op("pe", lambda e: e.matmul(p2[:, 0:nn], lhsT=cs.F("ones"), rhs=sq[:, 0:nn], start=(ct == 0), stop=(ct == 7)),
                              reads=[sq, cs.f], writes=[p2])
                    kb.op("dve", lambda e: e.tensor_scalar(out=mean[:, 0:nn], in0=p1[:, 0:nn], scalar1=1.0 / 1024, scalar2=None, op0=ALU.mult),
                          reads=[p1], writes=[mean])
                    kb.op("dve", lambda e: e.tensor_tensor(out=tmpn[:, 0:nn], in0=mean[:, 0:nn], in1=mean[:, 0:nn], op=ALU.mult),
                          reads=[mean], writes=[tmpn])
                    kb.op("dve", lambda e: e.scalar_tensor_tensor(out=rstd[:, 0:nn], in0=p2[:, 0:nn], scalar=1.0 / 1024, in1=tmpn[:, 0:nn],
                                                                  op0=ALU.mult, op1=ALU.subtract), reads=[p2, tmpn], writes=[rstd])
                    kb.op("act", lambda e: e.activation(out=tmpn[:, 0:nn], in_=rstd[:, 0:nn], func=AF.Sqrt, bias=cs.eps[:, 0:1]),
                          reads=[rstd, cs.eps], writes=[tmpn])
                    kb.op("dve", lambda e: e.reciprocal(out=rstd[:, 0:nn], in_=tmpn[:, 0:nn]), reads=[tmpn], writes=[rstd])
                    for ct in range(8):
                        kb.op("dve", lambda e: e.tensor_tensor(out=tmpn[:, 0:nn], in0=CV[:, ct, t0:t0 + nn], in1=mean[:, 0:nn], op=ALU.subtract),
                              reads=[CV, mean], writes=[tmpn])
                        kb.op("dve", lambda e: e.tensor_tensor(out=sq[:, 0:nn], in0=tmpn[:, 0:nn], in1=rstd[:, 0:nn], op=ALU.mult),
                              reads=[tmpn, rstd], writes=[sq])
                        kb.op("act", lambda e: e.activation(out=UcT[:, ct, 0:nn], in_=sq[:, 0:nn], func=AF.Silu, scale=cfp[:, ct, 32:33],
                                                            bias=cfp[:, ct, 33:34]), reads=[sq, cfp], writes=[UcT])
                    for half in range(2):
                        wc, _ = wl.load(W["conf_out"], 0, half * 512, 512)
                        for ci in range(4):
                            dm = half * 4 + ci
                            ps = nps(psA)
                            for ct in range(8):
                                kb.op("pe", lambda e: e.matmul(ps[:, 0:nn], lhsT=wc[:, ct, ci * 128:(ci + 1) * 128], rhs=UcT[:, ct, 0:nn],
                                                               start=(ct == 0), stop=(ct == 7)), reads=[wc, UcT], writes=[ps], sig=(ct == 7))
                            y_ = yo[k % 2]
                            k += 1
                            kb.op("act", lambda e: e.copy(out=y_[:, 0:nn], in_=ps[:, 0:nn]), reads=[ps], writes=[y_])
                            kb.dma(O["YcT"][dm * 128:(dm + 1) * 128, base + t0:base + t0 + nn], y_[:, 0:nn], reads=[y_],
                                   writes=[O["YcT"].r((dm, hh, t0))])


def ssd_scan(kb, cs, st, T, I, Yd, hstate, first):
    NC = T // 128
    with ExitStack() as s:
        Yacc = kb.sb(s, [128, NC, 128], F32, "Yacc")
        dsk = kb.sb(s, [128, 2], F32, "dsk")
        kb.dma(dsk[:, :], I["dsk"][:, :], reads=[I["dsk"]], writes=[dsk])
        Xc = [kb.sb(s, [128, 128], BF16, "Xc") for _ in range(2)]
        Bc = [kb.sb(s, [128, 128], BF16, "Bc") for _ in range(2)]
        BTc = [kb.sb(s, [128, 128], BF16, "BTc") for _ in range(2)]
        CTc = [kb.sb(s, [128, 128], BF16, "CTc") for _ in range(2)]
        dta = [kb.sb(s, [128, 8], F32, "dta") for _ in range(2)]
        cum = kb.sb(s, [128, 16], F32, "cum")
        tot = kb.sb(s, [128, 8], F32, "tot")
        aT = kb.sb(s, [128, 128], F32, "aT")
        decT = kb.sb(s, [128, 128], F32, "decT")
        WT = kb.sb(s, [128, 128], BF16, "WT")
        Xdt = kb.sb(s, [128, 64], BF16, "Xdt")
        Xw = kb.sb(s, [128, 64], BF16, "Xw")
        hbf = kb.sb(s, [128, 64], BF16, "hbf")
        ytmp = kb.sb(s, [128, 64], F32, "ytmp")
        p_sc = kb.ps(s, [128, 512], F32, "p_sc")
        p_cum = kb.ps(s, [128, 512], F32, "p_cum")
        p_dec = kb.ps(s, [128, 512], F32, "p_dec")
        p_yd = kb.ps(s, [128, 512], F32, "p_yd")
        p_yo = kb.ps(s, [128, 512], F32, "p_yo")
        p_st = kb.ps(s, [128, 512], F32, "p_st")
        it = 0
        for d in range(2):
            tri = "triu" if d == 0 else "tril"
            neg = "negu" if d == 0 else "negl"
            order = range(NC) if d == 0 else range(NC - 1, -1, -1)
            for c in order:
                b = it % 2
                it += 1
                X_, B_, BT_, CT_, dta_ = Xc[b], Bc[b], BTc[b], CTc[b], dta[b]
                r0 = c * 128
                if "load" in I:
                    I["load"](c, X_, B_, BT_, CT_, dta_)
                else:
                    kb.dma(X_[:, :], I["X"][r0:r0 + 128, :], reads=[I["X"]], writes=[X_])
                    kb.dma(B_[:, :], I["B"][r0:r0 + 128, :], reads=[I["B"]], writes=[B_])
                    kb.dma(BT_[:, :], I["BT"][:, r0:r0 + 128], reads=[I["BT"]], writes=[BT_])
                    kb.dma(CT_[:, :], I["CT"][:, r0:r0 + 128], reads=[I["CT"]], writes=[CT_])
                    kb.dma(dta_[:, :], I["DTA"][r0:r0 + 128, :], reads=[I["DTA"]], writes=[dta_])
                acol = 4 + 2 * d
                dcol = 2 * d
                kb.op("pe", lambda e: e.matmul(p_cum[:, 0:2], lhsT=cs.F(tri), rhs=dta_[:, acol:acol + 2], start=True, stop=True),
                      reads=[cs.f, dta_], writes=[p_cum])
                kb.op("pe", lambda e: e.matmul(p_cum[:, 2:4], lhsT=cs.F("ones"), rhs=dta_[:, acol:acol + 2], start=True, stop=True), reads=[cs.f, dta_], writes=[p_cum])
                kb.op("dve", lambda e: e.tensor_copy(out=cum[:, 0:2], in_=p_cum[:, 0:2]), reads=[p_cum], writes=[cum])
                kb.op("dve", lambda e: e.tensor_scalar(out=cum[:, 4:6], in0=p_cum[:, 0:2], scalar1=-1.0, scalar2=None, op0=ALU.mult),
                      reads=[p_cum], writes=[cum])
                kb.op("act", lambda e: e.activation(out=cum[:, 8:10], in_=p_cum[:, 0:2], func=AF.Exp), reads=[p_cum], writes=[cum])
                kb.op("dve", lambda e: e.tensor_tensor(out=tot[:, 0:2], in0=p_cum[:, 2:4], in1=cum[:, 0:2], op=ALU.subtract),
                      reads=[p_cum, cum], writes=[tot])
                kb.op("act", lambda e: e.activation(out=cum[:, 12:14], in_=tot[:, 0:2], func=AF.Exp), reads=[tot], writes=[cum])
                kb.op("act", lambda e: e.activation(out=tot[:, 4:6], in_=p_cum[:, 2:4], func=AF.Exp), reads=[p_cum], writes=[tot])
                kb.op("pe", lambda e: e.matmul(p_sc[:, 0:128], lhsT=BT_[:, :], rhs=CT_[:, :], start=True, stop=True), reads=[BT_, CT_], writes=[p_sc])
                for h in range(2):
                    hs = hstate[:, d, h, :]
                    kb.op("dve", lambda e: e.tensor_scalar(out=aT[:, :], in0=cs.F(tri), scalar1=dta_[:, acol + h:acol + h + 1], scalar2=None,
                                                           op0=ALU.mult), reads=[cs.f, dta_], writes=[aT])
                    kb.op("pe", lambda e: e.matmul(p_dec[:, 0:128], lhsT=cs.F("ones"), rhs=aT[:, :], start=True, stop=False), reads=[cs.f, aT], writes=[p_dec])
                    kb.op("pe", lambda e: e.matmul(p_dec[:, 0:128], lhsT=cs.F("ident"), rhs=cs.F(neg), start=False, stop=True), reads=[cs.f], writes=[p_dec])
                    kb.op("act", lambda e: e.activation(out=decT[:, :], in_=p_dec[:, 0:128], func=AF.Exp, bias=cum[:, 4 + h:5 + h]),
                          reads=[p_dec, cum], writes=[decT])
                    kb.op("dve", lambda e: e.tensor_tensor(out=WT[:, :], in0=p_sc[:, 0:128], in1=decT[:, :], op=ALU.mult), reads=[p_sc, decT], writes=[WT])
                    kb.op("dve", lambda e: e.tensor_scalar(out=Xdt[:, :], in0=X_[:, h * 64:(h + 1) * 64], scalar1=dta_[:, dcol + h:dcol + h + 1],
                                                           scalar2=None, op0=ALU.mult), reads=[X_, dta_], writes=[Xdt])
                    kb.op("dve", lambda e: e.tensor_scalar(out=Xw[:, :], in0=Xdt[:, :], scalar1=cum[:, 12 + h:13 + h], scalar2=None, op0=ALU.mult),
                          reads=[Xdt, cum], writes=[Xw])
                    kb.op("dve", lambda e: e.tensor_copy(out=hbf[:, :], in_=hs), reads=[hstate], writes=[hbf])
                    kb.op("pe", lambda e: e.matmul(p_yd[:, 0:64], lhsT=WT[:, :], rhs=Xdt[:, :], start=True, stop=True), reads=[WT, Xdt], writes=[p_yd])
                    kb.op("pe", lambda e: e.matmul(p_yo[:, 0:64], lhsT=CT_[:, :], rhs=hbf[:, :], start=True, stop=True), reads=[CT_, hbf], writes=[p_yo])
                    kb.op("pe", lambda e: e.matmul(p_st[:, 0:64], lhsT=B_[:, :], rhs=Xw[:, :], start=True, stop=True), reads=[B_, Xw], writes=[p_st])
                    yv = Yacc[:, c, h * 64:(h + 1) * 64]
                    if d == 0:
                        kb.op("dve", lambda e: e.scalar_tensor_tensor(out=ytmp[:, :], in0=X_[:, h * 64:(h + 1) * 64], scalar=dsk[:, h:h + 1],
                                                                      in1=p_yd[:, 0:64], op0=ALU.mult, op1=ALU.add), reads=[X_, dsk, p_yd], writes=[ytmp])
                    else:
                        kb.op("dve", lambda e: e.tensor_tensor(out=ytmp[:, :], in0=yv, in1=p_yd[:, 0:64], op=ALU.add), reads=[Yacc, p_yd], writes=[ytmp])
                    kb.op("dve", lambda e: e.scalar_tensor_tensor(out=yv, in0=p_yo[:, 0:64], scalar=cum[:, 8 + h:9 + h], in1=ytmp[:, :],
                                                                  op0=ALU.mult, op1=ALU.add), reads=[p_yo, cum, ytmp], writes=[Yacc])
                    kb.op("dve", lambda e: e.scalar_tensor_tensor(out=hs, in0=hs, scalar=tot[:, 4 + h:5 + h], in1=p_st[:, 0:64],
                                                                  op0=ALU.mult, op1=ALU.add), reads=[hstate, tot, p_st], writes=[hstate])
        kb.dma(Yd[:, :].rearrange("(c p) f -> p c f", p=128), Yacc[:, :, :], reads=[Yacc], writes=[Yd])


def attention(kb, cs, st, Tq, Tk, QT_d, KT_d, V_d, lam, O_d, QB=256):
    NK = Tk // 128
    NQ = Tq // 128
    with ExitStack() as s:
        QT = kb.sb(s, [128, Tq], BF16, "QT")
        KT = kb.sb(s, [128, Tk], BF16, "KT")
        Va = kb.sb(s, [128, NK, 129], BF16, "Va")
        Oall = kb.sb(s, [128, NQ, 128], F32, "Oall")
        if callable(QT_d):
            QT_d(QT)
            KT_d(KT)
            V_d(Va)
        else:
            for (t0, n) in [(i, min(4096, Tq - i)) for i in range(0, Tq, 4096)]:
                kb.dma(QT[:, t0:t0 + n], QT_d[:, t0:t0 + n], reads=[QT_d], writes=[QT.r(t0)])
            for (t0, n) in [(i, min(4096, Tk - i)) for i in range(0, Tk, 4096)]:
                kb.dma(KT[:, t0:t0 + n], KT_d[:, t0:t0 + n], reads=[KT_d], writes=[KT.r(t0)])
            for (k0, n) in [(i, min(32, NK - i)) for i in range(0, NK, 32)]:
                kb.dma(Va[:, k0:k0 + n, 0:128], V_d[k0 * 128:(k0 + n) * 128, :].rearrange("(k p) e -> p k e", p=128), reads=[V_d], writes=[Va.r(k0)])
        kb.op("pool", lambda e: e.memset(Va[:, :, 128:129], 1.0), writes=[Va.r("ones")])
        ps_s = [kb.ps(s, [128, 512], F32, "ps_s") for _ in range(2)]
        acc = [[kb.ps(s, [128, 512], F32, "acc") for _ in range(QB // 128)] for _ in range(2)]
        pT = [[kb.sb(s, [128, QB], BF16, "pT") for _ in range(2)] for _ in range(2)]
        rec = kb.sb(s, [128, 4], F32, "rec")
        o1 = kb.sb(s, [128, 128], F32, "o1")
        it = 0
        for q0 in range(0, Tq, QB):
            nq = min(QB, Tq - q0)
            nqs = nq // 128
            for kt in range(NK):
                for m in range(2):
                    pss = ps_s[it % 2]
                    half = (it // 2) % 2
                    p_ = pT[m][(it // 2) % 2]
                    it += 1
                    sv = pss[:, half * 256:half * 256 + nq]
                    kb.op("pe", lambda e: e.matmul(sv, lhsT=KT[m * 64:(m + 1) * 64, kt * 128:(kt + 1) * 128], rhs=QT[m * 64:(m + 1) * 64, q0:q0 + nq],
                                                   start=True, stop=True), reads=[KT, QT], writes=[pss.r(half)])
                    kb.op("act", lambda e: e.activation(out=p_[:, 0:nq], in_=sv, func=AF.Exp, scale=0.125), reads=[pss.r(half)], writes=[p_])
                    for qs in range(nqs):
                        kb.op("pe", lambda e: e.matmul(acc[m][qs][:, 0:129], lhsT=p_[:, qs * 128:(qs + 1) * 128], rhs=Va[:, kt, :],
                                                       start=(kt == 0), stop=(kt == NK - 1)), reads=[p_, Va], writes=[acc[m][qs]])
            for qs in range(nqs):
                qt = q0 // 128 + qs
                kb.op("dve", lambda e: e.reciprocal(out=rec[:, 0:1], in_=acc[0][qs][:, 128:129]), reads=[acc[0][qs]], writes=[rec])
                kb.op("dve", lambda e: e.reciprocal(out=rec[:, 1:2], in_=acc[1][qs][:, 128:129]), reads=[acc[1][qs]], writes=[rec])
                kb.op("dve", lambda e: e.tensor_tensor(out=rec[:, 2:3], in0=rec[:, 1:2], in1=lam[:, 0:1], op=ALU.mult), reads=[rec, lam], writes=[rec])
                kb.op("dve", lambda e: e.tensor_scalar(out=o1[:, :], in0=acc[1][qs][:, 0:128], scalar1=rec[:, 2:3], scalar2=None, op0=ALU.mult),
                      reads=[acc[1][qs], rec], writes=[o1])
                kb.op("dve", lambda e: e.scalar_tensor_tensor(out=Oall[:, qt, :], in0=acc[0][qs][:, 0:128], scalar=rec[:, 0:1], in1=o1[:, :],
                                                              op0=ALU.mult, op1=ALU.subtract), reads=[acc[0][qs], rec, o1], writes=[Oall])
        kb.dma(O_d[:, :].rearrange("(k p) e -> p k e", p=128), Oall[:, :, :], reads=[Oall], writes=[O_d])


def compute_lam(kb, st, lamp_d, lam_init):
    lam = kb.sb(st, [128, 4], F32, "lam")
    with ExitStack() as s:
        lp = kb.sb(s, [128, 256], F32, "lp")
        junk = kb.sb(s, [128, 64], F32, "lj")
        kb.dma(lp[:, :], lamp_d[:, :], reads=[lamp_d], writes=[lp])
        kb.op("dve", lambda e: e.memset(lam[:, :], 0.0), writes=[lam])
        for i in range(2):
            kb.op("dve", lambda e: e.tensor_tensor(out=junk[:, :], in0=lp[:, i * 128:i * 128 + 64], in1=lp[:, i * 128 + 64:i * 128 + 128], op=ALU.mult),
                  reads=[lp], writes=[junk])
            kb.op("dve", lambda e: e.reduce_sum(out=lam[:, 1 + i:2 + i], in_=junk[:, :], axis=AX.X), reads=[junk], writes=[lam])
        kb.op("act", lambda e: e.activation(out=lam[:, 1:3], in_=lam[:, 1:3], func=AF.Exp), reads=[lam], writes=[lam])
        kb.op("dve", lambda e: e.tensor_tensor(out=lam[:, 3:4], in0=lam[:, 1:2], in1=lam[:, 2:3], op=ALU.subtract), reads=[lam], writes=[lam])
        kb.op("dve", lambda e: e.tensor_scalar(out=lam[:, 0:1], in0=lam[:, 3:4], scalar1=float(lam_init), scalar2=None, op0=ALU.add),
              reads=[lam], writes=[lam])
    return lam


def bcast_rows(kb, cs, ps, dst, col_of, n_t):
    with ExitStack() as s:
        dg = kb.sb(s, [128, 128], F32, "dg")
        for t in range(n_t):
            ap, buf = col_of(t)
            kb.op("dve", lambda e: e.tensor_scalar(out=dg[:, :], in0=cs.F("ident"), scalar1=ap, scalar2=None, op0=ALU.mult),
                  reads=[cs.f, buf], writes=[dg])
            kb.op("pe", lambda e: e.matmul(ps[:, 0:128], lhsT=cs.F("ones"), rhs=dg[:, :], start=True, stop=True), reads=[cs.f, dg], writes=[ps])
            kb.op("act", lambda e: e.copy(out=dst[:, t * 128:(t + 1) * 128], in_=ps[:, 0:128]), reads=[ps], writes=[dst])


def load_w_full(kb, wl, dst, w_d):
    for half in range(2):
        wb, _ = wl.load(w_d, 0, half * 512, 512)
        kb.op("pool", lambda e: e.tensor_copy(out=dst[:, :, half * 512:(half + 1) * 512], in_=wb[:, :, :]), reads=[wb], writes=[dst])


def phase_d(kb, cs, A, v, T, I, W, O, lam_init):
    mods, gw2 = A["mods"], A["gw2"]
    with ExitStack() as st:
        Wa = kb.sb(st, [128, 8, 1024], BF16, "Wa")
        Wb = kb.sb(st, [128, 8, 1024], BF16, "Wb")
        Wo = kb.sb(st, [128, 8, 1024], BF16, "Wo")
        with ExitStack() as sw:
            wl = WLoader(kb, sw)
            load_w_full(kb, wl, Wa, W["ssd_out"])
            load_w_full(kb, wl, Wb, W["diff_out"])
            load_w_full(kb, wl, Wo, W["w_o"])
        Wr = kb.sb(st, [128, 8, 16], F32, "Wr")
        kb.dma(Wr[:, :, :], W["router"][:, :].rearrange("(t p) c -> p t c", p=128), reads=[W["router"]], writes=[Wr])
        nwb = kb.sb(st, [128, 1024], F32, "nwb")
        swb = kb.sb(st, [128, 1024], F32, "swb")
        g1b = kb.sb(st, [128, 1024], F32, "g1b")
        kb.dma(nwb[:, :], W["nw"][0:1, :].to_broadcast([128, 1024]), reads=[W["nw"]], writes=[nwb])
        kb.dma(swb[:, :], W["sw"][0:1, :].to_broadcast([128, 1024]), reads=[W["sw"]], writes=[swb])
        kb.op("dve", lambda e: e.tensor_scalar(out=swb[:, :], in0=swb[:, :], scalar1=float(1.0 - lam_init), scalar2=None, op0=ALU.mult),
              reads=[swb], writes=[swb])
        psA = [kb.ps(st, [128, 512], F32, "dpsA") for _ in range(2)]
        psB = [kb.ps(st, [128, 512], F32, "dpsB") for _ in range(2)]
        ptb = [kb.ps(st, [128, 8, 128], BF16, "dptb") for _ in range(2)]
        bcast_rows(kb, cs, psA[0], g1b, lambda t: (mods[:, 16 + t, v:v + 1], mods), 8)
        ys = kb.sb(st, [128, 1024], F32, "ys")
        zs = kb.sb(st, [128, 1024], BF16, "zs")
        oa = kb.sb(st, [128, 1024], F32, "oa")
        yz = kb.sb(st, [128, 1024], F32, "yz")
        junk = kb.sb(st, [128, 1024], F32, "djunk")
        ynb = kb.sb(st, [128, 1024], BF16, "ynb")
        onb = kb.sb(st, [128, 1024], BF16, "onb")
        sm = kb.sb(st, [128, 32], F32, "sm")
        YnT = kb.sb(st, [128, 8, 512], BF16, "YnT")
        OnT = kb.sb(st, [128, 8, 512], BF16, "OnT")
        mT = kb.sb(st, [128, 8, 512], BF16, "mT")
        gt = [kb.sb(st, [128, 3, 512], BF16, "gt") for _ in range(2)]
        yc = [kb.sb(st, [128, 512], F32, "yc") for _ in range(2)]
        m1 = kb.sb(st, [128, 512], F32, "m1")
        m2 = kb.sb(st, [128, 512], F32, "m2")
        xt = kb.sb(st, [128, 1024], F32, "xt")
        xm = kb.sb(st, [128, 1024], F32, "xm")
        xn2 = kb.sb(st, [128, 1024], F32, "xn2")
        h2f = kb.sb(st, [128, 8, 128], F32, "h2f")
        h2b = kb.sb(st, [128, 8, 512], BF16, "h2b")
        af = kb.sb(st, [128, 16], F32, "af")
        afT = kb.sb(st, [16, 512], F32, "afT")
        k = 0
        for (t0, n) in blocks(0, T):
            nt = n // 128
            for j in range(nt):
                r0 = t0 + j * 128
                if "ldY" in I:
                    I["ldY"](ys, r0)
                    I["ldO"](oa, r0)
                else:
                    kb.dma(ys[:, :], I["YS"][r0:r0 + 128, :], reads=[I["YS"]], writes=[ys])
                    kb.dma(oa[:, :], I["Oa"][r0:r0 + 128, :], reads=[I["Oa"]], writes=[oa])
                kb.dma(zs[:, :], I["ZS"][r0:r0 + 128, :], reads=[I["ZS"]], writes=[zs])
                kb.op("dve", lambda e: e.tensor_tensor(out=yz[:, :], in0=ys[:, :], in1=zs[:, :], op=ALU.mult), reads=[ys, zs], writes=[yz])
                kb.op("dve", lambda e: e.memset(sm[:, :], 0.0), writes=[sm])
                for g in range(2):
                    kb.op("act", lambda e: e.activation(out=junk[:, g * 512:(g + 1) * 512], in_=yz[:, g * 512:(g + 1) * 512], func=AF.Square,
                                                        accum_out=sm[:, g:g + 1]), reads=[yz], writes=[junk, sm])
                kb.op("act", lambda e: e.activation(out=sm[:, 2:4], in_=sm[:, 0:2], func=AF.Sqrt, scale=1.0 / 512, bias=cs.eps[:, 0:1]),
                      reads=[sm, cs.eps], writes=[sm])
                kb.op("dve", lambda e: e.reciprocal(out=sm[:, 4:6], in_=sm[:, 2:4]), reads=[sm], writes=[sm])
                for g in range(2):
                    kb.op("dve", lambda e: e.scalar_tensor_tensor(out=ynb[:, g * 512:(g + 1) * 512], in0=yz[:, g * 512:(g + 1) * 512],
                                                                  scalar=sm[:, 4 + g:5 + g], in1=nwb[:, g * 512:(g + 1) * 512],
                                                                  op0=ALU.mult, op1=ALU.mult), reads=[yz, sm, nwb], writes=[ynb])
                kb.op("dve", lambda e: e.tensor_tensor(out=junk[:, :], in0=oa[:, :], in1=oa[:, :], op=ALU.mult), reads=[oa], writes=[junk])
                kb.op("dve", lambda e: e.reduce_sum(out=sm[:, 8:16], in_=junk[:, :].rearrange("p (h e) -> p h e", e=128), axis=AX.X),
                      reads=[junk], writes=[sm])
                kb.op("act", lambda e: e.activation(out=sm[:, 16:24], in_=sm[:, 8:16], func=AF.Sqrt, scale=1.0 / 128, bias=cs.eps[:, 0:1]),
                      reads=[sm, cs.eps], writes=[sm])
                kb.op("dve", lambda e: e.reciprocal(out=sm[:, 24:32], in_=sm[:, 16:24]), reads=[sm], writes=[sm])
                for h in range(8):
                    kb.op("dve", lambda e: e.scalar_tensor_tensor(out=onb[:, h * 128:(h + 1) * 128], in0=oa[:, h * 128:(h + 1) * 128],
                                                                  scalar=sm[:, 24 + h:25 + h], in1=swb[:, h * 128:(h + 1) * 128],
                                                                  op0=ALU.mult, op1=ALU.mult), reads=[oa, sm, swb], writes=[onb])
                for (src, dstT) in ((ynb, YnT), (onb, OnT)):
                    p_ = ptb[k % 2]
                    k += 1
                    for ct in range(8):
                        kb.op("pe", lambda e: e.transpose(out=p_[:, ct, :], in_=src[:, ct * 128:(ct + 1) * 128], identity=cs.B("ident")),
                              reads=[src, cs.b], writes=[p_], sig=(ct == 7))
                    kb.op("act", lambda e: e.copy(out=dstT[:, :, j * 128:(j + 1) * 128], in_=p_[:, :, :]), reads=[p_], writes=[dstT])
            for dm in range(8):
                g_, yc_ = gt[dm % 2], yc[dm % 2]
                for b3 in range(3):
                    kb.dma(g_[:, b3, 0:n], I["G"][(b3 * 8 + dm) * 128:(b3 * 8 + dm + 1) * 128, t0:t0 + n], reads=[I["G"]], writes=[g_])
                kb.dma(yc_[:, 0:n], I["YcT"][dm * 128:(dm + 1) * 128, t0:t0 + n], reads=[I["YcT"]], writes=[yc_])
                pa_, pb_ = psA[dm % 2], psB[dm % 2]
                for ct in range(8):
                    kb.op("pe", lambda e: e.matmul(pa_[:, 0:n], lhsT=Wa[:, ct, dm * 128:(dm + 1) * 128], rhs=YnT[:, ct, 0:n], start=(ct == 0), stop=(ct == 7)),
                          reads=[Wa, YnT], writes=[pa_], sig=(ct == 7))
                for ct in range(8):
                    kb.op("pe", lambda e: e.matmul(pb_[:, 0:n], lhsT=Wb[:, ct, dm * 128:(dm + 1) * 128], rhs=OnT[:, ct, 0:n], start=(ct == 0), stop=(ct == 7)),
                          reads=[Wb, OnT], writes=[pb_], sig=(ct == 7))
                kb.op("dve", lambda e: e.tensor_tensor(out=m1[:, 0:n], in0=pa_[:, 0:n], in1=g_[:, 0, 0:n], op=ALU.mult), reads=[pa_, g_], writes=[m1])
                kb.op("dve", lambda e: e.tensor_tensor(out=m2[:, 0:n], in0=pb_[:, 0:n], in1=g_[:, 1, 0:n], op=ALU.mult), reads=[pb_, g_], writes=[m2])
                kb.op("pool", lambda e: e.tensor_tensor(out=m1[:, 0:n], in0=m1[:, 0:n], in1=m2[:, 0:n], op=ALU.add), reads=[m1, m2], writes=[m1])
                kb.op("pool", lambda e: e.tensor_tensor(out=m2[:, 0:n], in0=yc_[:, 0:n], in1=g_[:, 2, 0:n], op=ALU.mult), reads=[yc_, g_], writes=[m2])
                kb.op("dve", lambda e: e.tensor_tensor(out=mT[:, dm, 0:n], in0=m1[:, 0:n], in1=m2[:, 0:n], op=ALU.add), reads=[m1, m2], writes=[mT])
            for j in range(nt):
                r0 = t0 + j * 128
                kb.dma(xt[:, :], I["xh"][HALO + r0:HALO + r0 + 128, :], reads=[I["xh"]], writes=[xt])
                for half in range(2):
                    ps = psA[half]
                    for ct in range(8):
                        kb.op("pe", lambda e: e.matmul(ps[:, :], lhsT=mT[:, ct, j * 128:(j + 1) * 128], rhs=Wo[:, ct, half * 512:(half + 1) * 512],
                                                       start=(ct == 0), stop=(ct == 7)), reads=[mT, Wo], writes=[ps], sig=(ct == 7))
                    hsl = slice(half * 512, (half + 1) * 512)
                    kb.op("dve", lambda e: e.tensor_tensor(out=xm[:, hsl], in0=ps[:, :], in1=g1b[:, hsl], op=ALU.mult), reads=[ps, g1b], writes=[xm])
                    kb.op("dve", lambda e: e.tensor_tensor(out=xm[:, hsl], in0=xm[:, hsl], in1=xt[:, hsl], op=ALU.add), reads=[xm, xt], writes=[xm])
                kb.dma(O["xmid"][r0:r0 + 128, :], xm[:, :], reads=[xm], writes=[O["xmid"].r(r0)])
                kb.op("dve", lambda e: e.memset(sm[:, 0:1], 0.0), writes=[sm])
                kb.op("act", lambda e: e.activation(out=junk[:, :], in_=xm[:, :], func=AF.Square, accum_out=sm[:, 0:1]), reads=[xm], writes=[junk, sm])
                kb.op("act", lambda e: e.activation(out=sm[:, 1:2], in_=sm[:, 0:1], func=AF.Sqrt, scale=1.0 / 1024, bias=cs.eps[:, 0:1]),
                      reads=[sm, cs.eps], writes=[sm])
                kb.op("dve", lambda e: e.reciprocal(out=sm[:, 2:3], in_=sm[:, 1:2]), reads=[sm], writes=[sm])
                kb.op("dve", lambda e: e.tensor_scalar(out=xn2[:, :], in0=xm[:, :], scalar1=sm[:, 2:3], scalar2=None, op0=ALU.mult),
                      reads=[xm, sm], writes=[xn2])
                for hh in range(2):
                    ps = psB[hh]
                    for c4 in range(4):
                        dt = hh * 4 + c4
                        kb.op("pe", lambda e: e.transpose(out=ps[:, c4 * 128:(c4 + 1) * 128], in_=xn2[:, dt * 128:(dt + 1) * 128], identity=cs.F("ident")),
                              reads=[xn2, cs.f], writes=[ps])
                    for c4 in range(4):
                        dt = hh * 4 + c4
                        kb.op("act", lambda e: e.activation(out=h2f[:, dt, :], in_=ps[:, c4 * 128:(c4 + 1) * 128], func=AF.Identity,
                                                            scale=gw2[:, dt, v:v + 1], bias=mods[:, 24 + dt, v:v + 1]), reads=[ps, gw2, mods], writes=[h2f])
                kb.op("pool", lambda e: e.tensor_copy(out=h2b[:, :, j * 128:(j + 1) * 128], in_=h2f[:, :, :]), reads=[h2f], writes=[h2b])
                pr = psA[0]
                for dt in range(8):
                    kb.op("pe", lambda e: e.matmul(pr[:, 0:16], lhsT=h2f[:, dt, :], rhs=Wr[:, dt, :], start=(dt == 0), stop=(dt == 7)),
                          reads=[h2f, Wr], writes=[pr], sig=(dt == 7))
                kb.op("dve", lambda e: e.reduce_max(out=sm[:, 3:4], in_=pr[:, 0:16], axis=AX.X), reads=[pr], writes=[sm])
                kb.op("dve", lambda e: e.tensor_scalar(out=sm[:, 4:5], in0=sm[:, 3:4], scalar1=-1.0, scalar2=None, op0=ALU.mult), reads=[sm], writes=[sm])
                kb.op("dve", lambda e: e.memset(sm[:, 5:6], 0.0), writes=[sm])
                kb.op("act", lambda e: e.activation(out=af[:, :], in_=pr[:, 0:16], func=AF.Exp, bias=sm[:, 4:5], accum_out=sm[:, 5:6]),
                      reads=[pr, sm], writes=[af, sm])
                kb.op("dve", lambda e: e.reciprocal(out=sm[:, 6:7], in_=sm[:, 5:6]), reads=[sm], writes=[sm])
                kb.op("dve", lambda e: e.tensor_scalar(out=af[:, :], in0=af[:, :], scalar1=sm[:, 6:7], scalar2=None, op0=ALU.mult), reads=[af, sm], writes=[af])
                kb.dma(O["aff"][r0:r0 + 128, :], af[:, :], reads=[af], writes=[O["aff"].r(r0)])
                pt2 = psA[1]
                kb.op("pe", lambda e: e.transpose(out=pt2[0:16, 0:128], in_=af[:, :], identity=cs.F("ident")), reads=[af, cs.f], writes=[pt2])
                kb.op("act", lambda e: e.copy(out=afT[:, j * 128:(j + 1) * 128], in_=pt2[0:16, 0:128]), reads=[pt2], writes=[afT])
            for dt in range(8):
                kb.dma(O["h2T"][dt * 128:(dt + 1) * 128, t0:t0 + n], h2b[:, dt, 0:n], reads=[h2b], writes=[O["h2T"].r((dt, t0))])
            kb.dma(O["affT"][:, t0:t0 + n], afT[:, 0:n], reads=[afT], writes=[O["affT"].r(t0)])


def phase_e(kb, cs, A, v, T, N, cap, I, W, O, final_w=None):
    mods = A["mods"]
    NT = T // 128
    M = N // 8
    with ExitStack() as st:
        psA = [kb.ps(st, [128, 512], F32, "epsA") for _ in range(2)]
        psB = [kb.ps(st, [128, 512], F32, "epsB") for _ in range(2)]
        psC = [kb.ps(st, [128, 512], F32, "epsC") for _ in range(2)]
        g2b = kb.sb(st, [128, 1024], F32, "g2b")
        bcast_rows(kb, cs, psA[0], g2b, lambda t: (mods[:, 40 + t, v:v + 1], mods), 8)
        gs = kb.sb(st, [128, NT, 16], F32, "gs")
        with ExitStack() as s:
            at = kb.sb(s, [128, M], F32, "at")
            jk = kb.sb(s, [128, M], F32, "jk")
            if "ld_at" in I:
                I["ld_at"](at)
            else:
                kb.dma(at[:, :], I["affT_all"][:, :].rearrange("e (r n) -> (e r) n", r=8), reads=[I["affT_all"]], writes=[at])
            b = kb.sb(s, [128, 8], F32, "bis")
            kb.op("dve", lambda e: e.memset(b[:, 0:1], 0.0), writes=[b])
            kb.op("dve", lambda e: e.memset(b[:, 1:2], 1.0), writes=[b])
            for it in range(34):
                kb.op("dve", lambda e: e.tensor_tensor(out=b[:, 5:6], in0=b[:, 0:1], in1=b[:, 1:2], op=ALU.add), reads=[b], writes=[b])
                kb.op("dve", lambda e: e.tensor_scalar(out=b[:, 2:3], in0=b[:, 5:6], scalar1=0.5, scalar2=None, op0=ALU.mult), reads=[b], writes=[b])
                kb.op("dve", lambda e: e.tensor_scalar(out=jk[:, :], in0=at[:, :], scalar1=b[:, 2:3], scalar2=None, op0=ALU.is_gt), reads=[at, b], writes=[jk])
                kb.op("dve", lambda e: e.reduce_sum(out=b[:, 3:4], in_=jk[:, :], axis=AX.X), reads=[jk], writes=[b])
                kb.op("pe", lambda e: e.matmul(psA[1][:, 0:1], lhsT=cs.F("blk8"), rhs=b[:, 3:4], start=True, stop=True), reads=[cs.f, b], writes=[psA[1]])
                kb.op("dve", lambda e: e.tensor_scalar(out=b[:, 4:5], in0=psA[1][:, 0:1], scalar1=float(cap) - 0.5, scalar2=None, op0=ALU.is_ge),
                      reads=[psA[1]], writes=[b])
                kb.op("dve", lambda e: e.tensor_tensor(out=b[:, 5:6], in0=b[:, 2:3], in1=b[:, 0:1], op=ALU.subtract), reads=[b], writes=[b])
                kb.op("dve", lambda e: e.scalar_tensor_tensor(out=b[:, 0:1], in0=b[:, 5:6], scalar=b[:, 4:5], in1=b[:, 0:1], op0=ALU.mult, op1=ALU.add),
                      reads=[b], writes=[b])
                kb.op("dve", lambda e: e.tensor_tensor(out=b[:, 5:6], in0=b[:, 1:2], in1=b[:, 2:3], op=ALU.subtract), reads=[b], writes=[b])
                kb.op("dve", lambda e: e.scalar_tensor_tensor(out=b[:, 1:2], in0=b[:, 5:6], scalar=b[:, 4:5], in1=b[:, 2:3], op0=ALU.mult, op1=ALU.add),
                      reads=[b], writes=[b])
            sl = kb.sb(s, [128, 16], F32, "sl")
            thr = kb.sb(s, [128, 16], F32, "thr")
            kb.op("dve", lambda e: e.tensor_scalar(out=sl[:, :], in0=cs.F("sel16", 128, 16), scalar1=b[:, 0:1], scalar2=None, op0=ALU.mult),
                  reads=[cs.f, b], writes=[sl])
            kb.op("pe", lambda e: e.matmul(psA[1][:, 0:16], lhsT=cs.F("ones"), rhs=sl[:, :], start=True, stop=True), reads=[cs.f, sl], writes=[psA[1]])
            kb.op("dve", lambda e: e.tensor_copy(out=thr[:, :], in_=psA[1][:, 0:16]), reads=[psA[1]], writes=[thr])
            afl = kb.sb(s, [128, NT, 16], F32, "afl")
            kb.dma(afl[:, :, :], I["aff"][:, :].rearrange("(t p) e -> p t e", p=128), reads=[I["aff"]], writes=[afl])
            for t in range(NT):
                kb.op("dve", lambda e: e.tensor_tensor(out=gs[:, t, :], in0=afl[:, t, :], in1=thr[:, :], op=ALU.is_gt), reads=[afl, thr], writes=[gs])
                kb.op("dve", lambda e: e.tensor_tensor(out=gs[:, t, :], in0=gs[:, t, :], in1=afl[:, t, :], op=ALU.mult), reads=[gs, afl], writes=[gs])
        TH = min(T, 1024)
        NTH = TH // 128
        h2T = kb.sb(st, [128, 8, TH], BF16, "eh2T")
        acc = kb.sb(st, [128, NTH, 1024], F32, "eacc")
        wl13 = WLoader(kb, st, kt=8, n=512, nbuf=3, nstg=2)
        wl2 = WLoader(kb, st, kt=4, n=1024, nbuf=2, nstg=1)
        s1 = kb.sb(st, [128, 512], F32, "es1")
        heT = [kb.sb(st, [128, 4, 512], BF16, "heT") for _ in range(2)]
        xm = kb.sb(st, [128, 1024], F32, "exm")
        xo = kb.sb(st, [128, 1024], F32, "exo")
        sm = kb.sb(st, [128, 4], F32, "esm")
        if final_w is not None:
            fwb = kb.sb(st, [128, 1024], F32, "fwb")
            kb.dma(fwb[:, :], final_w[0:1, :].to_broadcast([128, 1024]), reads=[final_w], writes=[fwb])
        k = 0
        for hh in range(T // TH):
            tb0 = hh * TH
            for dt in range(8):
                kb.dma(h2T[:, dt, :], I["h2T"][dt * 128:(dt + 1) * 128, tb0:tb0 + TH], reads=[I["h2T"]], writes=[h2T.r(dt)])
            kb.op("pool", lambda e: e.memset(acc[:, :, :], 0.0), writes=[acc])
            for ex in range(16):
                for fc in range(4):
                    w1, _ = wl13.load(W["w1"], ex * 1024, fc * 512, 512)
                    w3, _ = wl13.load(W["w3"], ex * 1024, fc * 512, 512)
                    w2, _ = wl2.load(W["w2"], ex * 2048 + fc * 512, 0, 1024)
                    for (t0, n) in blocks(0, TH):
                        he = heT[k % 2]
                        k += 1
                        for fi in range(4):
                            p1, p3 = psA[fi % 2], psB[fi % 2]
                            for dt in range(8):
                                kb.op("pe", lambda e: e.matmul(p1[:, 0:n], lhsT=w1[:, dt, fi * 128:(fi + 1) * 128], rhs=h2T[:, dt, t0:t0 + n],
                                                               start=(dt == 0), stop=(dt == 7)), reads=[w1, h2T], writes=[p1], sig=(dt == 7))
                            for dt in range(8):
                                kb.op("pe", lambda e: e.matmul(p3[:, 0:n], lhsT=w3[:, dt, fi * 128:(fi + 1) * 128], rhs=h2T[:, dt, t0:t0 + n],
                                                               start=(dt == 0), stop=(dt == 7)), reads=[w3, h2T], writes=[p3], sig=(dt == 7))
                            kb.op("act", lambda e: e.activation(out=s1[:, 0:n], in_=p1[:, 0:n], func=AF.Silu), reads=[p1], writes=[s1])
                            kb.op("dve", lambda e: e.tensor_tensor(out=he[:, fi, 0:n], in0=s1[:, 0:n], in1=p3[:, 0:n], op=ALU.mult), reads=[s1, p3], writes=[he])
                        for j in range(n // 128):
                            tl = t0 // 128 + j
                            tt = tb0 // 128 + tl
                            for half in range(2):
                                pc = psC[half]
                                for fi in range(4):
                                    kb.op("pe", lambda e: e.matmul(pc[:, :], lhsT=he[:, fi, j * 128:(j + 1) * 128], rhs=w2[:, fi, half * 512:(half + 1) * 512],
                                                                   start=(fi == 0), stop=(fi == 3)), reads=[he, w2], writes=[pc], sig=(fi == 3))
                                av = acc[:, tl, half * 512:(half + 1) * 512]
                                kb.op("dve", lambda e: e.scalar_tensor_tensor(out=av, in0=pc[:, :], scalar=gs[:, tt, ex:ex + 1], in1=av,
                                                                              op0=ALU.mult, op1=ALU.add), reads=[pc, gs, acc.r(tl)], writes=[acc.r(tl)])
            for tl in range(NTH):
                t = tb0 // 128 + tl
                kb.dma(xm[:, :], I["xmid"][t * 128:(t + 1) * 128, :], reads=[I["xmid"]], writes=[xm])
                kb.op("dve", lambda e: e.tensor_tensor(out=xo[:, :], in0=acc[:, tl, :], in1=g2b[:, :], op=ALU.mult), reads=[acc, g2b], writes=[xo])
                kb.op("dve", lambda e: e.tensor_tensor(out=xo[:, :], in0=xo[:, :], in1=xm[:, :], op=ALU.add), reads=[xo, xm], writes=[xo])
                if final_w is not None:
                    kb.op("dve", lambda e: e.memset(sm[:, 0:1], 0.0), writes=[sm])
                    kb.op("act", lambda e: e.activation(out=xm[:, :], in_=xo[:, :], func=AF.Square, accum_out=sm[:, 0:1]), reads=[xo], writes=[xm, sm])
                    kb.op("act", lambda e: e.activation(out=sm[:, 1:2], in_=sm[:, 0:1], func=AF.Sqrt, scale=1.0 / 1024, bias=cs.eps[:, 0:1]),
                          reads=[sm, cs.eps], writes=[sm])
                    kb.op("dve", lambda e: e.reciprocal(out=sm[:, 2:3], in_=sm[:, 1:2]), reads=[sm], writes=[sm])
                    kb.op("dve", lambda e: e.scalar_tensor_tensor(out=xo[:, :], in0=xo[:, :], scalar=sm[:, 2:3], in1=fwb[:, :], op0=ALU.mult, op1=ALU.mult),
                          reads=[xo, sm, fwb], writes=[xo])
                kb.dma(O["xout"][t * 128:(t + 1) * 128, :], xo[:, :], reads=[xo], writes=[O["xout"].r(t)])


import ml_dtypes
from concourse.bass_utils import run_bass_kernel_spmd

NPDT = {F32: np.float32, BF16: ml_dtypes.bfloat16, I32: np.int32}
NCORE = 8
TL = 2048
TC = 256
SEQ = 16384
DEPTH = 2


class Launch:
    def __init__(self):
        self.nc = bass.Bass("TRN2", target_bir_lowering=False)
        self.kb = KB(self.nc)
        self.ins = {}
        self.outs = {}

    def inp(self, name, shape, dtype=F32):
        b = self.kb.dram(name, shape, dtype, kind="ExternalInput")
        self.ins[name] = (shape, dtype)
        return b

    def out(self, name, shape, dtype=F32):
        b = self.kb.dram(name, shape, dtype, kind="ExternalOutput")
        self.outs[name] = (shape, dtype)
        return b

    def run(self, in_maps):
        self.kb.finish()
        maps = []
        for m in in_maps:
            mm = {}
            for k, (shape, dtype) in self.ins.items():
                a = np.ascontiguousarray(np.asarray(m[k]).astype(NPDT[dtype], copy=False))
                assert tuple(a.shape) == tuple(shape), (k, a.shape, shape)
                mm[k] = a
            maps.append(mm)
        res = run_bass_kernel_spmd(self.nc, maps, core_ids=list(range(len(maps))))
        return res.results


def rope_tables_host(pos0, n):
    t = np.arange(pos0, pos0 + n)
    row = (t // 64).astype(np.float32)
    col = (t % 64).astype(np.float32)
    freqs = (np.float32(10000.0) ** (-np.arange(0, 32, 2, dtype=np.float32) / np.float32(32))).astype(np.float32)
    ang = np.concatenate([row[:, None] * freqs, col[:, None] * freqs], axis=-1).astype(np.float32)
    cos, sin = np.cos(ang.astype(np.float64)), np.sin(ang.astype(np.float64))
    p = np.arange(128)
    axis = (p % 64) // 32
    half = (p % 32) // 16
    f = p % 16
    idx = axis * 16 + f
    COS = cos[:, idx].T
    SIN = sin[:, idx].T * np.where(half == 0, -1.0, 1.0)[:, None]
    return COS.astype(np.float32), SIN.astype(np.float32)


A_OUT = [("X", lambda T: [T, 1024], BF16), ("B", lambda T: [T, 256], BF16), ("BT", lambda T: [256, T], BF16), ("CT", lambda T: [256, T], BF16),
         ("DTA", lambda T: [T, 64], F32), ("KT", lambda T: [1024, T], BF16), ("QT", lambda T: [1024, T], BF16), ("V", lambda T: [T, 1024], BF16),
         ("ZS", lambda T: [T, 1024], BF16), ("G", lambda T: [3072, T], BF16), ("YcT", lambda T: [1024, T], F32)]
GATHERED = ["X", "B", "BT", "CT", "DTA", "KT", "QT", "V"]


def build_program():
    L = Launch()
    kb = L.kb
    nc = L.nc
    ds = bass.ds
    cc_d = L.inp("cc", [2, 1024])
    cst_d = L.inp("cst", list(CONST_ARR.shape))
    xl_d = L.inp("xl", [TL + 32, 1024]); hml_d = L.inp("hml", [128, 2])
    xc_d = L.inp("xc", [TC + 32, 1024]); hmc_d = L.inp("hmc", [128, 2])
    cos_d = L.inp("cos", [128, TL]); sin_d = L.inp("sin", [128, TL])
    fw_d = L.inp("fw", [1, 1024])
    LW = []
    for l in range(DEPTH):
        p = "L%d_" % l
        LW.append({"adaw": L.inp(p + "adaw", [1024, 6144]), "adab": L.inp(p + "adab", [6144]), "n1": L.inp(p + "n1", [1024]), "n2": L.inp(p + "n2", [1024]),
                   "w_in": L.inp(p + "w_in", [1024, NCOLS]), "convp": L.inp(p + "convp", [4, 1536]), "dtp": L.inp(p + "dtp", [32, 2]),
                   "confp": L.inp(p + "confp", [34, 1024]), "conf_out": L.inp(p + "conf_out", [1024, 1024]),
                   "dsk": L.inp(p + "dsk", [128, 2]), "lamp": L.inp(p + "lamp", [128, 256]),
                   "ssd_out": L.inp(p + "ssd_out", [1024, 1024]), "diff_out": L.inp(p + "diff_out", [1024, 1024]), "w_o": L.inp(p + "w_o", [1024, 1024]),
                   "nw": L.inp(p + "nw", [1, 1024]), "sw": L.inp(p + "sw", [1, 1024]), "router": L.inp(p + "router", [1024, 16]),
                   "w1": L.inp(p + "w1", [16 * 1024, 2048]), "w3": L.inp(p + "w3", [16 * 1024, 2048]), "w2": L.inp(p + "w2", [16 * 2048, 1024])})
    out_d = L.out("out", [TL, 1024])
    TT = TL + TC
    Ol = {nm: kb.dram("l_" + nm, shp(TL), dt) for nm, shp, dt in A_OUT}
    Oc = {nm: kb.dram("c_" + nm, shp(TC), dt) for nm, shp, dt in A_OUT}
    HB = {"X": ([8 * TT, 128], BF16, 8), "V": ([8 * TT, 128], BF16, 8), "B": ([2 * TT, 128], BF16, 2), "DTA": ([8 * TT, 8], F32, 8),
          "BT": ([2 * 128, TT], BF16, 2), "CT": ([2 * 128, TT], BF16, 2), "KT": ([8 * 128, TT], BF16, 8), "QT": ([8 * 128, TT], BF16, 8)}
    hb, Gt, my = {}, {}, {}
    for nm, (shp, dt, nb) in HB.items():
        hb[nm] = kb.dram("hb_" + nm, shp, dt)
        Gt[nm] = kb.dram("g_" + nm, [8 * shp[0], shp[1]], dt)
        my[nm] = kb.dram("my_" + nm, [8 * shp[0] // nb, shp[1]], dt)
    Yl_d = kb.dram("l_Y", [SEQ, 128], F32); Ol_d = kb.dram("l_O", [SEQ, 128], F32)
    Yc_d = kb.dram("c_Y", [TC, 128], F32); Oc_d = kb.dram("c_O", [TC, 128], F32)
    GY = kb.dram("g_Y", [8 * SEQ, 128], F32); GO = kb.dram("g_O", [8 * SEQ, 128], F32)
    myY = kb.dram("my_Y", [8 * TL, 128], F32); myO = kb.dram("my_O", [8 * TL, 128], F32)
    GYc = kb.dram("g_Yc", [8 * TC, 128], F32); GOc = kb.dram("g_Oc", [8 * TC, 128], F32)
    Dl = {"xmid": kb.dram("l_xmid", [TL, 1024], F32), "h2T": kb.dram("l_h2T", [1024, TL], BF16), "aff": kb.dram("l_aff", [TL, 16], F32),
          "affT": kb.dram("l_affT", [16, TL], F32)}
    Dc = {"xmid": kb.dram("c_xmid", [TC, 1024], F32), "h2T": kb.dram("c_h2T", [1024, TC], BF16), "aff": kb.dram("c_aff", [TC, 16], F32),
          "affT": kb.dram("c_affT", [16, TC], F32)}
    GaffT = kb.dram("g_affT", [8 * 16, TL], F32)
    xh2 = kb.dram("xh2", [TL + 32, 1024], F32)
    xc2 = kb.dram("xc2", [TC + 32, 1024], F32)
    Eloc = kb.dram("e_loc", [32, 1024], F32)
    Eall = kb.dram("e_all", [8 * 32, 1024], F32)
    pid = nc.sync.partition_id()
    gid = pid // 4
    eL = ((pid + 7) % 8) * 32 + 16
    eR = ((pid + 1) % 8) * 32

    def fill_hb(S, t0, T):
        for j in range(8):
            kb.dma(hb["X"][j * TT + t0:j * TT + t0 + T, :], S["X"][:, j * 128:(j + 1) * 128], reads=[S["X"]], writes=[hb["X"].r((j, t0))])
            kb.dma(hb["V"][j * TT + t0:j * TT + t0 + T, :], S["V"][:, j * 128:(j + 1) * 128], reads=[S["V"]], writes=[hb["V"].r((j, t0))])
            kb.dma(hb["DTA"][j * TT + t0:j * TT + t0 + T, :].rearrange("t (k two) -> t k two", two=2),
                   S["DTA"].t.rearrange("t (k j two) -> t k j two", k=4, j=8)[:, :, j, :], reads=[S["DTA"]], writes=[hb["DTA"].r((j, t0))])
        for g in range(2):
            kb.dma(hb["B"][g * TT + t0:g * TT + t0 + T, :], S["B"][:, g * 128:(g + 1) * 128], reads=[S["B"]], writes=[hb["B"].r((g, t0))])
        for nm, nb in (("BT", 2), ("CT", 2), ("KT", 8), ("QT", 8)):
            kb.dma(hb[nm].t.rearrange("(g n) t -> g n t", g=nb)[:, :, t0:t0 + T], S[nm].t.rearrange("(g n) t -> g n t", g=nb),
                   reads=[S[nm]], writes=[hb[nm].r(t0)])

    def personalise():
        for nm, (shp, dt, nb) in HB.items():
            sel = pid if nb == 8 else gid
            src = Gt[nm].t.rearrange("(r j t) c -> r j (t c)", r=8, j=nb)[:, ds(sel, 1), :]
            dst = my[nm].t.rearrange("(r o t) c -> r o (t c)", r=8, o=1)
            kb.dma(dst, src, reads=[Gt[nm]], writes=[my[nm]])

    def srow(c, T):
        if T == TC:
            return 0, TL + c * 128, TL + c * 128
        r, cc_ = c // 16, c % 16
        return r, r * TT + cc_ * 128, cc_ * 128

    def make_load(T):
        def load(c, X_, B_, BT_, CT_, dta_):
            r, row, col = srow(c, T)
            kb.dma(X_[:, :], my["X"][row:row + 128, :], reads=[my["X"]], writes=[X_])
            kb.dma(B_[:, :], my["B"][row:row + 128, :], reads=[my["B"]], writes=[B_])
            kb.dma(BT_[:, :], my["BT"][r * 128:(r + 1) * 128, col:col + 128], reads=[my["BT"]], writes=[BT_])
            kb.dma(CT_[:, :], my["CT"][r * 128:(r + 1) * 128, col:col + 128], reads=[my["CT"]], writes=[CT_])
            kb.dma(dta_[:, :], my["DTA"][row:row + 128, :], reads=[my["DTA"]], writes=[dta_])
        return load

    def ldQc(QT):
        kb.dma(QT[:, :], my["QT"][0:128, TL:TT], reads=[my["QT"]], writes=[QT])

    def ldKc(KT):
        kb.dma(KT[:, :], my["KT"][0:128, TL:TT], reads=[my["KT"]], writes=[KT])

    def ldVc(Va):
        kb.dma(Va[:, 0:2, 0:128], my["V"][TL:TT, :].rearrange("(k p) e -> p k e", p=128), reads=[my["V"]], writes=[Va.r(0)])

    def ldQl(QT):
        for r in range(8):
            kb.dma(QT[:, r * TL:(r + 1) * TL], my["QT"][r * 128:(r + 1) * 128, 0:TL], reads=[my["QT"]], writes=[QT.r(r)])

    def ldKl(KT):
        kb.dma(KT[:, 0:TC], my["KT"][0:128, TL:TT], reads=[my["KT"]], writes=[KT.r("c")])
        for r in range(8):
            kb.dma(KT[:, TC + r * TL:TC + (r + 1) * TL], my["KT"][r * 128:(r + 1) * 128, 0:TL], reads=[my["KT"]], writes=[KT.r(r)])

    def ldVl(Va):
        kb.dma(Va[:, 0:2, 0:128], my["V"][TL:TT, :].rearrange("(k p) e -> p k e", p=128), reads=[my["V"]], writes=[Va.r("c")])
        for r in range(8):
            kb.dma(Va[:, 2 + r * 16:2 + (r + 1) * 16, 0:128], my["V"][r * TT:r * TT + TL, :].rearrange("(k p) e -> p k e", p=128),
                   reads=[my["V"]], writes=[Va.r(r)])
    myYv = myY.t.rearrange("(r t) c -> t r c", r=8)
    myOv = myO.t.rearrange("(r t) c -> t r c", r=8)
    gyc = GYc.t.rearrange("(r t) c -> t r c", r=8)
    goc = GOc.t.rearrange("(r t) c -> t r c", r=8)

    def ldYl(ys, r0):
        kb.dma(ys[:, :].rearrange("p (r c) -> p r c", r=8), myYv[r0:r0 + 128, :, :], reads=[myY], writes=[ys])

    def ldOl(oa, r0):
        kb.dma(oa[:, :].rearrange("p (r c) -> p r c", r=8), myOv[r0:r0 + 128, :, :], reads=[myO], writes=[oa])

    def ldYc(ys, r0):
        kb.dma(ys[:, :].rearrange("p (r c) -> p r c", r=8), gyc[r0:r0 + 128, :, :], reads=[GYc], writes=[ys])

    def ldOc(oa, r0):
        kb.dma(oa[:, :].rearrange("p (r c) -> p r c", r=8), goc[r0:r0 + 128, :, :], reads=[GOc], writes=[oa])
    ga = GaffT.t.rearrange("(r e) n -> e r n", e=16)

    def ld_at(at):
        for e_ in range(16):
            kb.dma(at[e_ * 8:(e_ + 1) * 8, :], ga[e_, :, :], reads=[GaffT], writes=[at.r(e_)])

    with ExitStack() as st0:
        cs = Consts(kb, st0, cst_d)
        for l in range(DEPTH):
            last = l == DEPTH - 1
            lam_init = 0.8 - 0.6 * math.exp(-0.3 * l)
            W = LW[l]
            xl_in = xl_d if l == 0 else xh2
            xc_in = xc_d if l == 0 else xc2
            with ExitStack() as st:
                A = phase_ada(kb, st, cs, cc_d, W["adaw"], W["adab"], W["n1"], W["n2"])
                phase_a(kb, cs, A, 1, TC, xc_in, hmc_d, W, Oc, rope=None, glu_split=1)
                phase_a(kb, cs, A, 0, TL, xl_in, hml_d, W, Ol, rope=(cos_d, sin_d), glu_split=2)
                fill_hb(Oc, TL, TC)
                fill_hb(Ol, 0, TL)
                for nm in HB:
                    kb.allgather(hb[nm], Gt[nm])
                personalise()
                hs = kb.sb(st, [128, 2, 2, 64], F32, "hstate")
                kb.op("dve", lambda e: e.memset(hs[:, :, :, :], 0.0), writes=[hs])
                ssd_scan(kb, cs, st, TC, {"load": make_load(TC), "dsk": W["dsk"]}, Yc_d, hs, True)
                ssd_scan(kb, cs, st, SEQ, {"load": make_load(SEQ), "dsk": W["dsk"]}, Yl_d, hs, False)
                lam_t = compute_lam(kb, st, W["lamp"], lam_init)
                if not last:
                    attention(kb, cs, st, TC, TC, ldQc, ldKc, ldVc, lam_t, Oc_d)
                attention(kb, cs, st, SEQ, SEQ + TC, ldQl, ldKl, ldVl, lam_t, Ol_d)
                kb.allgather(Yl_d, GY)
                kb.allgather(Ol_d, GO)
                for (G_, m_) in ((GY, myY), (GO, myO)):
                    kb.dma(m_.t.rearrange("(r o t) c -> r o (t c)", r=8, o=1),
                           G_.t.rearrange("(r j t) c -> r j (t c)", r=8, j=8)[:, ds(pid, 1), :], reads=[G_], writes=[m_])
                if not last:
                    kb.allgather(Yc_d, GYc)
                    kb.allgather(Oc_d, GOc)
                if not last:
                    phase_d(kb, cs, A, 1, TC, {"ldY": ldYc, "ldO": ldOc, "ZS": Oc["ZS"], "YcT": Oc["YcT"], "G": Oc["G"], "xh": xc_in}, W, Dc, lam_init)
                phase_d(kb, cs, A, 0, TL, {"ldY": ldYl, "ldO": ldOl, "ZS": Ol["ZS"], "YcT": Ol["YcT"], "G": Ol["G"], "xh": xl_in}, W, Dl, lam_init)
                kb.allgather(Dl["affT"], GaffT)
                if not last:
                    xoc = xc2.view(xc2.t[HALO:HALO + TC, :])
                    phase_e(kb, cs, A, 1, TC, TC, 2 * TC // 16, {"h2T": Dc["h2T"], "aff": Dc["aff"], "affT_all": Dc["affT"], "xmid": Dc["xmid"]},
                            W, {"xout": xoc}, final_w=None)
                    xol = xh2.view(xh2.t[HALO:HALO + TL, :])
                    phase_e(kb, cs, A, 0, TL, SEQ, 2 * SEQ // 16, {"h2T": Dl["h2T"], "aff": Dl["aff"], "ld_at": ld_at, "xmid": Dl["xmid"]},
                            W, {"xout": xol}, final_w=None)
                    zt = kb.sb(st, [16, 1024], F32, "zt")
                    kb.op("dve", lambda e: e.memset(zt[:, :], 0.0), writes=[zt])
                    kb.dma(xc2[0:HALO, :], zt[:, :], reads=[zt], writes=[xc2])
                    kb.dma(xc2[HALO + TC:HALO + TC + HALO, :], zt[:, :], reads=[zt], writes=[xc2])
                    kb.dma(Eloc[0:16, :], xh2[HALO:HALO + 16, :], reads=[xh2], writes=[Eloc])
                    kb.dma(Eloc[16:32, :], xh2[TL:TL + 16, :], reads=[xh2], writes=[Eloc])
                    kb.allgather(Eloc, Eall)
                    kb.dma(xh2[0:HALO, :], Eall[ds(eL, 16), :], reads=[Eall], writes=[xh2])
                    kb.dma(xh2[HALO + TL:HALO + TL + HALO, :], Eall[ds(eR, 16), :], reads=[Eall], writes=[xh2])
                else:
                    phase_e(kb, cs, A, 0, TL, SEQ, 2 * SEQ // 16, {"h2T": Dl["h2T"], "aff": Dl["aff"], "ld_at": ld_at, "xmid": Dl["xmid"]},
                            W, {"xout": out_d}, final_w=fw_d)
    return L


def kernel(x, c, ctx, c_ctx, ada_w, ada_b, norm1_w, norm2_w, w_in, ssd_conv_w, ssd_conv_b, ssd_dt_bias, ssd_a_log, ssd_d, ssd_norm_w,
           ssd_out, diff_lambda, diff_subln_w, diff_out, conf_dw_w, conf_dw_b, conf_ln_w, conf_ln_b, conf_out, w_o, router_w,
           exp_w1, exp_w3, exp_w2, final_norm_w):
    f32 = lambda a: np.asarray(a, np.float32)
    L = build_program()
    xpad = np.pad(f32(x)[0], ((HALO, HALO), (0, 0)))
    xcpad = np.pad(f32(ctx)[0], ((HALO, HALO), (0, 0)))
    base = {"cc": np.stack([f32(c)[0], f32(c_ctx)]), "cst": CONST_ARR, "xc": xcpad, "hmc": np.zeros((128, 2), np.float32),
            "fw": f32(final_norm_w)[None]}
    for l in range(DEPTH):
        p = "L%d_" % l
        base.update({p + "adaw": f32(ada_w)[l], p + "adab": f32(ada_b)[l], p + "n1": f32(norm1_w)[l], p + "n2": f32(norm2_w)[l],
                     p + "w_in": f32(w_in)[l], p + "convp": np.concatenate([f32(ssd_conv_w)[l], f32(ssd_conv_b)[l][None]], 0),
                     p + "dtp": np.stack([f32(ssd_dt_bias)[l].reshape(-1), f32(ssd_a_log)[l].reshape(-1)], 1),
                     p + "confp": np.concatenate([f32(conf_dw_w)[l], f32(conf_dw_b)[l][None], f32(conf_ln_w)[l][None], f32(conf_ln_b)[l][None]], 0),
                     p + "conf_out": f32(conf_out)[l],
                     p + "lamp": np.broadcast_to(f32(diff_lambda)[l].reshape(1, 256), (128, 256)),
                     p + "ssd_out": f32(ssd_out)[l], p + "diff_out": f32(diff_out)[l], p + "w_o": f32(w_o)[l], p + "nw": f32(ssd_norm_w)[l][None],
                     p + "sw": np.tile(f32(diff_subln_w)[l], 8)[None], p + "router": f32(router_w)[l],
                     p + "w1": f32(exp_w1)[l].reshape(-1, 2048), p + "w3": f32(exp_w3)[l].reshape(-1, 2048), p + "w2": f32(exp_w2)[l].reshape(-1, 1024)})
    maps = []
    for j in range(NCORE):
        cosj, sinj = rope_tables_host(j * TL, TL)
        hm = np.zeros((128, 2), np.float32)
        hm[:, 0] = 1.0 if j > 0 else 0.0
        hm[:, 1] = 1.0 if j < NCORE - 1 else 0.0
        m = {**base, "xl": xpad[j * TL:j * TL + TL + 32], "hml": hm, "cos": cosj, "sin": sinj}
        for l in range(DEPTH):
            m["L%d_dsk" % l] = np.broadcast_to(f32(ssd_d)[l][2 * j:2 * j + 2][None], (128, 2))
        maps.append(m)
    res = L.run(maps)
    return np.concatenate([res[j]["out"] for j in range(NCORE)], axis=0)[None].astype(np.float32)
```

```python
import math
from contextlib import ExitStack
import numpy as np
import concourse.bass as bass
import concourse.mybir as mybir

F32 = mybir.dt.float32
BF16 = mybir.dt.bfloat16
I32 = mybir.dt.int32
AF = mybir.ActivationFunctionType
ALU = mybir.AluOpType
AX = mybir.AxisListType

NDS = 48


class Trk:
    __slots__ = ("w", "r")

    def __init__(self):
        self.w = None
        self.r = {}


class Buf:
    def __init__(self, kb, t, name):
        self.kb, self.t, self.name = kb, t, name
        self.base = Trk()
        self.reg = {}

    def __getitem__(self, k):
        return self.t[k]

    def r(self, key):
        return (self, key)

    def view(self, ap):
        v = Buf(self.kb, ap, self.name)
        v.base, v.reg = self.base, self.reg
        return v


class KB:
    def __init__(self, nc):
        self.nc = nc
        self.es = ExitStack()
        self.eng = {"pe": nc.tensor, "act": nc.scalar, "dve": nc.vector, "pool": nc.gpsimd, "sp": nc.sync}
        self.sem = {k: self.es.enter_context(nc.semaphore("s_" + k)) for k in self.eng}
        self.cnt = {k: 0 for k in self.eng}
        self.seen = {k: {} for k in self.eng}
        self.dsem = [self.es.enter_context(nc.semaphore("d%d" % i)) for i in range(NDS)]
        self.dcnt = [0] * NDS
        self.dnext = 0
        self.nins = 0
        self.uid = 0
        self.freed = {}

    def _on_free(self, b):
        for tr in [b.base] + list(b.reg.values()):
            toks = list(tr.r.items())
            if tr.w is not None:
                toks.append(tr.w)
            for k, v in toks:
                if self.freed.get(k, 0) < v:
                    self.freed[k] = v

    def _new(self, stack, t, name):
        b = Buf(self, t, name)
        b.base.r = dict(self.freed)
        stack.callback(self._on_free, b)
        return b

    def sb(self, stack, shape, dtype, name=None):
        self.uid += 1
        name = "%s_%d" % (name or "t", self.uid)
        t = stack.enter_context(self.nc.sbuf_tensor(name, list(shape), dtype))
        return self._new(stack, t, name)

    def ps(self, stack, shape, dtype=F32, name=None):
        self.uid += 1
        name = "%s_%d" % (name or "p", self.uid)
        t = stack.enter_context(self.nc.psum_tensor(name, list(shape), dtype))
        return self._new(stack, t, name)

    def dram(self, name, shape, dtype, kind="Internal"):
        t = self.nc.dram_tensor(name, list(shape), dtype, kind=kind)
        return Buf(self, t.ap(), name)

    def _wait(self, e, tok):
        if tok is None:
            return
        key, val = tok
        if self.seen[e].get(key, 0) >= val:
            return
        sem = self.sem[key] if isinstance(key, str) else self.dsem[key[1]]
        self.eng[e].wait_ge(sem, val)
        self.nins += 1
        self.seen[e][key] = val

    @staticmethod
    def _norm(x):
        if isinstance(x, Buf):
            return (x, None)
        return x

    def _trks(self, b, key):
        if key is None:
            return [b.base] + list(b.reg.values())
        if key not in b.reg:
            b.reg[key] = Trk()
        return [b.base, b.reg[key]]

    def _deps(self, e, reads, writes, is_dma):
        toks = []
        for (b, key) in reads:
            for tr in self._trks(b, key):
                if tr.w is not None:
                    toks.append(tr.w)
        for (b, key) in writes:
            for tr in self._trks(b, key):
                if tr.w is not None:
                    if is_dma or tr.w[0] != e or e != "pe":
                        toks.append(tr.w)
                for k, v in tr.r.items():
                    toks.append((k, v))
        for tok in toks:
            self._wait(e, tok)

    def _mark(self, tok, reads, writes):
        for (b, key) in reads:
            if key is None:
                trs = [b.base]
            else:
                trs = [self._trks(b, key)[1]]
            for tr in trs:
                if tr.r.get(tok[0], 0) < tok[1]:
                    tr.r[tok[0]] = tok[1]
        for (b, key) in writes:
            if key is None:
                b.base.w = tok
                b.base.r = {}
                b.reg = {}
            else:
                tr = self._trks(b, key)[1]
                tr.w = tok
                tr.r = {}

    def op(self, e, fn, reads=(), writes=(), sig=True):
        reads = [self._norm(x) for x in reads]
        writes = [self._norm(x) for x in writes]
        self._deps(e, reads, writes, False)
        ins = fn(self.eng[e])
        self.nins += 1
        if sig:
            self.cnt[e] += 1
            ins.then_inc(self.sem[e], 1)
            tok = (e, self.cnt[e])
        else:
            tok = (e, self.cnt[e] + 1)
        self._mark(tok, reads, writes)
        return ins

    def dma(self, out, in_, reads=(), writes=(), q="sp", **kw):
        reads = [self._norm(x) for x in reads]
        writes = [self._norm(x) for x in writes]
        self._deps(q, reads, writes, True)
        s = self.dnext
        self.dnext = (self.dnext + 1) % NDS
        if self.dcnt[s] > 0:
            self._wait(q, (("d", s), self.dcnt[s]))
        ins = self.eng[q].dma_start(out=out, in_=in_, **kw)
        self.dcnt[s] += 16
        ins.then_inc(self.dsem[s], 16)
        self.nins += 1
        tok = (("d", s), self.dcnt[s])
        self._mark(tok, reads, writes)
        return ins

    def allgather(self, src, dst, n=8):
        q = "pool"
        reads, writes = [(src, None)], [(dst, None)]
        self._deps(q, reads, writes, True)
        s = self.dnext
        self.dnext = (self.dnext + 1) % NDS
        if self.dcnt[s] > 0:
            self._wait(q, (("d", s), self.dcnt[s]))
        ins = self.nc.gpsimd.collective_compute("AllGather", ALU.bypass, replica_groups=[list(range(n))],
                                                ins=[src.t.opt()], outs=[dst.t.opt()])
        self.dcnt[s] += 1
        ins.then_inc(self.dsem[s], 1)
        self.nins += 1
        self._mark((("d", s), self.dcnt[s]), reads, writes)

    def finish(self):
        for s in range(NDS):
            if self.dcnt[s] > 0:
                self._wait("sp", (("d", s), self.dcnt[s]))
        for e in ("pe", "act", "dve", "pool"):
            if self.cnt[e] > 0:
                self._wait("sp", (e, self.cnt[e]))
        self.es.close()


D = 1024
HALO = 16
EPS = 1e-6
C_X, C_B, C_DT, C_K, C_V, C_C, C_Q, C_Z, C_GLU, C_GATE = 0, 1024, 1280, 1312, 2336, 3360, 3616, 4640, 5664, 7712
NCOLS = 10784


def make_consts():
    c = {}
    c["ident"] = np.eye(128, dtype=np.float32)
    k = np.arange(128)
    c["triu"] = (k[:, None] <= k[None, :]).astype(np.float32)
    c["tril"] = (k[:, None] >= k[None, :]).astype(np.float32)
    c["negu"] = np.where(k[:, None] <= k[None, :], 0.0, -30000.0).astype(np.float32)
    c["negl"] = np.where(k[:, None] >= k[None, :], 0.0, -30000.0).astype(np.float32)
    c["ones"] = np.ones((128, 128), np.float32)
    blk = (k[:, None] // 8 == k[None, :] // 8).astype(np.float32)
    c["blk8"] = blk
    sel = np.zeros((128, 16), np.float32)
    sel[np.arange(16) * 8, np.arange(16)] = 1.0
    c["sel16"] = np.pad(sel, ((0, 0), (0, 112)))
    names = ["ident", "triu", "tril", "negu", "negl", "ones", "blk8", "sel16"]
    arr = np.concatenate([c[n] for n in names], axis=1)
    return names, arr


CONST_NAMES, CONST_ARR = make_consts()


class Consts:
    def __init__(self, kb, stack, cst_dram):
        n = len(CONST_NAMES)
        self.f = kb.sb(stack, [128, n * 128], F32, "cstf")
        self.b = kb.sb(stack, [128, n * 128], BF16, "cstb")
        kb.dma(self.f[:, :], cst_dram[:, :], reads=[cst_dram], writes=[self.f])
        kb.op("dve", lambda e: e.tensor_copy(out=self.b[:, :], in_=self.f[:, :]), reads=[self.f], writes=[self.b])
        self.idx = {nm: i for i, nm in enumerate(CONST_NAMES)}
        self.eps = kb.sb(stack, [128, 2], F32, "epsc")
        kb.op("dve", lambda e: e.memset(self.eps[:, 0:1], EPS), writes=[self.eps])
        kb.op("dve", lambda e: e.memset(self.eps[:, 1:2], 1.0), writes=[self.eps])

    def F(self, nm, rows=128, cols=128):
        i = self.idx[nm]
        return self.f[0:rows, i * 128:i * 128 + cols]

    def B(self, nm, rows=128, cols=128):
        i = self.idx[nm]
        return self.b[0:rows, i * 128:i * 128 + cols]


def phase_ada(kb, stack, cs, cc_d, adaw_d, adab_d, n1_d, n2_d):
    out = {}
    mods = kb.sb(stack, [128, 48, 2], F32, "mods")
    for nm in ("gw1", "gw2"):
        out[nm] = kb.sb(stack, [128, 8, 2], F32, nm)
    with ExitStack() as st:
        cT = kb.sb(st, [128, 2, 8], F32, "cT")
        sc = kb.sb(st, [128, 8, 2], F32, "sc")
        ab = kb.sb(st, [128, 48], F32, "ab")
        nw = kb.sb(st, [128, 2, 8], F32, "nw")
        for v in range(2):
            kb.dma(cT[:, v, :], cc_d[v, :].rearrange("(t p) -> p t", p=128), reads=[cc_d], writes=[cT],
                   allow_slow_non_contiguous=True)
        kb.dma(ab[:, :], adab_d[:].rearrange("(t p) -> p t", p=128), reads=[adab_d], writes=[ab],
               allow_slow_non_contiguous=True)
        kb.dma(nw[:, 0, :], n1_d[:].rearrange("(t p) -> p t", p=128), reads=[n1_d], writes=[nw],
               allow_slow_non_contiguous=True)
        kb.dma(nw[:, 1, :], n2_d[:].rearrange("(t p) -> p t", p=128), reads=[n2_d], writes=[nw],
               allow_slow_non_contiguous=True)
        kb.op("act", lambda e: e.activation(out=sc[:, :, :], in_=cT[:, :, :].rearrange("p v t -> p t v"), func=AF.Silu),
              reads=[cT], writes=[sc])
        pm = kb.ps(st, [128, 48, 2], F32, "pm")
        wb = [kb.sb(st, [128, 6144], F32, "adaw%d" % i) for i in range(2)]
        for dt in range(8):
            w = wb[dt % 2]
            kb.dma(w[:, :], adaw_d[dt * 128:(dt + 1) * 128, :], reads=[adaw_d], writes=[w])
            for ft in range(48):
                kb.op("pe", lambda e: e.matmul(pm[:, ft, :], lhsT=w[:, ft * 128:(ft + 1) * 128], rhs=sc[:, dt, :],
                                               start=(dt == 0 and ft == 0), stop=(dt == 7 and ft == 47), skip_group_check=True),
                      reads=[w, sc], writes=[pm], sig=(ft == 47))
        for v in range(2):
            kb.op("dve", lambda e: e.tensor_tensor(out=mods[:, :, v], in0=pm[:, :, v], in1=ab[:, :], op=ALU.add),
                  reads=[pm, ab], writes=[mods])
        for i, nm in enumerate(("gw1", "gw2")):
            sc0 = 8 + 24 * i
            for v in range(2):
                kb.op("dve", lambda e: e.scalar_tensor_tensor(out=out[nm][:, :, v], in0=mods[:, sc0:sc0 + 8, v], scalar=1.0,
                                                              in1=nw[:, i, :], op0=ALU.add, op1=ALU.mult),
                      reads=[mods, nw], writes=[out[nm]])
    out["mods"] = mods
    return out


def load_T(kb, st, dst, src_d, J, C, cs, pspool):
    with ExitStack() as s2:
        tmp = kb.sb(s2, [J, C], F32, "ldT")
        kb.dma(tmp[:, :], src_d[:, :], reads=[src_d], writes=[tmp])
        for ct in range(C // 128):
            kb.op("pe", lambda e: e.transpose(out=pspool[:, 0:J], in_=tmp[0:J, ct * 128:(ct + 1) * 128], identity=cs.F("ident", J, J)),
                  reads=[tmp, cs.f], writes=[pspool])
            kb.op("dve", lambda e: e.tensor_copy(out=dst[:, ct, 0:J], in_=pspool[:, 0:J]), reads=[pspool], writes=[dst])


class WLoader:
    def __init__(self, kb, st, kt=8, n=512, nbuf=2, nstg=None):
        self.kb = kb
        self.kt = kt
        self.nstg = nstg or nbuf
        self.stg = [kb.sb(st, [128, kt, n], F32, "wst") for _ in range(self.nstg)]
        self.wbf = [kb.sb(st, [128, kt, n], BF16, "wbf") for _ in range(nbuf)]
        self.i = 0
        self.j = 0
        self.nbuf = nbuf

    def load(self, w_d, r0, c0, n, q="sp"):
        kb = self.kb
        i = self.i
        self.i = (self.i + 1) % self.nbuf
        s, b = self.stg[self.j], self.wbf[i]
        self.j = (self.j + 1) % self.nstg
        kb.dma(s[:, :, 0:n], w_d[r0:r0 + self.kt * 128, c0:c0 + n].rearrange("(t p) c -> p t c", p=128),
               reads=[w_d], writes=[s], q=q)
        kb.op("pool", lambda e: e.tensor_copy(out=b[:, :, 0:n], in_=s[:, :, 0:n]), reads=[s], writes=[b])
        return b, s


def blocks(t0, t1, n=512):
    out = []
    while t0 < t1:
        m = min(n, t1 - t0)
        out.append((t0, m))
        t0 += m
    return out


def phase_a(kb, cs, A, v, T, xh_d, hm_d, W, O, rope=None, glu_split=1, stop=99):
    TE = T + 2 * HALO
    NT = T // 128
    with ExitStack() as st:
        hT = kb.sb(st, [128, 8, TE], BF16, "hT")
        hm = kb.sb(st, [128, 2], F32, "hm")
        kb.dma(hm[:, :], hm_d[:, :], reads=[hm_d], writes=[hm])
        mods, gw1 = A["mods"], A["gw1"]
        with ExitStack() as s1:
            xb = [kb.sb(s1, [128, 1024], F32, "xb") for _ in range(2)]
            junk = kb.sb(s1, [128, 1024], F32, "junk")
            xn = [kb.sb(s1, [128, 1024], BF16, "xn") for _ in range(2)]
            ss = [kb.sb(s1, [128, 2], F32, "ss") for _ in range(2)]
            pt = [kb.ps(s1, [128, 8, 128], BF16, "pt") for _ in range(2)]
            for i in range((TE + 127) // 128):
                r = min(128, TE - i * 128)
                x_, xn_, ss_, pt_ = xb[i % 2], xn[i % 2], ss[i % 2], pt[i % 2]
                kb.dma(x_[0:r, :], xh_d[i * 128:i * 128 + r, :], reads=[xh_d], writes=[x_])
                kb.op("dve", lambda e: e.memset(ss_[:, :], 0.0), writes=[ss_])
                kb.op("act", lambda e: e.activation(out=junk[0:r, :], in_=x_[0:r, :], func=AF.Square, accum_out=ss_[0:r, 0:1]),
                      reads=[x_], writes=[junk, ss_])
                kb.op("act", lambda e: e.activation(out=ss_[0:r, 1:2], in_=ss_[0:r, 0:1], func=AF.Sqrt, scale=1.0 / D, bias=cs.eps[0:r, 0:1]),
                      reads=[ss_, cs.eps], writes=[ss_])
                kb.op("dve", lambda e: e.reciprocal(out=ss_[0:r, 0:1], in_=ss_[0:r, 1:2]), reads=[ss_], writes=[ss_])
                kb.op("dve", lambda e: e.tensor_scalar(out=xn_[0:r, :], in0=x_[0:r, :], scalar1=ss_[0:r, 0:1], scalar2=None,
                                                       op0=ALU.mult), reads=[x_, ss_], writes=[xn_])
                for dt in range(8):
                    kb.op("pe", lambda e: e.transpose(out=pt_[:, dt, 0:r], in_=xn_[0:r, dt * 128:(dt + 1) * 128],
                                                      identity=cs.B("ident", r, r)), reads=[xn_, cs.b], writes=[pt_], sig=(dt == 7))
                for dt in range(8):
                    kb.op("act", lambda e: e.activation(out=hT[:, dt, i * 128:i * 128 + r], in_=pt_[:, dt, 0:r], func=AF.Identity,
                                                        scale=gw1[:, dt, v:v + 1], bias=mods[:, dt, v:v + 1]),
                          reads=[pt_, gw1, mods], writes=[hT.r(i)])
        win = W["w_in"]
        wl = WLoader(kb, st)
        psA = [kb.ps(st, [128, 512], F32, "psA") for _ in range(2)]
        psB = [kb.ps(st, [128, 512], F32, "psB") for _ in range(2)]
        pcnt = [0]

        def fm(ps, wb, cloc, ncol, t0, n):
            for dt in range(8):
                kb.op("pe", lambda e: e.matmul(ps[0:ncol, 0:n], lhsT=wb[:, dt, cloc:cloc + ncol], rhs=hT[:, dt, t0:t0 + n],
                                               start=(dt == 0), stop=(dt == 7)), reads=[wb, hT], writes=[ps], sig=(dt == 7))

        def tm(ps, wb, tt0, n):
            for dt in range(8):
                kb.op("pe", lambda e: e.matmul(ps[:, 0:n], lhsT=hT[:, dt, tt0:tt0 + 128], rhs=wb[:, dt, 0:n],
                                               start=(dt == 0), stop=(dt == 7)), reads=[wb, hT], writes=[ps], sig=(dt == 7))

        def nps(pool):
            pcnt[0] += 1
            return pool[pcnt[0] % 2]

        cwb = kb.sb(st, [128, 12, 4], F32, "cwb")
        load_T(kb, st, cwb, W["convp"], 4, 1536, cs, psA[0])
        if stop <= 0:
            return
        with ExitStack() as s2:
            Prow = [kb.sb(s2, [128, TE], F32, "Prow") for _ in range(2)]
            acc = kb.sb(s2, [128, T], F32, "acc")
            U = [kb.sb(s2, [128, T], BF16, "U") for _ in range(2)]
            Xtm = kb.sb(s2, [128, NT, 1024], BF16, "Xtm")
            Btm = kb.sb(s2, [128, NT, 256], BF16, "Btm")
            ptb = [kb.ps(s2, [128, 8, 128], BF16, "ptb") for _ in range(2)]
            k = 0
            for (gname, c0, ng, cch0) in (("x", C_X, 1024, 0), ("b", C_B, 256, 1024), ("c", C_C, 256, 1280)):
                for cc0 in range(0, ng, 512):
                    n = min(512, ng - cc0)
                    wb, _ = wl.load(win, 0, c0 + cc0, n)
                    for ci in range(n // 128):
                        ctg = (cc0 + ci * 128) // 128
                        cch = (cch0 // 128) + ctg
                        P_, U_ = Prow[k % 2], U[k % 2]
                        k += 1
                        for (t0, nn) in blocks(0, TE):
                            ps = nps(psA)
                            fm(ps, wb, ci * 128, 128, t0, nn)
                            kb.op("act", lambda e: e.copy(out=P_[:, t0:t0 + nn], in_=ps[:, 0:nn]), reads=[ps], writes=[P_])
                        kb.op("dve", lambda e: e.tensor_scalar(out=P_[:, 0:HALO], in0=P_[:, 0:HALO], scalar1=hm[:, 0:1], scalar2=None,
                                                               op0=ALU.mult), reads=[P_, hm], writes=[P_])
                        kb.op("dve", lambda e: e.tensor_scalar(out=P_[:, TE - HALO:TE], in0=P_[:, TE - HALO:TE], scalar1=hm[:, 1:2],
                                                               scalar2=None, op0=ALU.mult), reads=[P_, hm], writes=[P_])
                        kb.op("dve", lambda e: e.tensor_scalar(out=acc[:, :], in0=P_[:, 15:15 + T], scalar1=cwb[:, cch, 0:1], scalar2=None,
                                                               op0=ALU.mult), reads=[P_, cwb], writes=[acc])
                        for j in (1, 2):
                            kb.op("dve", lambda e: e.scalar_tensor_tensor(out=acc[:, :], in0=P_[:, 15 + j:15 + j + T],
                                                                          scalar=cwb[:, cch, j:j + 1], in1=acc[:, :],
                                                                          op0=ALU.mult, op1=ALU.add), reads=[P_, cwb, acc], writes=[acc])
                        kb.op("act", lambda e: e.activation(out=U_[:, :], in_=acc[:, :], func=AF.Silu, bias=cwb[:, cch, 3:4]),
                              reads=[acc, cwb], writes=[U_])
                        if gname in ("b", "c"):
                            od = O["BT"] if gname == "b" else O["CT"]
                            kb.dma(od[ctg * 128:(ctg + 1) * 128, :], U_[:, :], reads=[U_], writes=[od.r(ctg)])
                        if gname in ("x", "b"):
                            dst = Xtm if gname == "x" else Btm
                            for tt0 in range(0, NT, 8):
                                nt = min(8, NT - tt0)
                                p_ = nps(ptb)
                                for tt in range(nt):
                                    kb.op("pe", lambda e: e.transpose(out=p_[:, tt, :], in_=U_[:, (tt0 + tt) * 128:(tt0 + tt + 1) * 128],
                                                                      identity=cs.B("ident")), reads=[U_, cs.b], writes=[p_], sig=(tt == nt - 1))
                                kb.op("dve", lambda e: e.tensor_copy(out=dst[:, tt0:tt0 + nt, ctg * 128:(ctg + 1) * 128], in_=p_[:, 0:nt, :]),
                                      reads=[p_], writes=[dst])
            kb.dma(O["X"][:, :].rearrange("(t p) c -> p t c", p=128), Xtm[:, :, :], reads=[Xtm], writes=[O["X"]])
            kb.dma(O["B"][:, :].rearrange("(t p) c -> p t c", p=128), Btm[:, :, :], reads=[Btm], writes=[O["B"]])
        if stop <= 1:
            return
        with ExitStack() as s2:
            dtp = kb.sb(s2, [32, 4], F32, "dtp")
            kb.dma(dtp[:, 0:2], W["dtp"][:, :], reads=[W["dtp"]], writes=[dtp])
            kb.op("act", lambda e: e.activation(out=dtp[:, 2:3], in_=dtp[:, 1:2], func=AF.Exp), reads=[dtp], writes=[dtp])
            kb.op("dve", lambda e: e.tensor_scalar(out=dtp[:, 3:4], in0=dtp[:, 2:3], scalar1=-1.0, scalar2=None, op0=ALU.mult),
                  reads=[dtp], writes=[dtp])
            ex = kb.sb(s2, [32, T], F32, "ex")
            dta = kb.sb(s2, [32, 2, T], F32, "dta")
            DTtm = kb.sb(s2, [128, NT, 64], F32, "DTtm")
            wb, _ = wl.load(win, 0, C_DT, 32)
            for (t0, nn) in blocks(HALO, HALO + T):
                ps = nps(psA)
                fm(ps, wb, 0, 32, t0, nn)
                kb.op("act", lambda e: e.activation(out=ex[:, t0 - HALO:t0 - HALO + nn], in_=ps[0:32, 0:nn], func=AF.Exp, bias=dtp[:, 0:1]),
                      reads=[ps, dtp], writes=[ex])
            kb.op("act", lambda e: e.activation(out=dta[:, 0, :], in_=ex[:, :], func=AF.Ln, bias=cs.eps[0:32, 1:2]), reads=[ex, cs.eps], writes=[dta])
            kb.op("dve", lambda e: e.tensor_scalar(out=dta[:, 1, :], in0=dta[:, 0, :], scalar1=dtp[:, 3:4], scalar2=None, op0=ALU.mult),
                  reads=[dta, dtp], writes=[dta])
            for tt in range(NT):
                ps = nps(psA)
                for j in range(2):
                    kb.op("pe", lambda e: e.transpose(out=ps[:, j * 32:(j + 1) * 32], in_=dta[:, j, tt * 128:(tt + 1) * 128],
                                                      identity=cs.F("ident", 32, 32)), reads=[dta, cs.f], writes=[ps])
                kb.op("dve", lambda e: e.tensor_copy(out=DTtm[:, tt, :], in_=ps[:, 0:64]), reads=[ps], writes=[DTtm])
            kb.dma(O["DTA"][:, :].rearrange("(t p) c -> p t c", p=128), DTtm[:, :, :], reads=[DTtm], writes=[O["DTA"]])
        if stop <= 2:
            return
        with ExitStack() as s2:
            if rope is not None:
                cos = kb.sb(s2, [128, T], F32, "cos")
                sin = kb.sb(s2, [128, T], F32, "sin")
                kb.dma(cos[:, :], rope[0][:, :], reads=[rope[0]], writes=[cos])
                kb.dma(sin[:, :], rope[1][:, :], reads=[rope[1]], writes=[sin])
                wrot = kb.sb(s2, [128, 8, 512], BF16, "wrot")
                t1 = kb.sb(s2, [128, 512], F32, "rt1")
                t2 = kb.sb(s2, [128, 512], F32, "rt2")
            R = [kb.sb(s2, [128, T], BF16, "R") for _ in range(2)]
            k = 0
            for (c0, od) in ((C_K, O["KT"]), (C_Q, O["QT"])):
                for cc0 in range(0, 1024, 512):
                    wb, stg = wl.load(win, 0, c0 + cc0, 512)
                    if rope is not None:
                        sv = stg[:, :, :].rearrange("p t (g h f) -> p t g h f", h=2, f=16)
                        rv = wrot[:, :, :].rearrange("p t (g h f) -> p t g h f", h=2, f=16)
                        for h in range(2):
                            kb.op("pool", lambda e: e.tensor_copy(out=rv[:, :, :, h, :], in_=sv[:, :, :, 1 - h, :]), reads=[stg], writes=[wrot])
                    for ci in range(4):
                        ht = (cc0 // 128) + ci
                        R_ = R[k % 2]
                        k += 1
                        for (t0, nn) in blocks(HALO, HALO + T):
                            o0 = t0 - HALO
                            ps = nps(psA)
                            fm(ps, wb, ci * 128, 128, t0, nn)
                            if rope is None:
                                kb.op("act", lambda e: e.copy(out=R_[:, o0:o0 + nn], in_=ps[:, 0:nn]), reads=[ps], writes=[R_])
                            else:
                                ps2 = nps(psB)
                                fm(ps2, wrot, ci * 128, 128, t0, nn)
                                kb.op("dve", lambda e: e.tensor_tensor(out=t1[:, 0:nn], in0=ps[:, 0:nn], in1=cos[:, o0:o0 + nn], op=ALU.mult),
                                      reads=[ps, cos], writes=[t1])
                                kb.op("dve", lambda e: e.tensor_tensor(out=t2[:, 0:nn], in0=ps2[:, 0:nn], in1=sin[:, o0:o0 + nn], op=ALU.mult),
                                      reads=[ps2, sin], writes=[t2])
                                kb.op("pool", lambda e: e.tensor_tensor(out=R_[:, o0:o0 + nn], in0=t1[:, 0:nn], in1=t2[:, 0:nn], op=ALU.add),
                                      reads=[t1, t2], writes=[R_])
                        kb.dma(od[ht * 128:(ht + 1) * 128, :], R_[:, :], reads=[R_], writes=[od.r(ht)])
        if stop <= 3:
            return
        with ExitStack() as s2:
            Vtm = kb.sb(s2, [128, NT, 1024], BF16, "Vtm")
            for (c0, od, fn) in ((C_V, O["V"], None), (C_Z, O["ZS"], AF.Silu)):
                for cc0 in range(0, 1024, 512):
                    wb, _ = wl.load(win, 0, c0 + cc0, 512)
                    for tt in range(NT):
                        ps = nps(psA)
                        tm(ps, wb, HALO + tt * 128, 512)
                        if fn is None:
                            kb.op("act", lambda e: e.copy(out=Vtm[:, tt, cc0:cc0 + 512], in_=ps[:, :]), reads=[ps], writes=[Vtm])
                        else:
                            kb.op("act", lambda e: e.activation(out=Vtm[:, tt, cc0:cc0 + 512], in_=ps[:, :], func=fn), reads=[ps], writes=[Vtm])
                kb.dma(od[:, :].rearrange("(t p) c -> p t c", p=128), Vtm[:, :, :], reads=[Vtm], writes=[od])
        if stop <= 4:
            return
        with ExitStack() as s2:
            Gb = [kb.sb(s2, [128, T], BF16, "Gb") for _ in range(2)]
            k = 0
            for cc0 in range(0, 3072, 512):
                wb, _ = wl.load(win, 0, C_GATE + cc0, 512)
                for ci in range(4):
                    ct = cc0 // 128 + ci
                    G_ = Gb[k % 2]
                    k += 1
                    for (t0, nn) in blocks(HALO, HALO + T):
                        ps = nps(psA)
                        fm(ps, wb, ci * 128, 128, t0, nn)
                        kb.op("act", lambda e: e.activation(out=G_[:, t0 - HALO:t0 - HALO + nn], in_=ps[:, 0:nn], func=AF.Sigmoid),
                              reads=[ps], writes=[G_])
                    kb.dma(O["G"][ct * 128:(ct + 1) * 128, :], G_[:, :], reads=[G_], writes=[O["G"].r(ct)])
        if stop <= 5:
            return
        with ExitStack() as s2:
            cfp = kb.sb(s2, [128, 8, 34], F32, "cfp")
            load_T(kb, s2, cfp, W["confp"], 34, 1024, cs, psA[0])
            TH = T // glu_split
            THE = TH + 2 * HALO
            Urow = [kb.sb(s2, [128, THE], F32, "Urow") for _ in range(2)]
            sig = kb.sb(s2, [128, 512], F32, "sig")
            accA = kb.sb(s2, [128, TH], F32, "accA")
            accB = kb.sb(s2, [128, TH], F32, "accB")
            CV = kb.sb(s2, [128, 8, TH], F32, "CV")
            sq = kb.sb(s2, [128, 512], F32, "sq")
            mean = kb.sb(s2, [128, 512], F32, "mean")
            rstd = kb.sb(s2, [128, 512], F32, "rstd")
            tmpn = kb.sb(s2, [128, 512], F32, "tmpn")
            UcT = kb.sb(s2, [128, 8, 512], BF16, "UcT")
            yo = [kb.sb(s2, [128, 512], F32, "yo") for _ in range(2)]
            k = 0
            for hh in range(glu_split):
                base = hh * TH
                for half in range(2):
                    wa, _ = wl.load(win, 0, C_GLU + half * 512, 512)
                    wg, _ = wl.load(win, 0, C_GLU + 1024 + half * 512, 512)
                    for ci in range(4):
                        ct = half * 4 + ci
                        U_ = Urow[k % 2]
                        k += 1
                        for (t0, nn) in blocks(0, THE):
                            pa_, pg_ = nps(psA), nps(psB)
                            fm(pa_, wa, ci * 128, 128, base + t0, nn)
                            fm(pg_, wg, ci * 128, 128, base + t0, nn)
                            kb.op("act", lambda e: e.activation(out=sig[:, 0:nn], in_=pg_[:, 0:nn], func=AF.Sigmoid), reads=[pg_], writes=[sig])
                            kb.op("dve", lambda e: e.tensor_tensor(out=U_[:, t0:t0 + nn], in0=pa_[:, 0:nn], in1=sig[:, 0:nn], op=ALU.mult),
                                  reads=[pa_, sig], writes=[U_])
                        if hh == 0:
                            kb.op("dve", lambda e: e.tensor_scalar(out=U_[:, 0:HALO], in0=U_[:, 0:HALO], scalar1=hm[:, 0:1], scalar2=None,
                                                                   op0=ALU.mult), reads=[U_, hm], writes=[U_])
                        if hh == glu_split - 1:
                            kb.op("dve", lambda e: e.tensor_scalar(out=U_[:, THE - HALO:THE], in0=U_[:, THE - HALO:THE], scalar1=hm[:, 1:2],
                                                                   scalar2=None, op0=ALU.mult), reads=[U_, hm], writes=[U_])
                        kb.op("dve", lambda e: e.tensor_scalar(out=accA[:, :], in0=U_[:, 1:1 + TH], scalar1=cfp[:, ct, 0:1], scalar2=cfp[:, ct, 31:32],
                                                               op0=ALU.mult, op1=ALU.add), reads=[U_, cfp], writes=[accA])
                        for j in range(1, 31):
                            dst = CV[:, ct, :] if j == 30 else accA[:, :]
                            kb.op("dve", lambda e: e.scalar_tensor_tensor(out=dst, in0=U_[:, 1 + j:1 + j + TH], scalar=cfp[:, ct, j:j + 1],
                                                                          in1=accA[:, :], op0=ALU.mult, op1=ALU.add),
                                  reads=[U_, cfp, accA], writes=[accA, CV])
                for (t0, nn) in blocks(0, TH):
                    p1, p2 = nps(psA), nps(psB)
                    for ct in range(8):
                        kb.op("act", lambda e: e.activation(out=sq[:, 0:nn], in_=CV[:, ct, t0:t0 + nn], func=AF.Square), reads=[CV], writes=[sq])
                        kb.op("pe", lambda e: e.matmul(p1[:, 0:nn], lhsT=cs.F("ones"), rhs=CV[:, ct, t0:t0 + nn], start=(ct == 0), stop=(ct == 7)),
                              reads=[CV, cs.f], writes=[p1])
                        kb.op("pe", lambda e: e.matmul(p2[:, 0:nn], lhsT=cs.F("ones"), rhs=sq[:, 0:nn], start=(ct == 0), stop=(ct == 7)),
                              reads=[sq, cs.f], writes=[p2])
                    kb.op("dve", lambda e: e.tensor_scalar(out=mean[:, 0:nn], in0=p1[:, 0:nn], scalar1=1.0 / 1024, scalar2=None, op0=ALU.mult),
                          reads=[p1], writes=[mean])
                    kb.op("dve", lambda e: e.tensor_tensor(out=tmpn[:, 0:nn], in0=mean[:, 0:nn], in1=mean[:, 0:nn], op=ALU.mult),
                          reads=[mean], writes=[tmpn])
                    kb.op("dve", lambda e: e.scalar_tensor_tensor(out=rstd[:, 0:nn], in0=p2[:, 0:nn], scalar=1.0 / 1024, in1=tmpn[:, 0:nn],
                                                                  op0=ALU.mult, op1=ALU.subtract), reads=[p2, tmpn], writes=[rstd])
                    kb.op("act", lambda e: e.activation(out=tmpn[:, 0:nn], in_=rstd[:, 0:nn], func=AF.Sqrt, bias=cs.eps[:, 0:1]),
                          reads=[rstd, cs.eps], writes=[tmpn])
                    kb.op("dve", lambda e: e.reciprocal(out=rstd[:, 0:nn], in_=tmpn[:, 0:nn]), reads=[tmpn], writes=[rstd])
                    for ct in range(8):
                        kb.op("dve", lambda e: e.tensor_tensor(out=tmpn[:, 0:nn], in0=CV[:, ct, t0:t0 + nn], in1=mean[:, 0:nn], op=ALU.subtract),
                              reads=[CV, mean], writes=[tmpn])
                        kb.op("dve", lambda e: e.tensor_tensor(out=sq[:, 0:nn], in0=tmpn[:, 0:nn], in1=rstd[:, 0:nn], op=ALU.mult),
                              reads=[tmpn, rstd], writes=[sq])
                        kb.op("act", lambda e: e.activation(out=UcT[:, ct, 0:nn], in_=sq[:, 0:nn], func=AF.Silu, scale=cfp[:, ct, 32:33],
                                                            bias=cfp[:, ct, 33:34]), reads=[sq, cfp], writes=[UcT])
                    for half in range(2):
                        wc, _ = wl.load(W["conf_out"], 0, half * 512, 512)
                        for ci in range(4):
                            dm = half * 4 + ci
                            ps = nps(psA)
                            for ct in range(8):
                                kb.op("pe", lambda e: e.matmul(ps[:, 0:nn], lhsT=wc[:, ct, ci * 128:(ci + 1) * 128], rhs=UcT[:, ct, 0:nn],
                                                               start=(ct == 0), stop=(ct == 7)), reads=[wc, UcT], writes=[ps], sig=(ct == 7))
                            y_ = yo[k % 2]
                            k += 1
                            kb.op("act", lambda e: e.copy(out=y_[:, 0:nn], in_=ps[:, 0:nn]), reads=[ps], writes=[y_])
                            kb.dma(O["YcT"][dm * 128:(dm + 1) * 128, base + t0:base + t0 + nn], y_[:, 0:nn], reads=[y_],
                                   writes=[O["YcT"].r((dm, hh, t0))])


def ssd_scan(kb, cs, st, T, I, Yd, hstate, first):
    NC = T // 128
    with ExitStack() as s:
        Yacc = kb.sb(s, [128, NC, 128], F32, "Yacc")
        dsk = kb.sb(s, [128, 2], F32, "dsk")
        kb.dma(dsk[:, :], I["dsk"][:, :], reads=[I["dsk"]], writes=[dsk])
        Xc = [kb.sb(s, [128, 128], BF16, "Xc") for _ in range(2)]
        Bc = [kb.sb(s, [128, 128], BF16, "Bc") for _ in range(2)]
        BTc = [kb.sb(s, [128, 128], BF16, "BTc") for _ in range(2)]
        CTc = [kb.sb(s, [128, 128], BF16, "CTc") for _ in range(2)]
        dta = [kb.sb(s, [128, 8], F32, "dta") for _ in range(2)]
        cum = kb.sb(s, [128, 16], F32, "cum")
        tot = kb.sb(s, [128, 8], F32, "tot")
        aT = kb.sb(s, [128, 128], F32, "aT")
        decT = kb.sb(s, [128, 128], F32, "decT")
        WT = kb.sb(s, [128, 128], BF16, "WT")
        Xdt = kb.sb(s, [128, 64], BF16, "Xdt")
        Xw = kb.sb(s, [128, 64], BF16, "Xw")
        hbf = kb.sb(s, [128, 64], BF16, "hbf")
        ytmp = kb.sb(s, [128, 64], F32, "ytmp")
        p_sc = kb.ps(s, [128, 512], F32, "p_sc")
        p_cum = kb.ps(s, [128, 512], F32, "p_cum")
        p_dec = kb.ps(s, [128, 512], F32, "p_dec")
        p_yd = kb.ps(s, [128, 512], F32, "p_yd")
        p_yo = kb.ps(s, [128, 512], F32, "p_yo")
        p_st = kb.ps(s, [128, 512], F32, "p_st")
        it = 0
        for d in range(2):
            tri = "triu" if d == 0 else "tril"
            neg = "negu" if d == 0 else "negl"
            order = range(NC) if d == 0 else range(NC - 1, -1, -1)
            for c in order:
                b = it % 2
                it += 1
                X_, B_, BT_, CT_, dta_ = Xc[b], Bc[b], BTc[b], CTc[b], dta[b]
                r0 = c * 128
                if "load" in I:
                    I["load"](c, X_, B_, BT_, CT_, dta_)
                else:
                    kb.dma(X_[:, :], I["X"][r0:r0 + 128, :], reads=[I["X"]], writes=[X_])
                    kb.dma(B_[:, :], I["B"][r0:r0 + 128, :], reads=[I["B"]], writes=[B_])
                    kb.dma(BT_[:, :], I["BT"][:, r0:r0 + 128], reads=[I["BT"]], writes=[BT_])
                    kb.dma(CT_[:, :], I["CT"][:, r0:r0 + 128], reads=[I["CT"]], writes=[CT_])
                    kb.dma(dta_[:, :], I["DTA"][r0:r0 + 128, :], reads=[I["DTA"]], writes=[dta_])
                acol = 4 + 2 * d
                dcol = 2 * d
                kb.op("pe", lambda e: e.matmul(p_cum[:, 0:2], lhsT=cs.F(tri), rhs=dta_[:, acol:acol + 2], start=True, stop=True),
                      reads=[cs.f, dta_], writes=[p_cum])
                kb.op("pe", lambda e: e.matmul(p_cum[:, 2:4], lhsT=cs.F("ones"), rhs=dta_[:, acol:acol + 2], start=True, stop=True), reads=[cs.f, dta_], writes=[p_cum])
                kb.op("dve", lambda e: e.tensor_copy(out=cum[:, 0:2], in_=p_cum[:, 0:2]), reads=[p_cum], writes=[cum])
                kb.op("dve", lambda e: e.tensor_scalar(out=cum[:, 4:6], in0=p_cum[:, 0:2], scalar1=-1.0, scalar2=None, op0=ALU.mult),
                      reads=[p_cum], writes=[cum])
                kb.op("act", lambda e: e.activation(out=cum[:, 8:10], in_=p_cum[:, 0:2], func=AF.Exp), reads=[p_cum], writes=[cum])
                kb.op("dve", lambda e: e.tensor_tensor(out=tot[:, 0:2], in0=p_cum[:, 2:4], in1=cum[:, 0:2], op=ALU.subtract),
                      reads=[p_cum, cum], writes=[tot])
                kb.op("act", lambda e: e.activation(out=cum[:, 12:14], in_=tot[:, 0:2], func=AF.Exp), reads=[tot], writes=[cum])
                kb.op("act", lambda e: e.activation(out=tot[:, 4:6], in_=p_cum[:, 2:4], func=AF.Exp), reads=[p_cum], writes=[tot])
                kb.op("pe", lambda e: e.matmul(p_sc[:, 0:128], lhsT=BT_[:, :], rhs=CT_[:, :], start=True, stop=True), reads=[BT_, CT_], writes=[p_sc])
                for h in range(2):
                    hs = hstate[:, d, h, :]
                    kb.op("dve", lambda e: e.tensor_scalar(out=aT[:, :], in0=cs.F(tri), scalar1=dta_[:, acol + h:acol + h + 1], scalar2=None,
                                                           op0=ALU.mult), reads=[cs.f, dta_], writes=[aT])
                    kb.op("pe", lambda e: e.matmul(p_dec[:, 0:128], lhsT=cs.F("ones"), rhs=aT[:, :], start=True, stop=False), reads=[cs.f, aT], writes=[p_dec])
                    kb.op("pe", lambda e: e.matmul(p_dec[:, 0:128], lhsT=cs.F("ident"), rhs=cs.F(neg), start=False, stop=True), reads=[cs.f], writes=[p_dec])
                    kb.op("act", lambda e: e.activation(out=decT[:, :], in_=p_dec[:, 0:128], func=AF.Exp, bias=cum[:, 4 + h:5 + h]),
                          reads=[p_dec, cum], writes=[decT])
                    kb.op("dve", lambda e: e.tensor_tensor(out=WT[:, :], in0=p_sc[:, 0:128], in1=decT[:, :], op=ALU.mult), reads=[p_sc, decT], writes=[WT])
                    kb.op("dve", lambda e: e.tensor_scalar(out=Xdt[:, :], in0=X_[:, h * 64:(h + 1) * 64], scalar1=dta_[:, dcol + h:dcol + h + 1],
                                                           scalar2=None, op0=ALU.mult), reads=[X_, dta_], writes=[Xdt])
                    kb.op("dve", lambda e: e.tensor_scalar(out=Xw[:, :], in0=Xdt[:, :], scalar1=cum[:, 12 + h:13 + h], scalar2=None, op0=ALU.mult),
                          reads=[Xdt, cum], writes=[Xw])
                    kb.op("dve", lambda e: e.tensor_copy(out=hbf[:, :], in_=hs), reads=[hstate], writes=[hbf])
                    kb.op("pe", lambda e: e.matmul(p_yd[:, 0:64], lhsT=WT[:, :], rhs=Xdt[:, :], start=True, stop=True), reads=[WT, Xdt], writes=[p_yd])
                    kb.op("pe", lambda e: e.matmul(p_yo[:, 0:64], lhsT=CT_[:, :], rhs=hbf[:, :], start=True, stop=True), reads=[CT_, hbf], writes=[p_yo])
                    kb.op("pe", lambda e: e.matmul(p_st[:, 0:64], lhsT=B_[:, :], rhs=Xw[:, :], start=True, stop=True), reads=[B_, Xw], writes=[p_st])
                    yv = Yacc[:, c, h * 64:(h + 1) * 64]
                    if d == 0:
                        kb.op("dve", lambda e: e.scalar_tensor_tensor(out=ytmp[:, :], in0=X_[:, h * 64:(h + 1) * 64], scalar=dsk[:, h:h + 1],
                                                                      in1=p_yd[:, 0:64], op0=ALU.mult, op1=ALU.add), reads=[X_, dsk, p_yd], writes=[ytmp])
                    else:
                        kb.op("dve", lambda e: e.tensor_tensor(out=ytmp[:, :], in0=yv, in1=p_yd[:, 0:64], op=ALU.add), reads=[Yacc, p_yd], writes=[ytmp])
                    kb.op("dve", lambda e: e.scalar_tensor_tensor(out=yv, in0=p_yo[:, 0:64], scalar=cum[:, 8 + h:9 + h], in1=ytmp[:, :],
                                                                  op0=ALU.mult, op1=ALU.add), reads=[p_yo, cum, ytmp], writes=[Yacc])
                    kb.op("dve", lambda e: e.scalar_tensor_tensor(out=hs, in0=hs, scalar=tot[:, 4 + h:5 + h], in1=p_st[:, 0:64],
                                                                  op0=ALU.mult, op1=ALU.add), reads=[hstate, tot, p_st], writes=[hstate])
        kb.dma(Yd[:, :].rearrange("(c p) f -> p c f", p=128), Yacc[:, :, :], reads=[Yacc], writes=[Yd])


def attention(kb, cs, st, Tq, Tk, QT_d, KT_d, V_d, lam, O_d, QB=256):
    NK = Tk // 128
    NQ = Tq // 128
    with ExitStack() as s:
        QT = kb.sb(s, [128, Tq], BF16, "QT")
        KT = kb.sb(s, [128, Tk], BF16, "KT")
        Va = kb.sb(s, [128, NK, 129], BF16, "Va")
        Oall = kb.sb(s, [128, NQ, 128], F32, "Oall")
        if callable(QT_d):
            QT_d(QT)
            KT_d(KT)
            V_d(Va)
        else:
            for (t0, n) in [(i, min(4096, Tq - i)) for i in range(0, Tq, 4096)]:
                kb.dma(QT[:, t0:t0 + n], QT_d[:, t0:t0 + n], reads=[QT_d], writes=[QT.r(t0)])
            for (t0, n) in [(i, min(4096, Tk - i)) for i in range(0, Tk, 4096)]:
                kb.dma(KT[:, t0:t0 + n], KT_d[:, t0:t0 + n], reads=[KT_d], writes=[KT.r(t0)])
            for (k0, n) in [(i, min(32, NK - i)) for i in range(0, NK, 32)]:
                kb.dma(Va[:, k0:k0 + n, 0:128], V_d[k0 * 128:(k0 + n) * 128, :].rearrange("(k p) e -> p k e", p=128), reads=[V_d], writes=[Va.r(k0)])
        kb.op("pool", lambda e: e.memset(Va[:, :, 128:129], 1.0), writes=[Va.r("ones")])
        ps_s = [kb.ps(s, [128, 2, 512], F32, "ps_s") for _ in range(2)]
        acc = [[kb.ps(s, [128, 512], F32, "acc") for _ in range(QB // 128)] for _ in range(2)]
        pT = [kb.sb(s, [128, 2, 512], BF16, "pT") for _ in range(2)]
        rec = kb.sb(s, [128, 4], F32, "rec")
        o1 = kb.sb(s, [128, 128], F32, "o1")
        KP = 2
        assert QB == 256 and Tq % QB == 0 and NK % KP == 0
        it = 0
        for q0 in range(0, Tq, QB):
            nq = QB
            nqs = nq // 128
            for kp in range(0, NK, KP):
                pss, p_ = ps_s[it % 2], pT[it % 2]
                it += 1
                for kk in range(KP):
                    kt = kp + kk
                    for m in range(2):
                        kb.op("pe", lambda e: e.matmul(pss[:, m, kk * QB:(kk + 1) * QB], lhsT=KT[m * 64:(m + 1) * 64, kt * 128:(kt + 1) * 128],
                                                       rhs=QT[m * 64:(m + 1) * 64, q0:q0 + nq], start=True, stop=True),
                              reads=[KT, QT], writes=[pss], sig=(kk == KP - 1 and m == 1))
                kb.op("act", lambda e: e.activation(out=p_[:, :, :], in_=pss[:, :, :], func=AF.Exp, scale=0.125), reads=[pss], writes=[p_])
                for kk in range(KP):
                    kt = kp + kk
                    for m in range(2):
                        for qs in range(nqs):
                            c0 = kk * QB + qs * 128
                            kb.op("pe", lambda e: e.matmul(acc[m][qs][:, 0:129], lhsT=p_[:, m, c0:c0 + 128], rhs=Va[:, kt, :],
                                                           start=(kt == 0), stop=(kt == NK - 1)), reads=[p_, Va], writes=[acc[m][qs]],
                                  sig=(kk == KP - 1 and m == 1 and qs == nqs - 1))
            for qs in range(nqs):
                qt = q0 // 128 + qs
                kb.op("dve", lambda e: e.reciprocal(out=rec[:, 0:1], in_=acc[0][qs][:, 128:129]), reads=[acc[0][qs]], writes=[rec])
                kb.op("dve", lambda e: e.reciprocal(out=rec[:, 1:2], in_=acc[1][qs][:, 128:129]), reads=[acc[1][qs]], writes=[rec])
                kb.op("dve", lambda e: e.tensor_tensor(out=rec[:, 2:3], in0=rec[:, 1:2], in1=lam[:, 0:1], op=ALU.mult), reads=[rec, lam], writes=[rec])
                kb.op("dve", lambda e: e.tensor_scalar(out=o1[:, :], in0=acc[1][qs][:, 0:128], scalar1=rec[:, 2:3], scalar2=None, op0=ALU.mult),
                      reads=[acc[1][qs], rec], writes=[o1])
                kb.op("dve", lambda e: e.scalar_tensor_tensor(out=Oall[:, qt, :], in0=acc[0][qs][:, 0:128], scalar=rec[:, 0:1], in1=o1[:, :],
                                                              op0=ALU.mult, op1=ALU.subtract), reads=[acc[0][qs], rec, o1], writes=[Oall])
        kb.dma(O_d[:, :].rearrange("(k p) e -> p k e", p=128), Oall[:, :, :], reads=[Oall], writes=[O_d])


def compute_lam(kb, st, lamp_d, lam_init):
    lam = kb.sb(st, [128, 4], F32, "lam")
    with ExitStack() as s:
        lp = kb.sb(s, [128, 256], F32, "lp")
        junk = kb.sb(s, [128, 64], F32, "lj")
        kb.dma(lp[:, :], lamp_d[:, :], reads=[lamp_d], writes=[lp])
        kb.op("dve", lambda e: e.memset(lam[:, :], 0.0), writes=[lam])
        for i in range(2):
            kb.op("dve", lambda e: e.tensor_tensor(out=junk[:, :], in0=lp[:, i * 128:i * 128 + 64], in1=lp[:, i * 128 + 64:i * 128 + 128], op=ALU.mult),
                  reads=[lp], writes=[junk])
            kb.op("dve", lambda e: e.reduce_sum(out=lam[:, 1 + i:2 + i], in_=junk[:, :], axis=AX.X), reads=[junk], writes=[lam])
        kb.op("act", lambda e: e.activation(out=lam[:, 1:3], in_=lam[:, 1:3], func=AF.Exp), reads=[lam], writes=[lam])
        kb.op("dve", lambda e: e.tensor_tensor(out=lam[:, 3:4], in0=lam[:, 1:2], in1=lam[:, 2:3], op=ALU.subtract), reads=[lam], writes=[lam])
        kb.op("dve", lambda e: e.tensor_scalar(out=lam[:, 0:1], in0=lam[:, 3:4], scalar1=float(lam_init), scalar2=None, op0=ALU.add),
              reads=[lam], writes=[lam])
    return lam


def bcast_rows(kb, cs, ps, dst, col_of, n_t):
    with ExitStack() as s:
        dg = kb.sb(s, [128, 128], F32, "dg")
        for t in range(n_t):
            ap, buf = col_of(t)
            kb.op("dve", lambda e: e.tensor_scalar(out=dg[:, :], in0=cs.F("ident"), scalar1=ap, scalar2=None, op0=ALU.mult),
                  reads=[cs.f, buf], writes=[dg])
            kb.op("pe", lambda e: e.matmul(ps[:, 0:128], lhsT=cs.F("ones"), rhs=dg[:, :], start=True, stop=True), reads=[cs.f, dg], writes=[ps])
            kb.op("act", lambda e: e.copy(out=dst[:, t * 128:(t + 1) * 128], in_=ps[:, 0:128]), reads=[ps], writes=[dst])


def load_w_full(kb, wl, dst, w_d):
    for half in range(2):
        wb, _ = wl.load(w_d, 0, half * 512, 512)
        kb.op("pool", lambda e: e.tensor_copy(out=dst[:, :, half * 512:(half + 1) * 512], in_=wb[:, :, :]), reads=[wb], writes=[dst])


def phase_d(kb, cs, A, v, T, I, W, O, lam_init):
    mods, gw2 = A["mods"], A["gw2"]
    with ExitStack() as st:
        Wa = kb.sb(st, [128, 8, 1024], BF16, "Wa")
        Wb = kb.sb(st, [128, 8, 1024], BF16, "Wb")
        Wo = kb.sb(st, [128, 8, 1024], BF16, "Wo")
        with ExitStack() as sw:
            wl = WLoader(kb, sw)
            load_w_full(kb, wl, Wa, W["ssd_out"])
            load_w_full(kb, wl, Wb, W["diff_out"])
            load_w_full(kb, wl, Wo, W["w_o"])
        Wr = kb.sb(st, [128, 8, 16], F32, "Wr")
        kb.dma(Wr[:, :, :], W["router"][:, :].rearrange("(t p) c -> p t c", p=128), reads=[W["router"]], writes=[Wr])
        nwb = kb.sb(st, [128, 1024], F32, "nwb")
        swb = kb.sb(st, [128, 1024], F32, "swb")
        g1b = kb.sb(st, [128, 1024], F32, "g1b")
        kb.dma(nwb[:, :], W["nw"][0:1, :].to_broadcast([128, 1024]), reads=[W["nw"]], writes=[nwb])
        kb.dma(swb[:, :], W["sw"][0:1, :].to_broadcast([128, 1024]), reads=[W["sw"]], writes=[swb])
        kb.op("dve", lambda e: e.tensor_scalar(out=swb[:, :], in0=swb[:, :], scalar1=float(1.0 - lam_init), scalar2=None, op0=ALU.mult),
              reads=[swb], writes=[swb])
        psA = [kb.ps(st, [128, 512], F32, "dpsA") for _ in range(2)]
        psB = [kb.ps(st, [128, 512], F32, "dpsB") for _ in range(2)]
        ptb = [kb.ps(st, [128, 8, 128], BF16, "dptb") for _ in range(2)]
        bcast_rows(kb, cs, psA[0], g1b, lambda t: (mods[:, 16 + t, v:v + 1], mods), 8)
        ys = kb.sb(st, [128, 1024], F32, "ys")
        zs = kb.sb(st, [128, 1024], BF16, "zs")
        oa = kb.sb(st, [128, 1024], F32, "oa")
        yz = kb.sb(st, [128, 1024], F32, "yz")
        junk = kb.sb(st, [128, 1024], F32, "djunk")
        ynb = kb.sb(st, [128, 1024], BF16, "ynb")
        onb = kb.sb(st, [128, 1024], BF16, "onb")
        sm = kb.sb(st, [128, 32], F32, "sm")
        YnT = kb.sb(st, [128, 8, 512], BF16, "YnT")
        OnT = kb.sb(st, [128, 8, 512], BF16, "OnT")
        mT = kb.sb(st, [128, 8, 512], BF16, "mT")
        gt = [kb.sb(st, [128, 3, 512], BF16, "gt") for _ in range(2)]
        yc = [kb.sb(st, [128, 512], F32, "yc") for _ in range(2)]
        m1 = kb.sb(st, [128, 512], F32, "m1")
        m2 = kb.sb(st, [128, 512], F32, "m2")
        xt = kb.sb(st, [128, 1024], F32, "xt")
        xm = kb.sb(st, [128, 1024], F32, "xm")
        xn2 = kb.sb(st, [128, 1024], F32, "xn2")
        h2f = kb.sb(st, [128, 8, 128], F32, "h2f")
        h2b = kb.sb(st, [128, 8, 512], BF16, "h2b")
        af = kb.sb(st, [128, 16], F32, "af")
        afT = kb.sb(st, [16, 512], F32, "afT")
        k = 0
        for (t0, n) in blocks(0, T):
            nt = n // 128
            for j in range(nt):
                r0 = t0 + j * 128
                if "ldY" in I:
                    I["ldY"](ys, r0)
                    I["ldO"](oa, r0)
                else:
                    kb.dma(ys[:, :], I["YS"][r0:r0 + 128, :], reads=[I["YS"]], writes=[ys])
                    kb.dma(oa[:, :], I["Oa"][r0:r0 + 128, :], reads=[I["Oa"]], writes=[oa])
                kb.dma(zs[:, :], I["ZS"][r0:r0 + 128, :], reads=[I["ZS"]], writes=[zs])
                kb.op("dve", lambda e: e.tensor_tensor(out=yz[:, :], in0=ys[:, :], in1=zs[:, :], op=ALU.mult), reads=[ys, zs], writes=[yz])
                kb.op("dve", lambda e: e.memset(sm[:, :], 0.0), writes=[sm])
                for g in range(2):
                    kb.op("act", lambda e: e.activation(out=junk[:, g * 512:(g + 1) * 512], in_=yz[:, g * 512:(g + 1) * 512], func=AF.Square,
                                                        accum_out=sm[:, g:g + 1]), reads=[yz], writes=[junk, sm])
                kb.op("act", lambda e: e.activation(out=sm[:, 2:4], in_=sm[:, 0:2], func=AF.Sqrt, scale=1.0 / 512, bias=cs.eps[:, 0:1]),
                      reads=[sm, cs.eps], writes=[sm])
                kb.op("dve", lambda e: e.reciprocal(out=sm[:, 4:6], in_=sm[:, 2:4]), reads=[sm], writes=[sm])
                for g in range(2):
                    kb.op("dve", lambda e: e.scalar_tensor_tensor(out=ynb[:, g * 512:(g + 1) * 512], in0=yz[:, g * 512:(g + 1) * 512],
                                                                  scalar=sm[:, 4 + g:5 + g], in1=nwb[:, g * 512:(g + 1) * 512],
                                                                  op0=ALU.mult, op1=ALU.mult), reads=[yz, sm, nwb], writes=[ynb])
                kb.op("dve", lambda e: e.tensor_tensor(out=junk[:, :], in0=oa[:, :], in1=oa[:, :], op=ALU.mult), reads=[oa], writes=[junk])
                kb.op("dve", lambda e: e.reduce_sum(out=sm[:, 8:16], in_=junk[:, :].rearrange("p (h e) -> p h e", e=128), axis=AX.X),
                      reads=[junk], writes=[sm])
                kb.op("act", lambda e: e.activation(out=sm[:, 16:24], in_=sm[:, 8:16], func=AF.Sqrt, scale=1.0 / 128, bias=cs.eps[:, 0:1]),
                      reads=[sm, cs.eps], writes=[sm])
                kb.op("dve", lambda e: e.reciprocal(out=sm[:, 24:32], in_=sm[:, 16:24]), reads=[sm], writes=[sm])
                for h in range(8):
                    kb.op("dve", lambda e: e.scalar_tensor_tensor(out=onb[:, h * 128:(h + 1) * 128], in0=oa[:, h * 128:(h + 1) * 128],
                                                                  scalar=sm[:, 24 + h:25 + h], in1=swb[:, h * 128:(h + 1) * 128],
                                                                  op0=ALU.mult, op1=ALU.mult), reads=[oa, sm, swb], writes=[onb])
                for (src, dstT) in ((ynb, YnT), (onb, OnT)):
                    p_ = ptb[k % 2]
                    k += 1
                    for ct in range(8):
                        kb.op("pe", lambda e: e.transpose(out=p_[:, ct, :], in_=src[:, ct * 128:(ct + 1) * 128], identity=cs.B("ident")),
                              reads=[src, cs.b], writes=[p_], sig=(ct == 7))
                    kb.op("act", lambda e: e.copy(out=dstT[:, :, j * 128:(j + 1) * 128], in_=p_[:, :, :]), reads=[p_], writes=[dstT])
            for dm in range(8):
                g_, yc_ = gt[dm % 2], yc[dm % 2]
                for b3 in range(3):
                    kb.dma(g_[:, b3, 0:n], I["G"][(b3 * 8 + dm) * 128:(b3 * 8 + dm + 1) * 128, t0:t0 + n], reads=[I["G"]], writes=[g_])
                kb.dma(yc_[:, 0:n], I["YcT"][dm * 128:(dm + 1) * 128, t0:t0 + n], reads=[I["YcT"]], writes=[yc_])
                pa_, pb_ = psA[dm % 2], psB[dm % 2]
                for ct in range(8):
                    kb.op("pe", lambda e: e.matmul(pa_[:, 0:n], lhsT=Wa[:, ct, dm * 128:(dm + 1) * 128], rhs=YnT[:, ct, 0:n], start=(ct == 0), stop=(ct == 7)),
                          reads=[Wa, YnT], writes=[pa_], sig=(ct == 7))
                for ct in range(8):
                    kb.op("pe", lambda e: e.matmul(pb_[:, 0:n], lhsT=Wb[:, ct, dm * 128:(dm + 1) * 128], rhs=OnT[:, ct, 0:n], start=(ct == 0), stop=(ct == 7)),
                          reads=[Wb, OnT], writes=[pb_], sig=(ct == 7))
                kb.op("dve", lambda e: e.tensor_tensor(out=m1[:, 0:n], in0=pa_[:, 0:n], in1=g_[:, 0, 0:n], op=ALU.mult), reads=[pa_, g_], writes=[m1])
                kb.op("dve", lambda e: e.tensor_tensor(out=m2[:, 0:n], in0=pb_[:, 0:n], in1=g_[:, 1, 0:n], op=ALU.mult), reads=[pb_, g_], writes=[m2])
                kb.op("pool", lambda e: e.tensor_tensor(out=m1[:, 0:n], in0=m1[:, 0:n], in1=m2[:, 0:n], op=ALU.add), reads=[m1, m2], writes=[m1])
                kb.op("pool", lambda e: e.tensor_tensor(out=m2[:, 0:n], in0=yc_[:, 0:n], in1=g_[:, 2, 0:n], op=ALU.mult), reads=[yc_, g_], writes=[m2])
                kb.op("dve", lambda e: e.tensor_tensor(out=mT[:, dm, 0:n], in0=m1[:, 0:n], in1=m2[:, 0:n], op=ALU.add), reads=[m1, m2], writes=[mT])
            for j in range(nt):
                r0 = t0 + j * 128
                kb.dma(xt[:, :], I["xh"][HALO + r0:HALO + r0 + 128, :], reads=[I["xh"]], writes=[xt])
                for half in range(2):
                    ps = psA[half]
                    for ct in range(8):
                        kb.op("pe", lambda e: e.matmul(ps[:, :], lhsT=mT[:, ct, j * 128:(j + 1) * 128], rhs=Wo[:, ct, half * 512:(half + 1) * 512],
                                                       start=(ct == 0), stop=(ct == 7)), reads=[mT, Wo], writes=[ps], sig=(ct == 7))
                    hsl = slice(half * 512, (half + 1) * 512)
                    kb.op("dve", lambda e: e.tensor_tensor(out=xm[:, hsl], in0=ps[:, :], in1=g1b[:, hsl], op=ALU.mult), reads=[ps, g1b], writes=[xm])
                    kb.op("dve", lambda e: e.tensor_tensor(out=xm[:, hsl], in0=xm[:, hsl], in1=xt[:, hsl], op=ALU.add), reads=[xm, xt], writes=[xm])
                kb.dma(O["xmid"][r0:r0 + 128, :], xm[:, :], reads=[xm], writes=[O["xmid"].r(r0)])
                kb.op("dve", lambda e: e.memset(sm[:, 0:1], 0.0), writes=[sm])
                kb.op("act", lambda e: e.activation(out=junk[:, :], in_=xm[:, :], func=AF.Square, accum_out=sm[:, 0:1]), reads=[xm], writes=[junk, sm])
                kb.op("act", lambda e: e.activation(out=sm[:, 1:2], in_=sm[:, 0:1], func=AF.Sqrt, scale=1.0 / 1024, bias=cs.eps[:, 0:1]),
                      reads=[sm, cs.eps], writes=[sm])
                kb.op("dve", lambda e: e.reciprocal(out=sm[:, 2:3], in_=sm[:, 1:2]), reads=[sm], writes=[sm])
                kb.op("dve", lambda e: e.tensor_scalar(out=xn2[:, :], in0=xm[:, :], scalar1=sm[:, 2:3], scalar2=None, op0=ALU.mult),
                      reads=[xm, sm], writes=[xn2])
                for hh in range(2):
                    ps = psB[hh]
                    for c4 in range(4):
                        dt = hh * 4 + c4
                        kb.op("pe", lambda e: e.transpose(out=ps[:, c4 * 128:(c4 + 1) * 128], in_=xn2[:, dt * 128:(dt + 1) * 128], identity=cs.F("ident")),
                              reads=[xn2, cs.f], writes=[ps])
                    for c4 in range(4):
                        dt = hh * 4 + c4
                        kb.op("act", lambda e: e.activation(out=h2f[:, dt, :], in_=ps[:, c4 * 128:(c4 + 1) * 128], func=AF.Identity,
                                                            scale=gw2[:, dt, v:v + 1], bias=mods[:, 24 + dt, v:v + 1]), reads=[ps, gw2, mods], writes=[h2f])
                kb.op("pool", lambda e: e.tensor_copy(out=h2b[:, :, j * 128:(j + 1) * 128], in_=h2f[:, :, :]), reads=[h2f], writes=[h2b])
                pr = psA[0]
                for dt in range(8):
                    kb.op("pe", lambda e: e.matmul(pr[:, 0:16], lhsT=h2f[:, dt, :], rhs=Wr[:, dt, :], start=(dt == 0), stop=(dt == 7)),
                          reads=[h2f, Wr], writes=[pr], sig=(dt == 7))
                kb.op("dve", lambda e: e.reduce_max(out=sm[:, 3:4], in_=pr[:, 0:16], axis=AX.X), reads=[pr], writes=[sm])
                kb.op("dve", lambda e: e.tensor_scalar(out=sm[:, 4:5], in0=sm[:, 3:4], scalar1=-1.0, scalar2=None, op0=ALU.mult), reads=[sm], writes=[sm])
                kb.op("dve", lambda e: e.memset(sm[:, 5:6], 0.0), writes=[sm])
                kb.op("act", lambda e: e.activation(out=af[:, :], in_=pr[:, 0:16], func=AF.Exp, bias=sm[:, 4:5], accum_out=sm[:, 5:6]),
                      reads=[pr, sm], writes=[af, sm])
                kb.op("dve", lambda e: e.reciprocal(out=sm[:, 6:7], in_=sm[:, 5:6]), reads=[sm], writes=[sm])
                kb.op("dve", lambda e: e.tensor_scalar(out=af[:, :], in0=af[:, :], scalar1=sm[:, 6:7], scalar2=None, op0=ALU.mult), reads=[af, sm], writes=[af])
                kb.dma(O["aff"][r0:r0 + 128, :], af[:, :], reads=[af], writes=[O["aff"].r(r0)])
                pt2 = psA[1]
                kb.op("pe", lambda e: e.transpose(out=pt2[0:16, 0:128], in_=af[:, :], identity=cs.F("ident")), reads=[af, cs.f], writes=[pt2])
                kb.op("act", lambda e: e.copy(out=afT[:, j * 128:(j + 1) * 128], in_=pt2[0:16, 0:128]), reads=[pt2], writes=[afT])
            for dt in range(8):
                kb.dma(O["h2T"][dt * 128:(dt + 1) * 128, t0:t0 + n], h2b[:, dt, 0:n], reads=[h2b], writes=[O["h2T"].r((dt, t0))])
            kb.dma(O["affT"][:, t0:t0 + n], afT[:, 0:n], reads=[afT], writes=[O["affT"].r(t0)])


def phase_e(kb, cs, A, v, T, N, cap, I, W, O, final_w=None):
    mods = A["mods"]
    NT = T // 128
    M = N // 8
    with ExitStack() as st:
        psA = [kb.ps(st, [128, 512], F32, "epsA") for _ in range(2)]
        psB = [kb.ps(st, [128, 512], F32, "epsB") for _ in range(2)]
        psC = [kb.ps(st, [128, 512], F32, "epsC") for _ in range(2)]
        g2b = kb.sb(st, [128, 1024], F32, "g2b")
        bcast_rows(kb, cs, psA[0], g2b, lambda t: (mods[:, 40 + t, v:v + 1], mods), 8)
        gs = kb.sb(st, [128, NT, 16], F32, "gs")
        with ExitStack() as s:
            at = kb.sb(s, [128, M], F32, "at")
            jk = kb.sb(s, [128, M], F32, "jk")
            if "ld_at" in I:
                I["ld_at"](at)
            else:
                kb.dma(at[:, :], I["affT_all"][:, :].rearrange("e (r n) -> (e r) n", r=8), reads=[I["affT_all"]], writes=[at])
            b = kb.sb(s, [128, 8], F32, "bis")
            kb.op("dve", lambda e: e.memset(b[:, 0:1], 0.0), writes=[b])
            kb.op("dve", lambda e: e.memset(b[:, 1:2], 1.0), writes=[b])
            for it in range(34):
                kb.op("dve", lambda e: e.tensor_tensor(out=b[:, 5:6], in0=b[:, 0:1], in1=b[:, 1:2], op=ALU.add), reads=[b], writes=[b])
                kb.op("dve", lambda e: e.tensor_scalar(out=b[:, 2:3], in0=b[:, 5:6], scalar1=0.5, scalar2=None, op0=ALU.mult), reads=[b], writes=[b])
                kb.op("dve", lambda e: e.tensor_scalar(out=jk[:, :], in0=at[:, :], scalar1=b[:, 2:3], scalar2=None, op0=ALU.is_gt), reads=[at, b], writes=[jk])
                kb.op("dve", lambda e: e.reduce_sum(out=b[:, 3:4], in_=jk[:, :], axis=AX.X), reads=[jk], writes=[b])
                kb.op("pe", lambda e: e.matmul(psA[1][:, 0:1], lhsT=cs.F("blk8"), rhs=b[:, 3:4], start=True, stop=True), reads=[cs.f, b], writes=[psA[1]])
                kb.op("dve", lambda e: e.tensor_scalar(out=b[:, 4:5], in0=psA[1][:, 0:1], scalar1=float(cap) - 0.5, scalar2=None, op0=ALU.is_ge),
                      reads=[psA[1]], writes=[b])
                kb.op("dve", lambda e: e.tensor_tensor(out=b[:, 5:6], in0=b[:, 2:3], in1=b[:, 0:1], op=ALU.subtract), reads=[b], writes=[b])
                kb.op("dve", lambda e: e.scalar_tensor_tensor(out=b[:, 0:1], in0=b[:, 5:6], scalar=b[:, 4:5], in1=b[:, 0:1], op0=ALU.mult, op1=ALU.add),
                      reads=[b], writes=[b])
                kb.op("dve", lambda e: e.tensor_tensor(out=b[:, 5:6], in0=b[:, 1:2], in1=b[:, 2:3], op=ALU.subtract), reads=[b], writes=[b])
                kb.op("dve", lambda e: e.scalar_tensor_tensor(out=b[:, 1:2], in0=b[:, 5:6], scalar=b[:, 4:5], in1=b[:, 2:3], op0=ALU.mult, op1=ALU.add),
                      reads=[b], writes=[b])
            sl = kb.sb(s, [128, 16], F32, "sl")
            thr = kb.sb(s, [128, 16], F32, "thr")
            kb.op("dve", lambda e: e.tensor_scalar(out=sl[:, :], in0=cs.F("sel16", 128, 16), scalar1=b[:, 0:1], scalar2=None, op0=ALU.mult),
                  reads=[cs.f, b], writes=[sl])
            kb.op("pe", lambda e: e.matmul(psA[1][:, 0:16], lhsT=cs.F("ones"), rhs=sl[:, :], start=True, stop=True), reads=[cs.f, sl], writes=[psA[1]])
            kb.op("dve", lambda e: e.tensor_copy(out=thr[:, :], in_=psA[1][:, 0:16]), reads=[psA[1]], writes=[thr])
            afl = kb.sb(s, [128, NT, 16], F32, "afl")
            kb.dma(afl[:, :, :], I["aff"][:, :].rearrange("(t p) e -> p t e", p=128), reads=[I["aff"]], writes=[afl])
            for t in range(NT):
                kb.op("dve", lambda e: e.tensor_tensor(out=gs[:, t, :], in0=afl[:, t, :], in1=thr[:, :], op=ALU.is_gt), reads=[afl, thr], writes=[gs])
                kb.op("dve", lambda e: e.tensor_tensor(out=gs[:, t, :], in0=gs[:, t, :], in1=afl[:, t, :], op=ALU.mult), reads=[gs, afl], writes=[gs])
        TH = min(T, 1024)
        NTH = TH // 128
        h2T = kb.sb(st, [128, 8, TH], BF16, "eh2T")
        acc = kb.sb(st, [128, NTH, 1024], F32, "eacc")
        wl13 = WLoader(kb, st, kt=8, n=512, nbuf=3, nstg=2)
        wl2 = WLoader(kb, st, kt=4, n=1024, nbuf=2, nstg=1)
        s1 = kb.sb(st, [128, 512], F32, "es1")
        heT = [kb.sb(st, [128, 4, 512], BF16, "heT") for _ in range(2)]
        xm = kb.sb(st, [128, 1024], F32, "exm")
        xo = kb.sb(st, [128, 1024], F32, "exo")
        sm = kb.sb(st, [128, 4], F32, "esm")
        if final_w is not None:
            fwb = kb.sb(st, [128, 1024], F32, "fwb")
            kb.dma(fwb[:, :], final_w[0:1, :].to_broadcast([128, 1024]), reads=[final_w], writes=[fwb])
        k = 0
        for hh in range(T // TH):
            tb0 = hh * TH
            for dt in range(8):
                kb.dma(h2T[:, dt, :], I["h2T"][dt * 128:(dt + 1) * 128, tb0:tb0 + TH], reads=[I["h2T"]], writes=[h2T.r(dt)])
            kb.op("pool", lambda e: e.memset(acc[:, :, :], 0.0), writes=[acc])
            for ex in range(16):
                for fc in range(4):
                    w1, _ = wl13.load(W["w1"], ex * 1024, fc * 512, 512)
                    w3, _ = wl13.load(W["w3"], ex * 1024, fc * 512, 512)
                    w2, _ = wl2.load(W["w2"], ex * 2048 + fc * 512, 0, 1024)
                    for (t0, n) in blocks(0, TH):
                        he = heT[k % 2]
                        k += 1
                        for fi in range(4):
                            p1, p3 = psA[fi % 2], psB[fi % 2]
                            for dt in range(8):
                                kb.op("pe", lambda e: e.matmul(p1[:, 0:n], lhsT=w1[:, dt, fi * 128:(fi + 1) * 128], rhs=h2T[:, dt, t0:t0 + n],
                                                               start=(dt == 0), stop=(dt == 7)), reads=[w1, h2T], writes=[p1], sig=(dt == 7))
                            for dt in range(8):
                                kb.op("pe", lambda e: e.matmul(p3[:, 0:n], lhsT=w3[:, dt, fi * 128:(fi + 1) * 128], rhs=h2T[:, dt, t0:t0 + n],
                                                               start=(dt == 0), stop=(dt == 7)), reads=[w3, h2T], writes=[p3], sig=(dt == 7))
                            kb.op("act", lambda e: e.activation(out=s1[:, 0:n], in_=p1[:, 0:n], func=AF.Silu), reads=[p1], writes=[s1])
                            kb.op("dve", lambda e: e.tensor_tensor(out=he[:, fi, 0:n], in0=s1[:, 0:n], in1=p3[:, 0:n], op=ALU.mult), reads=[s1, p3], writes=[he])
                        for j in range(n // 128):
                            tl = t0 // 128 + j
                            tt = tb0 // 128 + tl
                            for half in range(2):
                                pc = psC[half]
                                for fi in range(4):
                                    kb.op("pe", lambda e: e.matmul(pc[:, :], lhsT=he[:, fi, j * 128:(j + 1) * 128], rhs=w2[:, fi, half * 512:(half + 1) * 512],
                                                                   start=(fi == 0), stop=(fi == 3)), reads=[he, w2], writes=[pc], sig=(fi == 3))
                                av = acc[:, tl, half * 512:(half + 1) * 512]
                                kb.op("dve", lambda e: e.scalar_tensor_tensor(out=av, in0=pc[:, :], scalar=gs[:, tt, ex:ex + 1], in1=av,
                                                                              op0=ALU.mult, op1=ALU.add), reads=[pc, gs, acc.r(tl)], writes=[acc.r(tl)])
            for tl in range(NTH):
                t = tb0 // 128 + tl
                kb.dma(xm[:, :], I["xmid"][t * 128:(t + 1) * 128, :], reads=[I["xmid"]], writes=[xm])
                kb.op("dve", lambda e: e.tensor_tensor(out=xo[:, :], in0=acc[:, tl, :], in1=g2b[:, :], op=ALU.mult), reads=[acc, g2b], writes=[xo])
                kb.op("dve", lambda e: e.tensor_tensor(out=xo[:, :], in0=xo[:, :], in1=xm[:, :], op=ALU.add), reads=[xo, xm], writes=[xo])
                if final_w is not None:
                    kb.op("dve", lambda e: e.memset(sm[:, 0:1], 0.0), writes=[sm])
                    kb.op("act", lambda e: e.activation(out=xm[:, :], in_=xo[:, :], func=AF.Square, accum_out=sm[:, 0:1]), reads=[xo], writes=[xm, sm])
                    kb.op("act", lambda e: e.activation(out=sm[:, 1:2], in_=sm[:, 0:1], func=AF.Sqrt, scale=1.0 / 1024, bias=cs.eps[:, 0:1]),
                          reads=[sm, cs.eps], writes=[sm])
                    kb.op("dve", lambda e: e.reciprocal(out=sm[:, 2:3], in_=sm[:, 1:2]), reads=[sm], writes=[sm])
                    kb.op("dve", lambda e: e.scalar_tensor_tensor(out=xo[:, :], in0=xo[:, :], scalar=sm[:, 2:3], in1=fwb[:, :], op0=ALU.mult, op1=ALU.mult),
                          reads=[xo, sm, fwb], writes=[xo])
                kb.dma(O["xout"][t * 128:(t + 1) * 128, :], xo[:, :], reads=[xo], writes=[O["xout"].r(t)])


import ml_dtypes
from concourse.bass_utils import run_bass_kernel_spmd

NPDT = {F32: np.float32, BF16: ml_dtypes.bfloat16, I32: np.int32}
NCORE = 8
TL = 2048
TC = 256
SEQ = 16384
DEPTH = 2


class Launch:
    def __init__(self):
        self.nc = bass.Bass("TRN2", target_bir_lowering=False)
        self.kb = KB(self.nc)
        self.ins = {}
        self.outs = {}

    def inp(self, name, shape, dtype=F32):
        b = self.kb.dram(name, shape, dtype, kind="ExternalInput")
        self.ins[name] = (shape, dtype)
        return b

    def out(self, name, shape, dtype=F32):
        b = self.kb.dram(name, shape, dtype, kind="ExternalOutput")
        self.outs[name] = (shape, dtype)
        return b

    def run(self, in_maps):
        self.kb.finish()
        maps = []
        for m in in_maps:
            mm = {}
            for k, (shape, dtype) in self.ins.items():
                a = np.ascontiguousarray(np.asarray(m[k]).astype(NPDT[dtype], copy=False))
                assert tuple(a.shape) == tuple(shape), (k, a.shape, shape)
                mm[k] = a
            maps.append(mm)
        res = run_bass_kernel_spmd(self.nc, maps, core_ids=list(range(len(maps))))
        return res.results


def rope_tables_host(pos0, n):
    t = np.arange(pos0, pos0 + n)
    row = (t // 64).astype(np.float32)
    col = (t % 64).astype(np.float32)
    freqs = (np.float32(10000.0) ** (-np.arange(0, 32, 2, dtype=np.float32) / np.float32(32))).astype(np.float32)
    ang = np.concatenate([row[:, None] * freqs, col[:, None] * freqs], axis=-1).astype(np.float32)
    cos, sin = np.cos(ang.astype(np.float64)), np.sin(ang.astype(np.float64))
    p = np.arange(128)
    axis = (p % 64) // 32
    half = (p % 32) // 16
    f = p % 16
    idx = axis * 16 + f
    COS = cos[:, idx].T
    SIN = sin[:, idx].T * np.where(half == 0, -1.0, 1.0)[:, None]
    return COS.astype(np.float32), SIN.astype(np.float32)


A_OUT = [("X", lambda T: [T, 1024], BF16), ("B", lambda T: [T, 256], BF16), ("BT", lambda T: [256, T], BF16), ("CT", lambda T: [256, T], BF16),
         ("DTA", lambda T: [T, 64], F32), ("KT", lambda T: [1024, T], BF16), ("QT", lambda T: [1024, T], BF16), ("V", lambda T: [T, 1024], BF16),
         ("ZS", lambda T: [T, 1024], BF16), ("G", lambda T: [3072, T], BF16), ("YcT", lambda T: [1024, T], F32)]
GATHERED = ["X", "B", "BT", "CT", "DTA", "KT", "QT", "V"]


def build_program():
    L = Launch()
    kb = L.kb
    nc = L.nc
    ds = bass.ds
    cc_d = L.inp("cc", [2, 1024])
    cst_d = L.inp("cst", list(CONST_ARR.shape))
    xl_d = L.inp("xl", [TL + 32, 1024]); hml_d = L.inp("hml", [128, 2])
    xc_d = L.inp("xc", [TC + 32, 1024]); hmc_d = L.inp("hmc", [128, 2])
    cos_d = L.inp("cos", [128, TL]); sin_d = L.inp("sin", [128, TL])
    fw_d = L.inp("fw", [1, 1024])
    LW = []
    for l in range(DEPTH):
        p = "L%d_" % l
        LW.append({"adaw": L.inp(p + "adaw", [1024, 6144]), "adab": L.inp(p + "adab", [6144]), "n1": L.inp(p + "n1", [1024]), "n2": L.inp(p + "n2", [1024]),
                   "w_in": L.inp(p + "w_in", [1024, NCOLS]), "convp": L.inp(p + "convp", [4, 1536]), "dtp": L.inp(p + "dtp", [32, 2]),
                   "confp": L.inp(p + "confp", [34, 1024]), "conf_out": L.inp(p + "conf_out", [1024, 1024]),
                   "dsk": L.inp(p + "dsk", [128, 2]), "lamp": L.inp(p + "lamp", [128, 256]),
                   "ssd_out": L.inp(p + "ssd_out", [1024, 1024]), "diff_out": L.inp(p + "diff_out", [1024, 1024]), "w_o": L.inp(p + "w_o", [1024, 1024]),
                   "nw": L.inp(p + "nw", [1, 1024]), "sw": L.inp(p + "sw", [1, 1024]), "router": L.inp(p + "router", [1024, 16]),
                   "w1": L.inp(p + "w1", [16 * 1024, 2048]), "w3": L.inp(p + "w3", [16 * 1024, 2048]), "w2": L.inp(p + "w2", [16 * 2048, 1024])})
    out_d = L.out("out", [TL, 1024])
    TT = TL + TC
    Ol = {nm: kb.dram("l_" + nm, shp(TL), dt) for nm, shp, dt in A_OUT}
    Oc = {nm: kb.dram("c_" + nm, shp(TC), dt) for nm, shp, dt in A_OUT}
    HB = {"X": ([8 * TT, 128], BF16, 8), "V": ([8 * TT, 128], BF16, 8), "B": ([2 * TT, 128], BF16, 2), "DTA": ([8 * TT, 8], F32, 8),
          "BT": ([2 * 128, TT], BF16, 2), "CT": ([2 * 128, TT], BF16, 2), "KT": ([8 * 128, TT], BF16, 8)}
    PERSONAL = ("X", "B", "DTA", "BT", "CT")
    hb, Gt, my = {}, {}, {}
    for nm, (shp, dt, nb) in HB.items():
        hb[nm] = kb.dram("hb_" + nm, shp, dt)
        Gt[nm] = kb.dram("g_" + nm, [8 * shp[0], shp[1]], dt)
        if nm in PERSONAL:
            my[nm] = kb.dram("my_" + nm, [8 * shp[0] // nb, shp[1]], dt)
    Yl_d = kb.dram("l_Y", [SEQ, 128], F32)
    Yc_d = kb.dram("c_Y", [TC, 128], F32)
    GY = kb.dram("g_Y", [8 * SEQ, 128], F32)
    myY = kb.dram("my_Y", [8 * TL, 128], F32)
    GYc = kb.dram("g_Yc", [8 * TC, 128], F32)
    Oa_l = kb.dram("l_Oa", [TL, 1024], F32)
    Oa_c = kb.dram("c_Oa", [TC, 1024], F32)
    Dl = {"xmid": kb.dram("l_xmid", [TL, 1024], F32), "h2T": kb.dram("l_h2T", [1024, TL], BF16), "aff": kb.dram("l_aff", [TL, 16], F32),
          "affT": kb.dram("l_affT", [16, TL], F32)}
    Dc = {"xmid": kb.dram("c_xmid", [TC, 1024], F32), "h2T": kb.dram("c_h2T", [1024, TC], BF16), "aff": kb.dram("c_aff", [TC, 16], F32),
          "affT": kb.dram("c_affT", [16, TC], F32)}
    GaffT = kb.dram("g_affT", [8 * 16, TL], F32)
    xh2 = kb.dram("xh2", [TL + 32, 1024], F32)
    xc2 = kb.dram("xc2", [TC + 32, 1024], F32)
    Eloc = kb.dram("e_loc", [32, 1024], F32)
    Eall = kb.dram("e_all", [8 * 32, 1024], F32)
    pid = nc.sync.partition_id()
    gid = pid // 4
    eL = ((pid + 7) % 8) * 32 + 16
    eR = ((pid + 1) % 8) * 32

    def fill_hb(S, t0, T):
        for j in range(8):
            kb.dma(hb["X"][j * TT + t0:j * TT + t0 + T, :], S["X"][:, j * 128:(j + 1) * 128], reads=[S["X"]], writes=[hb["X"].r((j, t0))])
            kb.dma(hb["V"][j * TT + t0:j * TT + t0 + T, :], S["V"][:, j * 128:(j + 1) * 128], reads=[S["V"]], writes=[hb["V"].r((j, t0))])
            kb.dma(hb["DTA"][j * TT + t0:j * TT + t0 + T, :].rearrange("t (k two) -> t k two", two=2),
                   S["DTA"].t.rearrange("t (k j two) -> t k j two", k=4, j=8)[:, :, j, :], reads=[S["DTA"]], writes=[hb["DTA"].r((j, t0))])
        for g in range(2):
            kb.dma(hb["B"][g * TT + t0:g * TT + t0 + T, :], S["B"][:, g * 128:(g + 1) * 128], reads=[S["B"]], writes=[hb["B"].r((g, t0))])
        for nm, nb in (("BT", 2), ("CT", 2), ("KT", 8)):
            kb.dma(hb[nm].t.rearrange("(g n) t -> g n t", g=nb)[:, :, t0:t0 + T], S[nm].t.rearrange("(g n) t -> g n t", g=nb),
                   reads=[S[nm]], writes=[hb[nm].r(t0)])

    def personalise():
        for nm, (shp, dt, nb) in HB.items():
            if nm not in PERSONAL:
                continue
            sel = pid if nb == 8 else gid
            src = Gt[nm].t.rearrange("(r j t) c -> r j (t c)", r=8, j=nb)[:, ds(sel, 1), :]
            dst = my[nm].t.rearrange("(r o t) c -> r o (t c)", r=8, o=1)
            kb.dma(dst, src, reads=[Gt[nm]], writes=[my[nm]])

    def srow(c, T):
        if T == TC:
            return 0, TL + c * 128, TL + c * 128
        r, cc_ = c // 16, c % 16
        return r, r * TT + cc_ * 128, cc_ * 128

    def make_load(T):
        def load(c, X_, B_, BT_, CT_, dta_):
            r, row, col = srow(c, T)
            kb.dma(X_[:, :], my["X"][row:row + 128, :], reads=[my["X"]], writes=[X_])
            kb.dma(B_[:, :], my["B"][row:row + 128, :], reads=[my["B"]], writes=[B_])
            kb.dma(BT_[:, :], my["BT"][r * 128:(r + 1) * 128, col:col + 128], reads=[my["BT"]], writes=[BT_])
            kb.dma(CT_[:, :], my["CT"][r * 128:(r + 1) * 128, col:col + 128], reads=[my["CT"]], writes=[CT_])
            kb.dma(dta_[:, :], my["DTA"][row:row + 128, :], reads=[my["DTA"]], writes=[dta_])
        return load

    def mk_c(h):
        hs_ = slice(h * 128, (h + 1) * 128)

        def ldQ(QT):
            kb.dma(QT[:, :], Oc["QT"][hs_, :], reads=[Oc["QT"]], writes=[QT])

        def ldK(KT):
            kb.dma(KT[:, :], Oc["KT"][hs_, :], reads=[Oc["KT"]], writes=[KT])

        def ldV(Va):
            kb.dma(Va[:, 0:2, 0:128], Oc["V"].t[:, hs_].rearrange("(k p) e -> p k e", p=128), reads=[Oc["V"]], writes=[Va.r(0)])
        return ldQ, ldK, ldV

    def mk_l(h):
        hs_ = slice(h * 128, (h + 1) * 128)

        def ldQ(QT):
            kb.dma(QT[:, :], Ol["QT"][hs_, :], reads=[Ol["QT"]], writes=[QT])

        def ldK(KT):
            kb.dma(KT[:, 0:TC], Oc["KT"][hs_, :], reads=[Oc["KT"]], writes=[KT.r("c")])
            for r in range(8):
                row = (r * 8 + h) * 128
                kb.dma(KT[:, TC + r * TL:TC + (r + 1) * TL], Gt["KT"][row:row + 128, 0:TL], reads=[Gt["KT"]], writes=[KT.r(r)])

        def ldV(Va):
            kb.dma(Va[:, 0:2, 0:128], Oc["V"].t[:, hs_].rearrange("(k p) e -> p k e", p=128), reads=[Oc["V"]], writes=[Va.r("c")])
            for r in range(8):
                row = (r * 8 + h) * TT
                kb.dma(Va[:, 2 + r * 16:2 + (r + 1) * 16, 0:128], Gt["V"][row:row + TL, :].rearrange("(k p) e -> p k e", p=128),
                       reads=[Gt["V"]], writes=[Va.r(r)])
        return ldQ, ldK, ldV
    myYv = myY.t.rearrange("(r t) c -> t r c", r=8)
    gyc = GYc.t.rearrange("(r t) c -> t r c", r=8)

    def ldYl(ys, r0):
        kb.dma(ys[:, :].rearrange("p (r c) -> p r c", r=8), myYv[r0:r0 + 128, :, :], reads=[myY], writes=[ys])

    def ldOl(oa, r0):
        kb.dma(oa[:, :], Oa_l[r0:r0 + 128, :], reads=[Oa_l], writes=[oa])

    def ldYc(ys, r0):
        kb.dma(ys[:, :].rearrange("p (r c) -> p r c", r=8), gyc[r0:r0 + 128, :, :], reads=[GYc], writes=[ys])

    def ldOc(oa, r0):
        kb.dma(oa[:, :], Oa_c[r0:r0 + 128, :], reads=[Oa_c], writes=[oa])
    ga = GaffT.t.rearrange("(r e) n -> e r n", e=16)

    def ld_at(at):
        for e_ in range(16):
            kb.dma(at[e_ * 8:(e_ + 1) * 8, :], ga[e_, :, :], reads=[GaffT], writes=[at.r(e_)])

    with ExitStack() as st0:
        cs = Consts(kb, st0, cst_d)
        for l in range(DEPTH):
            last = l == DEPTH - 1
            lam_init = 0.8 - 0.6 * math.exp(-0.3 * l)
            W = LW[l]
            xl_in = xl_d if l == 0 else xh2
            xc_in = xc_d if l == 0 else xc2
            with ExitStack() as st:
                A = phase_ada(kb, st, cs, cc_d, W["adaw"], W["adab"], W["n1"], W["n2"])
                phase_a(kb, cs, A, 1, TC, xc_in, hmc_d, W, Oc, rope=None, glu_split=1)
                phase_a(kb, cs, A, 0, TL, xl_in, hml_d, W, Ol, rope=(cos_d, sin_d), glu_split=2)
                fill_hb(Oc, TL, TC)
                fill_hb(Ol, 0, TL)
                for nm in HB:
                    kb.allgather(hb[nm], Gt[nm])
                personalise()
                hs = kb.sb(st, [128, 2, 2, 64], F32, "hstate")
                kb.op("dve", lambda e: e.memset(hs[:, :, :, :], 0.0), writes=[hs])
                ssd_scan(kb, cs, st, TC, {"load": make_load(TC), "dsk": W["dsk"]}, Yc_d, hs, True)
                ssd_scan(kb, cs, st, SEQ, {"load": make_load(SEQ), "dsk": W["dsk"]}, Yl_d, hs, False)
                lam_t = compute_lam(kb, st, W["lamp"], lam_init)
                kb.allgather(Yl_d, GY)
                for h in range(8):
                    hv = slice(h * 128, (h + 1) * 128)
                    if not last:
                        q_, k_, v_ = mk_c(h)
                        attention(kb, cs, st, TC, TC, q_, k_, v_, lam_t, Oa_c.view(Oa_c.t[:, hv]))
                    q_, k_, v_ = mk_l(h)
                    attention(kb, cs, st, TL, SEQ + TC, q_, k_, v_, lam_t, Oa_l.view(Oa_l.t[:, hv]))
                for (G_, m_) in ((GY, myY),):
                    kb.dma(m_.t.rearrange("(r o t) c -> r o (t c)", r=8, o=1),
                           G_.t.rearrange("(r j t) c -> r j (t c)", r=8, j=8)[:, ds(pid, 1), :], reads=[G_], writes=[m_])
                if not last:
                    kb.allgather(Yc_d, GYc)
                if not last:
                    phase_d(kb, cs, A, 1, TC, {"ldY": ldYc, "ldO": ldOc, "ZS": Oc["ZS"], "YcT": Oc["YcT"], "G": Oc["G"], "xh": xc_in}, W, Dc, lam_init)
                phase_d(kb, cs, A, 0, TL, {"ldY": ldYl, "ldO": ldOl, "ZS": Ol["ZS"], "YcT": Ol["YcT"], "G": Ol["G"], "xh": xl_in}, W, Dl, lam_init)
                kb.allgather(Dl["affT"], GaffT)
                if not last:
                    xoc = xc2.view(xc2.t[HALO:HALO + TC, :])
                    phase_e(kb, cs, A, 1, TC, TC, 2 * TC // 16, {"h2T": Dc["h2T"], "aff": Dc["aff"], "affT_all": Dc["affT"], "xmid": Dc["xmid"]},
                            W, {"xout": xoc}, final_w=None)
                    xol = xh2.view(xh2.t[HALO:HALO + TL, :])
                    phase_e(kb, cs, A, 0, TL, SEQ, 2 * SEQ // 16, {"h2T": Dl["h2T"], "aff": Dl["aff"], "ld_at": ld_at, "xmid": Dl["xmid"]},
                            W, {"xout": xol}, final_w=None)
                    zt = kb.sb(st, [16, 1024], F32, "zt")
                    kb.op("dve", lambda e: e.memset(zt[:, :], 0.0), writes=[zt])
                    kb.dma(xc2[0:HALO, :], zt[:, :], reads=[zt], writes=[xc2])
                    kb.dma(xc2[HALO + TC:HALO + TC + HALO, :], zt[:, :], reads=[zt], writes=[xc2])
                    kb.dma(Eloc[0:16, :], xh2[HALO:HALO + 16, :], reads=[xh2], writes=[Eloc])
                    kb.dma(Eloc[16:32, :], xh2[TL:TL + 16, :], reads=[xh2], writes=[Eloc])
                    kb.allgather(Eloc, Eall)
                    kb.dma(xh2[0:HALO, :], Eall[ds(eL, 16), :], reads=[Eall], writes=[xh2])
                    kb.dma(xh2[HALO + TL:HALO + TL + HALO, :], Eall[ds(eR, 16), :], reads=[Eall], writes=[xh2])
                else:
                    phase_e(kb, cs, A, 0, TL, SEQ, 2 * SEQ // 16, {"h2T": Dl["h2T"], "aff": Dl["aff"], "ld_at": ld_at, "xmid": Dl["xmid"]},
                            W, {"xout": out_d}, final_w=fw_d)
    return L


def kernel(x, c, ctx, c_ctx, ada_w, ada_b, norm1_w, norm2_w, w_in, ssd_conv_w, ssd_conv_b, ssd_dt_bias, ssd_a_log, ssd_d, ssd_norm_w,
           ssd_out, diff_lambda, diff_subln_w, diff_out, conf_dw_w, conf_dw_b, conf_ln_w, conf_ln_b, conf_out, w_o, router_w,
           exp_w1, exp_w3, exp_w2, final_norm_w):
    f32 = lambda a: np.asarray(a, np.float32)
    L = build_program()
    xpad = np.pad(f32(x)[0], ((HALO, HALO), (0, 0)))
    xcpad = np.pad(f32(ctx)[0], ((HALO, HALO), (0, 0)))
    base = {"cc": np.stack([f32(c)[0], f32(c_ctx)]), "cst": CONST_ARR, "xc": xcpad, "hmc": np.zeros((128, 2), np.float32),
            "fw": f32(final_norm_w)[None]}
    for l in range(DEPTH):
        p = "L%d_" % l
        base.update({p + "adaw": f32(ada_w)[l], p + "adab": f32(ada_b)[l], p + "n1": f32(norm1_w)[l], p + "n2": f32(norm2_w)[l],
                     p + "w_in": f32(w_in)[l], p + "convp": np.concatenate([f32(ssd_conv_w)[l], f32(ssd_conv_b)[l][None]], 0),
                     p + "dtp": np.stack([f32(ssd_dt_bias)[l].reshape(-1), f32(ssd_a_log)[l].reshape(-1)], 1),
                     p + "confp": np.concatenate([f32(conf_dw_w)[l], f32(conf_dw_b)[l][None], f32(conf_ln_w)[l][None], f32(conf_ln_b)[l][None]], 0),
                     p + "conf_out": f32(conf_out)[l],
                     p + "lamp": np.broadcast_to(f32(diff_lambda)[l].reshape(1, 256), (128, 256)),
                     p + "ssd_out": f32(ssd_out)[l], p + "diff_out": f32(diff_out)[l], p + "w_o": f32(w_o)[l], p + "nw": f32(ssd_norm_w)[l][None],
                     p + "sw": np.tile(f32(diff_subln_w)[l], 8)[None], p + "router": f32(router_w)[l],
                     p + "w1": f32(exp_w1)[l].reshape(-1, 2048), p + "w3": f32(exp_w3)[l].reshape(-1, 2048), p + "w2": f32(exp_w2)[l].reshape(-1, 1024)})
    maps = []
    for j in range(NCORE):
        cosj, sinj = rope_tables_host(j * TL, TL)
        hm = np.zeros((128, 2), np.float32)
        hm[:, 0] = 1.0 if j > 0 else 0.0
        hm[:, 1] = 1.0 if j < NCORE - 1 else 0.0
        m = {**base, "xl": xpad[j * TL:j * TL + TL + 32], "hml": hm, "cos": cosj, "sin": sinj}
        for l in range(DEPTH):
            m["L%d_dsk" % l] = np.broadcast_to(f32(ssd_d)[l][2 * j:2 * j + 2][None], (128, 2))
        maps.append(m)
    res = L.run(maps)
    return np.concatenate([res[j]["out"] for j in range(NCORE)], axis=0)[None].astype(np.float32)
```
